# Optimizing a Trainium2 kernel written in Bass

```python
import math
import jax, jax.numpy as jnp
from jax import lax
import numpy as np

D_MODEL = 1024
BATCH = 8
SEQ = 4096
DEPTH = 4

DA_HEADS = 4
DA_QK_DIM = 64
DA_V_DIM = 2 * DA_QK_DIM
DA_Q_W = DA_HEADS * 2 * DA_QK_DIM
DA_V_W = DA_HEADS * DA_V_DIM
Q_BLOCK = 128
ML_HEADS = 4
ML_QK_DIM = 64
ML_V_DIM = 128
ML_QK_W = ML_HEADS * ML_QK_DIM
ML_V_W = ML_HEADS * ML_V_DIM
CONV_WIDTH = 4
CHUNK = 64
MIX_W = DA_V_W + ML_V_W
IN_SPLITS = (DA_Q_W, DA_Q_W, DA_V_W, 2 * ML_QK_W, ML_V_W, ML_V_W)
IN_W = sum(IN_SPLITS) + 2 * ML_HEADS
N_EXPERTS = 32
TOP_K = 4
D_FF = 1024
SWIGLU_LIMIT = 7.0
SWIGLU_ALPHA = 1.702
MOE_BLOCK = 128
DN_ALPHA = (2 * DEPTH) ** 0.25
DN_BETA = (8 * DEPTH) ** -0.25
LN_EPS = 1e-5
RMS_EPS = 1e-5

kernel_name = "hybrid_diffattn_mlstm_moe_deepnorm"


def layer_norm(x, g, b):
    xf = x.astype(jnp.float32)
    mu = jnp.mean(xf, -1, keepdims=True)
    var = jnp.mean(jnp.square(xf - mu), -1, keepdims=True)
    return ((xf - mu) * lax.rsqrt(var + LN_EPS) * g + b).astype(x.dtype)


def rms_norm(x, g):
    xf = x.astype(jnp.float32)
    return (xf * lax.rsqrt(jnp.mean(xf * xf, -1, keepdims=True) + RMS_EPS) * g).astype(x.dtype)


def causal_depthwise_conv(x, w, b):
    y = lax.conv_general_dilated(x, w[:, None, :].astype(x.dtype), window_strides=(1,),
                                 padding=[(CONV_WIDTH - 1, 0)],
                                 dimension_numbers=('NWC', 'WIO', 'NWC'),
                                 feature_group_count=x.shape[-1])
    return y + b


def diff_attention(q, k, v, lam, lam_init, norm_g):
    B, S = q.shape[:2]
    nq = S // Q_BLOCK
    scale = DA_QK_DIM ** -0.5
    q_blocks = jnp.moveaxis(q.reshape(B, nq, Q_BLOCK, DA_HEADS, 2, DA_QK_DIM), 1, 0)
    k_pos = jnp.arange(S)

    def one_block(args):
        qb, blk = args
        s = jnp.einsum('bqhcd,bkhcd->bhcqk', qb, k).astype(jnp.float32) * scale
        q_pos = blk * Q_BLOCK + jnp.arange(Q_BLOCK)
        s = jnp.where(q_pos[:, None] >= k_pos[None, :], s, -jnp.inf)
        p = jax.nn.softmax(s, axis=-1)
        a = p[:, :, 0] - lam * p[:, :, 1]
        return jnp.einsum('bhqk,bkhe->bqhe', a.astype(v.dtype), v)

    o = lax.map(one_block, (q_blocks, jnp.arange(nq)))
    o = jnp.moveaxis(o, 0, 1).reshape(B, S, DA_HEADS, DA_V_DIM)
    o = rms_norm(o, norm_g) * (1.0 - lam_init)
    return o.reshape(B, S, DA_V_W)


def mlstm_chunkwise(q, k, v, i_pre, f_pre):
    B, S = q.shape[:2]
    nc = S // CHUNK
    f32 = jnp.float32

    def chunks(t):
        t = t.reshape((B, nc, CHUNK, ML_HEADS) + t.shape[3:])
        return jnp.moveaxis(t, 3, 1)

    q = chunks(q).astype(f32) * (ML_QK_DIM ** -0.5)
    k = chunks(k).astype(f32)
    v = chunks(v).astype(f32)
    ig = chunks(i_pre).astype(f32)
    lf = jax.nn.log_sigmoid(chunks(f_pre).astype(f32))
    bcum = jnp.cumsum(lf, axis=-1)
    g = bcum[..., -1]

    w_end = g[..., None] - bcum + ig
    m_loc = jnp.max(w_end, -1)
    e_end = jnp.exp(w_end - m_loc[..., None])
    C_loc = jnp.einsum('bhcs,bhcsv,bhcsk->bhcvk', e_end, v, k)
    n_loc = jnp.einsum('bhcs,bhcsk->bhck', e_end, k)

    def step(carry, xs):
        C, n, m = carry
        g_c, m_l, C_l, n_l = xs
        m_new = jnp.maximum(g_c + m, m_l)
        a = jnp.exp(g_c + m - m_new)
        bb = jnp.exp(m_l - m_new)
        C_new = a[..., None, None] * C + bb[..., None, None] * C_l
        n_new = a[..., None] * n + bb[..., None] * n_l
        return (C_new, n_new, m_new), (C, n, m)

    init = (jnp.zeros((B, ML_HEADS, ML_V_DIM, ML_QK_DIM), f32),
            jnp.zeros((B, ML_HEADS, ML_QK_DIM), f32),
            jnp.full((B, ML_HEADS), -jnp.inf, f32))
    xs = (jnp.moveaxis(g, 2, 0), jnp.moveaxis(m_loc, 2, 0),
          jnp.moveaxis(C_loc, 2, 0), jnp.moveaxis(n_loc, 2, 0))
    _, (C_prev, n_prev, m_prev) = lax.scan(step, init, xs)
    C_prev = jnp.moveaxis(C_prev, 0, 2)
    n_prev = jnp.moveaxis(n_prev, 0, 2)
    m_prev = jnp.moveaxis(m_prev, 0, 2)

    causal = jnp.tril(jnp.ones((CHUNK, CHUNK), bool))
    D = jnp.where(causal, bcum[..., :, None] - bcum[..., None, :] + ig[..., None, :], -jnp.inf)
    inter_log = bcum + m_prev[..., None]
    m_t = jnp.maximum(inter_log, jnp.max(D, -1))
    d_exp = jnp.exp(D - m_t[..., None])
    inter_w = jnp.exp(inter_log - m_t)
    s = jnp.einsum('bhctk,bhcsk->bhcts', q, k) * d_exp
    num = (inter_w[..., None] * jnp.einsum('bhcvk,bhctk->bhctv', C_prev, q)
           + jnp.einsum('bhcts,bhcsv->bhctv', s, v))
    den = inter_w * jnp.einsum('bhck,bhctk->bhct', n_prev, q) + jnp.sum(s, -1)
    h = num / jnp.maximum(jnp.abs(den), jnp.exp(-m_t))[..., None]
    return jnp.moveaxis(h, 1, 3).reshape(B, S, ML_HEADS, ML_V_DIM)


def mixer(x, w_in, conv_w, conv_b, gate_b, lam_q1, lam_k1, lam_q2, lam_k2, da_norm_g, w_out, lam_init):
    B, S, _ = x.shape
    proj = x @ w_in
    da_q, da_k, da_v, ml_qk, ml_v, ml_o, ml_g = jnp.split(proj, list(np.cumsum(IN_SPLITS)), axis=-1)
    lam = (jnp.exp(jnp.sum(lam_q1 * lam_k1).astype(jnp.float32))
           - jnp.exp(jnp.sum(lam_q2 * lam_k2).astype(jnp.float32)) + lam_init)
    da_out = diff_attention(da_q.reshape(B, S, DA_HEADS, 2, DA_QK_DIM),
                            da_k.reshape(B, S, DA_HEADS, 2, DA_QK_DIM),
                            da_v.reshape(B, S, DA_HEADS, DA_V_DIM), lam, lam_init, da_norm_g)
    ml_qk = jax.nn.silu(causal_depthwise_conv(ml_qk, conv_w, conv_b))
    ml_q, ml_k = jnp.split(ml_qk, 2, axis=-1)
    gates = ml_g + gate_b
    h = mlstm_chunkwise(ml_q.reshape(B, S, ML_HEADS, ML_QK_DIM),
                        ml_k.reshape(B, S, ML_HEADS, ML_QK_DIM),
                        ml_v.reshape(B, S, ML_HEADS, ML_V_DIM),
                        gates[..., :ML_HEADS], gates[..., ML_HEADS:])
    ml_out = (h.reshape(B, S, ML_V_W) * jax.nn.sigmoid(ml_o.astype(jnp.float32))).astype(x.dtype)
    return jnp.concatenate([da_out, ml_out], axis=-1) @ w_out


def moe(x, w_router, b_router, w_gu, b_gu, w_down, b_down):
    B, S, D = x.shape
    N = B * S
    A = N * TOP_K
    xt = x.reshape(N, D)
    logits = (xt @ w_router + b_router).astype(jnp.float32)
    top_v, top_e = lax.top_k(logits, TOP_K)
    gates = jax.nn.softmax(top_v, axis=-1)
    e_flat = top_e.reshape(A)
    tok_flat = jnp.arange(A, dtype=jnp.int32) // TOP_K
    order = jnp.argsort(e_flat)
    e_sorted = e_flat[order]
    tok_sorted = tok_flat[order]
    gate_sorted = gates.reshape(A)[order]
    counts = jnp.zeros((N_EXPERTS,), jnp.int32).at[e_flat].add(1)
    starts = jnp.cumsum(counts) - counts
    padded = (counts + MOE_BLOCK - 1) // MOE_BLOCK * MOE_BLOCK
    pad_ends = jnp.cumsum(padded)
    pad_starts = pad_ends - padded
    dest = pad_starts[e_sorted] + jnp.arange(A, dtype=jnp.int32) - starts[e_sorted]
    n_rows = A + N_EXPERTS * MOE_BLOCK
    n_blocks = n_rows // MOE_BLOCK
    row_tok = jnp.full((n_rows,), N, jnp.int32).at[dest].set(tok_sorted)
    x_pad = jnp.concatenate([xt, jnp.zeros((1, D), xt.dtype)], axis=0)
    xs = x_pad[row_tok].reshape(n_blocks, MOE_BLOCK, D)
    block_e = jnp.minimum(jnp.searchsorted(pad_ends, jnp.arange(n_blocks) * MOE_BLOCK, side='right'),
                          N_EXPERTS - 1)

    def expert_block(args):
        xb, e = args
        hgu = xb @ w_gu[e] + b_gu[e]
        gate = jnp.minimum(hgu[:, :D_FF], SWIGLU_LIMIT)
        up = jnp.clip(hgu[:, D_FF:], -SWIGLU_LIMIT, SWIGLU_LIMIT)
        glu = gate * jax.nn.sigmoid(SWIGLU_ALPHA * gate)
        return ((up + 1.0) * glu) @ w_down[e] + b_down[e]

    ys = lax.map(expert_block, (xs, block_e)).reshape(n_rows, D)
    y = jax.ops.segment_sum(ys[dest] * gate_sorted[:, None].astype(ys.dtype), tok_sorted, num_segments=N)
    return y.reshape(B, S, D)


def setup_inputs(seed: int = 0) -> dict:
    key = jax.random.key(seed)
    ks = jax.random.split(key, 22)
    nrm = jax.random.normal
    f32 = jnp.float32
    x = nrm(ks[0], (BATCH, SEQ, D_MODEL), f32)
    col_scale = np.ones((IN_W,), np.float32)
    off_v = 2 * DA_Q_W
    col_scale[off_v:off_v + DA_V_W] = DN_BETA
    off_mv = 2 * DA_Q_W + DA_V_W + 2 * ML_QK_W
    col_scale[off_mv:off_mv + ML_V_W] = DN_BETA
    w_in = nrm(ks[1], (DEPTH, D_MODEL, IN_W), f32) * (D_MODEL ** -0.5) * jnp.asarray(col_scale)
    conv_w = nrm(ks[2], (DEPTH, CONV_WIDTH, 2 * ML_QK_W), f32) * (CONV_WIDTH ** -0.5)
    conv_b = 0.01 * nrm(ks[3], (DEPTH, 2 * ML_QK_W), f32)
    gate_b = jnp.concatenate([
        0.1 * nrm(ks[4], (DEPTH, ML_HEADS), f32),
        jnp.linspace(3.0, 6.0, ML_HEADS, dtype=f32)[None, :] + 0.1 * nrm(ks[5], (DEPTH, ML_HEADS), f32)], axis=-1)
    lam_q1 = 0.1 * nrm(ks[6], (DEPTH, DA_QK_DIM), f32)
    lam_k1 = 0.1 * nrm(ks[7], (DEPTH, DA_QK_DIM), f32)
    lam_q2 = 0.1 * nrm(ks[8], (DEPTH, DA_QK_DIM), f32)
    lam_k2 = 0.1 * nrm(ks[9], (DEPTH, DA_QK_DIM), f32)
    da_norm_g = 1.0 + 0.01 * nrm(ks[10], (DEPTH, DA_V_DIM), f32)
    w_out = nrm(ks[11], (DEPTH, MIX_W, D_MODEL), f32) * (MIX_W ** -0.5) * DN_BETA
    ln1_g = 1.0 + 0.01 * nrm(ks[12], (DEPTH, D_MODEL), f32)
    ln1_b = 0.01 * nrm(ks[13], (DEPTH, D_MODEL), f32)
    w_router = nrm(ks[14], (DEPTH, D_MODEL, N_EXPERTS), f32) * (D_MODEL ** -0.5)
    b_router = 0.01 * nrm(ks[15], (DEPTH, N_EXPERTS), f32)
    w_gu = nrm(ks[16], (DEPTH, N_EXPERTS, D_MODEL, 2 * D_FF), f32) * (D_MODEL ** -0.5) * DN_BETA
    b_gu = 0.01 * nrm(ks[17], (DEPTH, N_EXPERTS, 2 * D_FF), f32)
    w_down = nrm(ks[18], (DEPTH, N_EXPERTS, D_FF, D_MODEL), f32) * (D_FF ** -0.5) * DN_BETA
    b_down = 0.01 * nrm(ks[19], (DEPTH, N_EXPERTS, D_MODEL), f32)
    ln2_g = 1.0 + 0.01 * nrm(ks[20], (DEPTH, D_MODEL), f32)
    ln2_b = 0.01 * nrm(ks[21], (DEPTH, D_MODEL), f32)
    return {"x": x, "w_in": w_in, "conv_w": conv_w, "conv_b": conv_b, "gate_b": gate_b,
            "lam_q1": lam_q1, "lam_k1": lam_k1, "lam_q2": lam_q2, "lam_k2": lam_k2,
            "da_norm_g": da_norm_g, "w_out": w_out, "ln1_g": ln1_g, "ln1_b": ln1_b,
            "w_router": w_router, "b_router": b_router, "w_gu": w_gu, "b_gu": b_gu,
            "w_down": w_down, "b_down": b_down, "ln2_g": ln2_g, "ln2_b": ln2_b}


def reference(x, w_in, conv_w, conv_b, gate_b, lam_q1, lam_k1, lam_q2, lam_k2, da_norm_g, w_out,
              ln1_g, ln1_b, w_router, b_router, w_gu, b_gu, w_down, b_down, ln2_g, ln2_b):
    for l in range(DEPTH):
        lam_init = 0.8 - 0.6 * math.exp(-0.3 * l)
        mix = mixer(x, w_in[l], conv_w[l], conv_b[l], gate_b[l], lam_q1[l], lam_k1[l], lam_q2[l],
                    lam_k2[l], da_norm_g[l], w_out[l], lam_init)
        x = layer_norm(DN_ALPHA * x + mix, ln1_g[l], ln1_b[l])
        ffn = moe(x, w_router[l], b_router[l], w_gu[l], b_gu[l], w_down[l], b_down[l])
        x = layer_norm(DN_ALPHA * x + ffn, ln2_g[l], ln2_b[l])
    return x
```

```python
import contextlib
import math
import numpy as np
import ml_dtypes
import concourse.bass as bass
import concourse.mybir as mybir
from concourse.bass_utils import run_bass_kernel_spmd

F32 = mybir.dt.float32
BF16 = mybir.dt.bfloat16
I32 = mybir.dt.int32
ALU = mybir.AluOpType
AF = mybir.ActivationFunctionType
AX = mybir.AxisListType

ENGS = ("pe", "act", "dve", "pool", "sp")
NDMASEM = 8
SEM_EPOCH = 30000
DMA_EPOCH = 1800

DEPTH = 4
T = 4096
NT = 32
D = 1024
KC = 8
IN_W = 3080
NE = 32
CAP = 640
NROWS = NE * CAP
DN_ALPHA = (2 * DEPTH) ** 0.25
LN_EPS = 1e-5
RMS_EPS = 1e-5


class Op:
    __slots__ = ("eng", "emit", "deps", "isdma", "sig", "sem", "val", "pre", "idx")


class Sched:
    def __init__(self, nc):
        self.nc = nc
        self.ops = {e: [] for e in ENGS}
        self.track = {}
        self.ndma = {e: 0 for e in ENGS}
        self.fence = []
        self.fenced = set(ENGS)

    def barrier(self):
        f = []
        for e in ENGS:
            last = None
            dm = []
            for op in reversed(self.ops[e]):
                if op.isdma:
                    if len(dm) < NDMASEM:
                        dm.append(op)
                elif last is None:
                    last = op
                if last is not None and len(dm) >= NDMASEM:
                    break
            if last is not None:
                f.append(last)
            f.extend(dm)
        self.fence = f
        self.fenced = set()

    def add(self, eng, emit, reads=(), writes=(), dma=False):
        op = Op()
        op.eng = eng
        op.emit = emit
        op.isdma = dma
        op.sig = False
        op.sem = None
        op.val = 0
        op.pre = None
        op.idx = len(self.ops[eng])
        deps = {}

        def need(p, raw):
            if p is op:
                return
            if p.isdma or p.eng != eng or dma:
                deps[id(p)] = p
            elif raw and eng != "pe":
                deps[id(p)] = p

        if eng not in self.fenced:
            self.fenced.add(eng)
            for p in self.fence:
                if p.isdma or p.eng != eng:
                    deps[id(p)] = p
        for k in reads:
            t = self.track.get(k)
            if t is None:
                t = [None, {}]
                self.track[k] = t
            if t[0] is not None:
                need(t[0], True)
        for k in writes:
            t = self.track.get(k)
            if t is None:
                t = [None, {}]
                self.track[k] = t
            if t[0] is not None:
                need(t[0], False)
            for r in t[1].values():
                need(r, False)
        for k in reads:
            t = self.track[k]
            key = (eng, op.idx) if dma else eng
            t[1][key] = op
        for k in writes:
            t = self.track[k]
            t[0] = op
            t[1] = {}
        op.deps = list(deps.values())
        if dma:
            j = self.ndma[eng]
            self.ndma[eng] = j + 1
            r = j // NDMASEM
            ep = r // DMA_EPOCH
            op.sem = ("dma", eng, j % NDMASEM, ep)
            op.val = 16 * (r % DMA_EPOCH + 1)
            if j >= NDMASEM:
                rp = r - 1
                op.pre = (("dma", eng, j % NDMASEM, rp // DMA_EPOCH), 16 * (rp % DMA_EPOCH + 1))
        self.ops[eng].append(op)
        return op

    def end_phase(self, final=False):
        nc = self.nc
        if not hasattr(self, "upto"):
            self.upto = {e: 0 for e in ENGS}
            self.semcount = {e: 0 for e in ENGS}
            self.sems = {}
            self.waited = {e: {} for e in ENGS}
        self.barrier()
        for p in self.fence:
            if not p.isdma:
                p.sig = True
        for e in ENGS:
            for op in self.ops[e][self.upto[e]:]:
                for p in op.deps:
                    if not p.isdma and p.idx >= self.upto[p.eng]:
                        p.sig = True
        for e in ENGS:
            c = self.semcount[e]
            for op in self.ops[e][self.upto[e]:]:
                if (not op.isdma) and op.sig:
                    op.sem = ("c", e, c // SEM_EPOCH)
                    op.val = c % SEM_EPOCH + 1
                    c += 1
            self.semcount[e] = c

        def getsem(k):
            if k not in self.sems:
                self.sems[k] = self.semstack.enter_context(nc.semaphore("s%d" % len(self.sems)))
            return self.sems[k]

        with nc.Block() as block:
            def replay(e, eng):
                waited = self.waited[e]
                for op in self.ops[e][self.upto[e]:]:
                    ws = [(p.sem, p.val) for p in op.deps if p.sem is not None]
                    if op.pre is not None:
                        ws.append(op.pre)
                    for s_, v in ws:
                        if waited.get(s_, 0) < v:
                            eng.wait_ge(getsem(s_), v)
                            waited[s_] = v
                    ins = op.emit(eng)
                    if op.isdma:
                        ins.then_inc(getsem(op.sem), 16)
                    elif op.sig:
                        ins.then_inc(getsem(op.sem), 1)
                    op.emit = None
                if final and e == "sp":
                    for q in ENGS:
                        dm = [op for op in self.ops[q] if op.isdma][-NDMASEM:]
                        for op in dm:
                            if waited.get(op.sem, 0) < op.val:
                                eng.wait_ge(getsem(op.sem), op.val)
                                waited[op.sem] = op.val
                self.upto[e] = len(self.ops[e])

            @block.tensor
            def _(eng):
                replay("pe", eng)

            @block.scalar
            def _(eng):
                replay("act", eng)

            @block.vector
            def _(eng):
                replay("dve", eng)

            @block.gpsimd
            def _(eng):
                replay("pool", eng)

            @block.sync
            def _(eng):
                replay("sp", eng)


class B:
    def __init__(self, nc):
        self.nc = nc
        self.S = Sched(nc)
        self.rr = 0

    def mm(self, out, lhsT, rhs, start=True, stop=True, r=(), w=()):
        self.S.add("pe", lambda e: e.matmul(out, lhsT, rhs, start=start, stop=stop), r, w)

    def tr(self, out, in_, ident, r=(), w=()):
        self.S.add("pe", lambda e: e.transpose(out, in_, ident), r, w)

    def act(self, out, in_, func, bias=0.0, scale=1.0, r=(), w=(), accum_out=None):
        if accum_out is None:
            self.S.add("act", lambda e: e.activation(out, in_, func, bias=bias, scale=scale), r, w)
        else:
            self.S.add("act", lambda e: e.activation(out, in_, func, bias=bias, scale=scale,
                                                     accum_out=accum_out), r, w)

    def ts(self, eng, out, in0, s1, s2, op0, op1=None, r=(), w=(), accum_out=None):
        if op1 is None:
            self.S.add(eng, lambda e: e.tensor_scalar(out, in0, s1, None, op0), r, w)
        elif accum_out is None:
            self.S.add(eng, lambda e: e.tensor_scalar(out, in0, s1, s2, op0, op1), r, w)
        else:
            self.S.add(eng, lambda e: e.tensor_scalar(out, in0, s1, s2, op0, op1, accum_out), r, w)

    def tt(self, eng, out, in0, in1, op, r=(), w=()):
        self.S.add(eng, lambda e: e.tensor_tensor(out, in0, in1, op), r, w)

    def stt(self, out, in0, scalar, in1, op0, op1, r=(), w=(), accum_out=None):
        if accum_out is None:
            self.S.add("dve", lambda e: e.scalar_tensor_tensor(out, in0, scalar, in1, op0, op1), r, w)
        else:
            self.S.add("dve", lambda e: e.scalar_tensor_tensor(out, in0, scalar, in1, op0, op1,
                                                               accum_out), r, w)

    def cp(self, eng, out, in_, r=(), w=()):
        if eng == "act":
            self.S.add("act", lambda e: e.copy(out, in_), r, w)
        else:
            self.S.add(eng, lambda e: e.tensor_copy(out, in_), r, w)

    def evac(self, out, in_, r=(), w=()):
        self.rr ^= 1
        self.cp("act" if self.rr else "dve", out, in_, r, w)

    def memset(self, eng, ap, val, r=(), w=()):
        self.S.add(eng, lambda e: e.memset(ap, val), r, w)

    def dma(self, eng, out, in_, r=(), w=(), nc_ok=False):
        if nc_ok:
            self.S.add(eng, lambda e: e.dma_start(out=out, in_=in_, allow_slow_non_contiguous=True),
                       r, w, dma=True)
        else:
            self.S.add(eng, lambda e: e.dma_start(out=out, in_=in_), r, w, dma=True)

    def scatter(self, out_dram, idx, in_sb, r=(), w=()):
        self.S.add("pool", lambda e: e.indirect_dma_start(
            out=out_dram, out_offset=bass.IndirectOffsetOnAxis(ap=idx, axis=0),
            in_=in_sb, in_offset=None), r, w, dma=True)

    def gather(self, out_sb, in_dram, idx, r=(), w=()):
        self.S.add("pool", lambda e: e.indirect_dma_start(
            out=out_sb, out_offset=None, in_=in_dram,
            in_offset=bass.IndirectOffsetOnAxis(ap=idx, axis=0),
            ), r, w, dma=True)


def host_consts():
    c = {}
    c["ident_bf"] = np.eye(128, dtype=np.float32).astype(ml_dtypes.bfloat16)
    c["ident_f"] = np.eye(128, dtype=np.float32)
    tri = (np.arange(128)[:, None] <= np.arange(128)[None, :]).astype(np.float32)
    c["tri_bf"] = tri.astype(ml_dtypes.bfloat16)
    c["tri_f"] = tri
    c["tris_bf"] = (np.arange(128)[:, None] < np.arange(128)[None, :]).astype(np.float32).astype(ml_dtypes.bfloat16)
    c["ones_bf"] = np.ones((128, 128), dtype=ml_dtypes.bfloat16)
    c["ones_f"] = np.ones((128, 128), dtype=np.float32)
    c["ebase"] = np.broadcast_to((NROWS - np.arange(NE) * CAP).astype(np.float32)[None, :], (128, NE)).copy()
    return c


CONST_SPECS = [("ident_bf", BF16, [128, 128]), ("ident_f", F32, [128, 128]), ("tri_bf", BF16, [128, 128]),
               ("tri_f", F32, [128, 128]), ("tris_bf", BF16, [128, 128]), ("ones_bf", BF16, [128, 128]),
               ("ones_f", F32, [128, 128]), ("ebase", F32, [128, NE])]

IN_SPECS = [("x", [T, D]), ("w_in", [DEPTH, D, IN_W]), ("conv_w", [DEPTH, 4, 512]), ("conv_b", [DEPTH, 512]),
            ("gate_b", [DEPTH, 8]), ("lam_q1", [DEPTH, 64]), ("lam_k1", [DEPTH, 64]), ("lam_q2", [DEPTH, 64]),
            ("lam_k2", [DEPTH, 64]), ("da_norm_g", [DEPTH, 128]), ("w_out", [DEPTH, D, D]),
            ("ln1_g", [DEPTH, D]), ("ln1_b", [DEPTH, D]), ("w_router", [DEPTH, D, NE]), ("b_router", [DEPTH, NE]),
            ("w_gu", [DEPTH, NE, D, 2048]), ("b_gu", [DEPTH, NE, 2048]), ("w_down", [DEPTH, NE, D, D]),
            ("b_down", [DEPTH, NE, D]), ("ln2_g", [DEPTH, D]), ("ln2_b", [DEPTH, D])]


def build(nlayers=DEPTH, phases=("da", "ml", "op", "ex", "cb"), debug=()):
    nc = bass.Bass("TRN2", target_bir_lowering=False)
    I = {}
    for name, shp in IN_SPECS:
        I[name] = nc.dram_tensor(name, shp, F32, kind="ExternalInput").ap()
    for name, dt, shp in CONST_SPECS:
        I[name] = nc.dram_tensor(name, shp, dt, kind="ExternalInput").ap()
    y_out = nc.dram_tensor("y", [T, D], F32, kind="ExternalOutput").ap()

    def scratch(name, shp, dt):
        kind = "ExternalOutput" if name in debug else "Internal"
        return nc.dram_tensor(name, shp, dt, kind=kind).ap()

    mixT_d = scratch("mixT_d", [D, T], BF16)
    x1_d = scratch("x1_d", [T, D], F32)
    xa_d = scratch("xa_d", [T, D], F32)
    xb_d = scratch("xb_d", [T, D], F32)
    xs_d = scratch("xs_d", [NROWS + 1, D], BF16)
    ys_d = scratch("ys_d", [NROWS + 1, D], F32)

    b = B(nc)
    S = b.S
    _cnt = [0]

    def uniq(name):
        _cnt[0] += 1
        return "sb%d_%s" % (_cnt[0], name)
    st = contextlib.ExitStack()
    with st:
        S.semstack = st
        def sb(name, shp, dt):
            return st.enter_context(nc.sbuf_tensor(uniq(name), shp, dt))

        ident_bf = sb("ident_bf", [128, 128], BF16)
        ident_f = sb("ident_f", [128, 128], F32)
        tri_bf = sb("tri_bf", [128, 128], BF16)
        tri_f = sb("tri_f", [128, 128], F32)
        tris_bf = sb("tris_bf", [128, 128], BF16)
        ones_bf = sb("ones_bf", [128, 128], BF16)
        ones_f = sb("ones_f", [128, 128], F32)
        ebase = sb("ebase", [128, NE], F32)
        for name, t_ in (("ident_bf", ident_bf), ("ident_f", ident_f), ("tri_bf", tri_bf), ("tri_f", tri_f),
                         ("tris_bf", tris_bf), ("ones_bf", ones_bf), ("ones_f", ones_f), ("ebase", ebase)):
            b.dma("sp", t_[:], I[name], w=["const"])
        idx_all = sb("idx_all", [128, NT, 4], I32)
        gate_all = sb("gate_all", [128, NT, 4], F32)
        with nc.sbuf_tensor("sb_zrow", [1, D], F32) as zrow:
            b.memset("dve", zrow[:], 0.0, w=["zrow"])
            b.dma("sp", ys_d[NROWS:NROWS + 1, :], zrow[:], r=["zrow"])

        ps = [st.enter_context(nc.psum_tensor("ps%d" % i, [128, 512], F32)) for i in range(8)]

        def PS(i):
            return ("ps", i)

        def stream_xT(xsrc, blk, xstage, xT_blk, tag, nbuf=2):
            buf = blk % nbuf
            for j in range(4):
                tt = blk * 4 + j
                sbuf_i = tt % 2
                b.dma("pool", xstage[:, sbuf_i, :], xsrc[tt * 128:(tt + 1) * 128, :],
                      w=[("xstage", sbuf_i)])
                pst = ps[7][:].bitcast(BF16)
                for kc in range(KC):
                    b.tr(pst[:, kc * 128:(kc + 1) * 128], xstage[:, sbuf_i, kc * 128:(kc + 1) * 128], ident_bf[:],
                         r=[("xstage", sbuf_i), "const"], w=[PS(7)])
                b.evac(xT_blk[:, buf, :, j * 128:(j + 1) * 128],
                       pst.rearrange("p (k t) -> p k t", k=KC), r=[PS(7)], w=[(tag, buf)])

        def layer_norm(xt, g_bc, b_bc, out, keyin, keyout, tmpstat, tmpmv, tmpr):
            b.S.add("dve", lambda e: e.bn_stats(tmpstat[:, 0, :], xt[:, 0:512]), [keyin], ["lnstat"])
            b.S.add("dve", lambda e: e.bn_stats(tmpstat[:, 1, :], xt[:, 512:1024]), [keyin], ["lnstat"])
            b.S.add("dve", lambda e: e.bn_aggr(tmpmv[:], tmpstat[:].rearrange("p a b -> p (a b)")), ["lnstat"], ["lnmv"])
            b.act(tmpr[:, 0:1], tmpmv[:, 1:2], AF.Sqrt, bias=epsc[:, 0:1], scale=1.0, r=["lnmv", "epsc"], w=["lnr0"])
            b.S.add("dve", lambda e: e.reciprocal(tmpr[:, 1:2], tmpr[:, 0:1]), ["lnr0"], ["lnr1"])
            b.ts("dve", xt, xt, tmpmv[:, 0:1], tmpr[:, 1:2], ALU.subtract, ALU.mult, r=[keyin, "lnmv", "lnr1"], w=[keyin])
            b.tt("pool", xt, xt, g_bc, ALU.mult, r=[keyin, "lnp"], w=[keyin])
            b.tt("pool", out, xt, b_bc, ALU.add, r=[keyin, "lnp"], w=[keyout])

        epsc = sb("epsc", [128, 2], F32)
        b.memset("dve", epsc[:, 0:1], LN_EPS, w=["epsc"])
        b.memset("dve", epsc[:, 1:2], 128.0 * RMS_EPS, w=["epsc"])

        xcur = I["x"]
        for l in range(nlayers):
            lam_init = 0.8 - 0.6 * math.exp(-0.3 * l)
            xnext = y_out if l == nlayers - 1 else (xa_d if l % 2 == 0 else xb_d)
            if "da" in phases:
                with contextlib.ExitStack() as ph:
                    def pb(name, shp, dt):
                        return ph.enter_context(nc.sbuf_tensor(uniq(name), shp, dt))
                    w_da = pb("w_da", [128, KC, 1536], BF16)
                    for kc in range(KC):
                        b.dma("pool", w_da[:, kc, :], I["w_in"][l, kc * 128:(kc + 1) * 128, 0:1536], w=["w_da"])
                    xstage = pb("xstage", [128, 2, D], BF16)
                    xT_blk = pb("xT_blk", [128, 2, KC, 512], BF16)
                    qT = pb("qT", [128, 4, T], BF16)
                    kT = pb("kT", [128, 4, T], BF16)
                    Vd = pb("Vd", [128, NT, 512], BF16)
                    lamt = pb("lamt", [128, 4, 64], F32)
                    lamv = pb("lamv", [128, 8], F32)
                    gcol = pb("gcol", [128, 2], F32)
                    for i, nm in enumerate(("lam_q1", "lam_k1", "lam_q2", "lam_k2")):
                        b.dma("sp", lamt[:, i, :], I[nm][l:l + 1, :].partition_broadcast(128), w=["lamt"])
                    b.dma("sp", gcol[:, 0:1], I["da_norm_g"][l:l + 1, :].rearrange("o p -> p o"), w=["gcol"], nc_ok=True)
                    b.tt("dve", lamt[:, 0, :], lamt[:, 0, :], lamt[:, 1, :], ALU.mult, r=["lamt"], w=["lamt"])
                    b.tt("dve", lamt[:, 2, :], lamt[:, 2, :], lamt[:, 3, :], ALU.mult, r=["lamt"], w=["lamt"])
                    b.S.add("dve", lambda e: e.reduce_sum(lamv[:, 0:1], lamt[:, 0, :], AX.X), ["lamt"], ["lamv"])
                    b.S.add("dve", lambda e: e.reduce_sum(lamv[:, 1:2], lamt[:, 2, :], AX.X), ["lamt"], ["lamv"])
                    b.act(lamv[:, 2:4], lamv[:, 0:2], AF.Exp, r=["lamv"], w=["lamv2"])
                    b.tt("dve", lamv[:, 4:5], lamv[:, 3:4], lamv[:, 2:3], ALU.subtract, r=["lamv2"], w=["lamv3"])
                    b.ts("dve", lamv[:, 5:6], lamv[:, 4:5], -lam_init, None, ALU.add, r=["lamv3"], w=["neglam"])
                    neglam = lamv[:, 5:6]
                    b.ts("dve", gcol[:, 1:2], gcol[:, 0:1], (1.0 - lam_init) * math.sqrt(128.0), None, ALU.mult,
                         r=["gcol"], w=["gcol2"])
                    for blk in range(8):
                        stream_xT(xcur, blk, xstage, xT_blk, "xT")
                        buf = blk % 2
                        for grp in range(8):
                            pa = ps[grp % 2]
                            for kc in range(KC):
                                b.mm(pa[:], w_da[:, kc, grp * 128:(grp + 1) * 128], xT_blk[:, buf, kc, :],
                                     start=(kc == 0), stop=(kc == KC - 1), r=["w_da", ("xT", buf)], w=[PS(grp % 2)])
                            dst = qT if grp < 4 else kT
                            b.evac(dst[:, grp % 4, blk * 512:(blk + 1) * 512], pa[:], r=[PS(grp % 2)],
                                   w=[("q" if grp < 4 else "k", blk)])
                        for j in range(4):
                            tt = blk * 4 + j
                            pa = ps[j % 2]
                            for kc in range(KC):
                                b.mm(pa[:], xT_blk[:, buf, kc, j * 128:(j + 1) * 128], w_da[:, kc, 1024:1536],
                                     start=(kc == 0), stop=(kc == KC - 1), r=["w_da", ("xT", buf)], w=[PS(j % 2)])
                            b.evac(Vd[:, tt, :], pa[:], r=[PS(j % 2)], w=[("v", tt)])
                    Pt = pb("Pt", [128, 3, 512], BF16)
                    Rc = pb("Rc", [128, 2, 512], F32)
                    rec = pb("rec", [128, 512], F32)
                    oc = pb("oc", [128, 512], F32)
                    sq = pb("sq", [128, 512], F32)
                    rs = pb("rs", [128, 512], F32)
                    ob = pb("ob", [128, 2, 512], BF16)
                    items = []
                    for Qb in range(8):
                        for h in range(4):
                            for c in range(2):
                                nk = (Qb + 1) * 4
                                for kt in range(nk):
                                    items.append((Qb, h, c, kt, nk))

                    def emit_S(i):
                        Qb, h, c, kt, nk = items[i]
                        diag = kt >= Qb * 4
                        q0 = (kt - Qb * 4) * 128 if diag else 0
                        n = 512 - q0
                        lo = c * 64
                        sbank = i % 2
                        b.mm(ps[sbank][:, 0:n], kT[lo:lo + 64, h, kt * 128:(kt + 1) * 128],
                             qT[lo:lo + 64, h, Qb * 512 + q0:(Qb + 1) * 512],
                             r=[("k", kt // 4), ("q", Qb)], w=[PS(sbank)])

                    emit_S(0)
                    for i, (Qb, h, c, kt, nk) in enumerate(items):
                        if i + 1 < len(items):
                            emit_S(i + 1)
                        it = (Qb * 4 + h) * 2 + c
                        Ob = 2 + 2 * (it % 2)
                        Lb = Ob + 1
                        diag = kt >= Qb * 4
                        q0 = (kt - Qb * 4) * 128 if diag else 0
                        n = 512 - q0
                        sbank = i % 2
                        pbuf = i % 3
                        b.act(Pt[:, pbuf, 0:n], ps[sbank][:, 0:n], AF.Exp, scale=0.125,
                              r=[PS(sbank)], w=[("Pt", pbuf)])
                        if diag:
                            b.tt("pool", Pt[:, pbuf, 0:128], Pt[:, pbuf, 0:128], tri_bf[:], ALU.mult,
                                 r=[("Pt", pbuf), "const"], w=[("Pt", pbuf)])
                        b.mm(ps[Ob][:, q0:512], Vd[:, kt, h * 128:(h + 1) * 128], Pt[:, pbuf, 0:n],
                             start=(kt == 0), stop=(kt == nk - 1), r=[("v", kt), ("Pt", pbuf)], w=[PS(Ob)])
                        b.mm(ps[Lb][:, q0:512], ones_bf[:], Pt[:, pbuf, 0:n],
                             start=(kt == 0), stop=(kt == nk - 1), r=["const", ("Pt", pbuf)], w=[PS(Lb)])
                        if kt != nk - 1:
                            continue
                        b.S.add("dve", lambda e, Lb=Lb: e.reciprocal(rec[:], ps[Lb][:]), [PS(Lb)], ["rec"])
                        b.tt("dve", Rc[:, c, :], ps[Ob][:], rec[:], ALU.mult, r=[PS(Ob), "rec"], w=[("Rc", c)])
                        if c != 1:
                            continue
                        b.stt(oc[:], Rc[:, 1, :], neglam, Rc[:, 0, :], ALU.mult, ALU.add,
                              r=[("Rc", 0), ("Rc", 1), "neglam"], w=["oc"])
                        b.act(sq[:], oc[:], AF.Square, r=["oc"], w=["sq"])
                        b.mm(ps[6][:], ones_f[:], sq[:], r=["sq", "const"], w=[PS(6)])
                        b.act(rs[:], ps[6][:], AF.Sqrt, bias=epsc[:, 1:2], r=[PS(6), "epsc"], w=["rs"])
                        b.S.add("dve", lambda e: e.reciprocal(rs[:], rs[:]), ["rs"], ["rs"])
                        ob_i = (Qb * 4 + h) % 2
                        b.stt(ob[:, ob_i, :], oc[:], gcol[:, 1:2], rs[:], ALU.mult, ALU.mult,
                              r=["oc", "rs", "gcol2"], w=[("ob", ob_i)])
                        b.dma("sp", mixT_d[h * 128:(h + 1) * 128, Qb * 512:(Qb + 1) * 512], ob[:, ob_i, :],
                              r=[("ob", ob_i)])
                    S.end_phase()

            if "ml" in phases:
                with contextlib.ExitStack() as ph:
                    def pb(name, shp, dt):
                        return ph.enter_context(nc.sbuf_tensor(uniq(name), shp, dt))
                    mq = pb("mq", [64, 4, T], BF16)
                    mk = pb("mk", [64, 4, T], BF16)
                    mv = pb("mv", [128, NT, 512], BF16)
                    mo = pb("mo", [128, 4, T], BF16)
                    gtm = pb("gtm", [128, NT, 8], F32)
                    lf = pb("lf", [128, NT, 4], F32)
                    btm = pb("btm", [128, NT, 4], F32)
                    gbc = pb("gbc", [128, NT, 4], F32)
                    ew = pb("ew", [128, NT, 4], F32)
                    ebias = pb("ebias", [128, NT, 4], F32)
                    eg = pb("eg", [128, NT, 4], F32)
                    nl8 = pb("nl8", [128, 2], F32)
                    b.memset("dve", nl8[:, 0:1], -math.log(8.0), w=["nl8"])
                    b.memset("dve", nl8[:, 1:2], 1.0, w=["nl8"])
                    with contextlib.ExitStack() as ph2:
                        def pb2(name, shp, dt):
                            return ph2.enter_context(nc.sbuf_tensor(uniq(name), shp, dt))
                        w_ml = pb2("w_ml", [128, KC, 1544], BF16)
                        for kc in range(KC):
                            b.dma("pool", w_ml[:, kc, :], I["w_in"][l, kc * 128:(kc + 1) * 128, 1536:3080], w=["w_ml"])
                        xstage = pb2("xstage", [128, 2, D], BF16)
                        xT_blk = pb2("xT_blk", [128, 1, KC, 512], BF16)
                        gb_bc = pb2("gb_bc", [128, 8], F32)
                        b.dma("sp", gb_bc[:], I["gate_b"][l:l + 1, :].partition_broadcast(128), w=["gb_bc"])
                        cw = pb2("cw", [64, 4, 8], F32)
                        cb = pb2("cb", [64, 8], F32)
                        for j in range(4):
                            b.dma("sp", cw[:, j, :], I["conv_w"][l, j:j + 1, :].rearrange("o (g p) -> p (o g)", p=64), w=["cw"], nc_ok=True)
                        b.dma("sp", cb[:], I["conv_b"][l:l + 1, :].rearrange("o (g p) -> p (o g)", p=64), w=["cw"], nc_ok=True)
                        cin = pb2("cin", [64, 8, 515], F32)
                        ctmp = pb2("ctmp", [64, 2, 512], F32)
                        b.memset("dve", cin[:, :, 0:3], 0.0, w=[("cin", g) for g in range(8)])
                        for blk in range(8):
                            stream_xT(xcur, blk, xstage, xT_blk, "xT", nbuf=1)
                            buf = 0
                            for grp in range(8):
                                pa = ps[grp % 2]
                                for kc in range(KC):
                                    b.mm(pa[0:64, :], w_ml[:, kc, grp * 64:(grp + 1) * 64], xT_blk[:, buf, kc, :],
                                         start=(kc == 0), stop=(kc == KC - 1), r=["w_ml", ("xT", buf)], w=[PS(grp % 2)])
                                b.cp("act", cin[:, grp, 3:515], pa[0:64, :], r=[PS(grp % 2)], w=[("cin", grp)])
                                ct = ctmp[:, grp % 2, :]
                                b.ts("dve", ct, cin[:, grp, 0:512], cw[:, 0, grp:grp + 1], cb[:, grp:grp + 1], ALU.mult, ALU.add,
                                     r=[("cin", grp), "cw"], w=[("ctmp", grp % 2)])
                                for j in range(1, 4):
                                    b.stt(ct, cin[:, grp, j:j + 512], cw[:, j, grp:grp + 1], ct, ALU.mult, ALU.add,
                                          r=[("cin", grp), "cw", ("ctmp", grp % 2)], w=[("ctmp", grp % 2)])
                                dst = mq if grp < 4 else mk
                                b.act(dst[:, grp % 4, blk * 512:(blk + 1) * 512], ct, AF.Silu,
                                      r=[("ctmp", grp % 2)], w=[("mqk", blk)])
                                b.cp("pool", cin[:, grp, 0:3], cin[:, grp, 512:515], r=[("cin", grp)], w=[("cin", grp)])
                            for h in range(4):
                                pa = ps[2 + h % 2]
                                for kc in range(KC):
                                    b.mm(pa[:], w_ml[:, kc, 1024 + h * 128:1024 + (h + 1) * 128], xT_blk[:, buf, kc, :],
                                         start=(kc == 0), stop=(kc == KC - 1), r=["w_ml", ("xT", buf)], w=[PS(2 + h % 2)])
                                b.act(mo[:, h, blk * 512:(blk + 1) * 512], pa[:], AF.Sigmoid, r=[PS(2 + h % 2)], w=[("mo", blk)])
                            for j in range(4):
                                tt = blk * 4 + j
                                pa = ps[4 + j % 2]
                                for kc in range(KC):
                                    b.mm(pa[:], xT_blk[:, buf, kc, j * 128:(j + 1) * 128], w_ml[:, kc, 512:1024],
                                         start=(kc == 0), stop=(kc == KC - 1), r=["w_ml", ("xT", buf)], w=[PS(4 + j % 2)])
                                b.evac(mv[:, tt, :], pa[:], r=[PS(4 + j % 2)], w=[("mv", tt)])
                                for kc in range(KC):
                                    b.mm(ps[6][:, 0:8], xT_blk[:, buf, kc, j * 128:(j + 1) * 128], w_ml[:, kc, 1536:1544],
                                         start=(kc == 0), stop=(kc == KC - 1), r=["w_ml", ("xT", buf)], w=[PS(6)])
                                b.tt("dve", gtm[:, tt, :], ps[6][:, 0:8], gb_bc[:], ALU.add, r=[PS(6), "gb_bc"], w=["gtm"])
                    S.barrier()
                    b.act(lf[:], gtm[:, :, 4:8], AF.Exp, scale=-1.0, r=["gtm"], w=["lf"])
                    b.act(lf[:], lf[:], AF.Ln, bias=nl8[:, 1:2], r=["lf", "nl8"], w=["lf"])
                    b.ts("dve", lf[:], lf[:], -1.0, None, ALU.mult, r=["lf"], w=["lf"])
                    lf2 = lf[:].rearrange("p a b -> p (a b)")
                    b.mm(ps[6][:, 0:128], tri_f[:], lf2, r=["lf", "const"], w=[PS(6)])
                    b.cp("dve", btm[:].rearrange("p a b -> p (a b)"), ps[6][:, 0:128], r=[PS(6)], w=["btm"])
                    b.mm(ps[6][:, 128:256], ones_f[:], lf2, r=["lf", "const"], w=[PS(6)])
                    b.cp("dve", gbc[:].rearrange("p a b -> p (a b)"), ps[6][:, 128:256], r=[PS(6)], w=["gbc"])
                    b.tt("dve", ebias[:], gtm[:, :, 0:4], btm[:], ALU.subtract, r=["gtm", "btm"], w=["ebias"])
                    b.tt("dve", ew[:], ebias[:], gbc[:], ALU.add, r=["ebias", "gbc"], w=["ew"])
                    b.act(ew[:], ew[:], AF.Exp, r=["ew"], w=["ew"])
                    b.ts("dve", ebias[:], ebias[:], -math.log(8.0), None, ALU.add, r=["ebias", "ew"], w=["ebias2"])
                    b.act(eg[:], gbc[:], AF.Exp, r=["gbc"], w=["eg"])
                    Cst = pb("Cst", [64, 4, 256], F32)
                    Cbf = pb("Cbf", [64, 4, 256], BF16)
                    b.memset("dve", Cst[:], 0.0, w=[("C", h) for h in range(4)])
                    b.memset("pool", Cbf[:], 0.0, w=[("Cbf", h) for h in range(4)])
                    lfd = pb("lfd", [128, 2, 4, 128], F32)
                    Et = pb("Et", [128, 2, 4, 128], F32)
                    Eq = pb("Eq", [64, 2, 4, 128], F32)
                    qs = pb("qs", [64, 2, 4, 128], BF16)
                    STt = pb("STt", [128, 2, 4, 128], BF16)
                    kh = pb("kh", [128, 2, 4, 64], BF16)
                    dm = pb("dm", [128, 2, 4, 128], F32)
                    hT = pb("hT", [128, 2, 4, 128], F32)
                    hst = pb("hst", [128, 2, 4, 512], BF16)
                    H4 = range(4)
                    for tt in range(NT):
                        c0 = tt * 128
                        u = tt % 2
                        blk = tt // 4
                        for h in H4:
                            b.ts("pool", lfd[:, u, h, :], ones_f[:], lf[:, tt, h:h + 1], None, ALU.mult,
                                 r=["lf", "const"], w=[("lfd", u, h)])
                        for h in H4:
                            b.mm(ps[0][:, h * 128:(h + 1) * 128], lfd[:, u, h, :], tri_f[:], r=[("lfd", u, h), "const"], w=[PS(0)])
                        for h in H4:
                            b.mm(ps[1][:, h * 128:(h + 1) * 128], mk[:, h, c0:c0 + 128], mq[:, h, c0:c0 + 128],
                                 r=[("mqk", blk)], w=[PS(1)])
                        pkt = ps[4][:].bitcast(BF16)
                        for h in H4:
                            b.tr(pkt[:, h * 64:(h + 1) * 64], mk[:, h, c0:c0 + 128], ident_bf[0:64, 0:64],
                                 r=[("mqk", blk), "const"], w=[PS(4)])
                        for h in H4:
                            b.act(Et[:, u, h, :], ps[0][:, h * 128:(h + 1) * 128], AF.Exp, bias=ebias[:, tt, h:h + 1],
                                  r=[PS(0), "ebias2"], w=[("Et", u, h)])
                        b.act(Eq[:, u, :, :], ps[0][0:64, :].rearrange("p (h t) -> p h t", h=4), AF.Exp, bias=nl8[0:64, 0:1],
                              r=[PS(0), "nl8"], w=[("Eq", u)])
                        b.tt("dve", qs[:, u, :, :], mq[:, :, c0:c0 + 128], Eq[:, u, :, :], ALU.mult,
                             r=[("mqk", blk), ("Eq", u)], w=[("qs", u)])
                        for h in H4:
                            b.tt("pool", Et[:, u, h, :], Et[:, u, h, :], tri_f[:], ALU.mult, r=[("Et", u, h), "const"], w=[("Et", u, h)])
                        b.tt("dve", STt[:, u, :, :], ps[1][:].rearrange("p (h t) -> p h t", h=4), Et[:, u, :, :], ALU.mult,
                             r=[PS(1)] + [("Et", u, h) for h in H4], w=[("ST", u)])
                        for h in H4:
                            b.ts("dve", kh[:, u, h, :], pkt[:, h * 64:(h + 1) * 64], ew[:, tt, h:h + 1], None, ALU.mult,
                                 r=[PS(4), "ew"], w=[("kh", u, h)])
                        for h in H4:
                            b.mm(ps[2][:, h * 128:(h + 1) * 128], mv[:, tt, h * 128:(h + 1) * 128], STt[:, u, h, :],
                                 start=True, stop=False, r=[("mv", tt), ("ST", u)], w=[PS(2)])
                            b.mm(ps[2][:, h * 128:(h + 1) * 128], Cbf[:, h, 0:128], qs[:, u, h, :],
                                 start=False, stop=True, r=[("Cbf", h), ("qs", u)], w=[PS(2)])
                        for h in H4:
                            b.mm(ps[3][:, h * 128:(h + 1) * 128], ones_bf[:], STt[:, u, h, :],
                                 start=True, stop=False, r=["const", ("ST", u)], w=[PS(3)])
                            b.mm(ps[3][:, h * 128:(h + 1) * 128], Cbf[:, h, 128:256], qs[:, u, h, :],
                                 start=False, stop=True, r=[("Cbf", h), ("qs", u)], w=[PS(3)])
                        for h in H4:
                            pu = ps[5 + h // 2]
                            o0 = (h % 2) * 256
                            b.mm(pu[0:64, o0:o0 + 128], kh[:, u, h, :], mv[:, tt, h * 128:(h + 1) * 128],
                                 r=[("kh", u, h), ("mv", tt)], w=[PS(5 + h // 2)])
                            b.mm(pu[0:64, o0 + 128:o0 + 256], kh[:, u, h, :], ones_bf[:], r=[("kh", u, h), "const"],
                                 w=[PS(5 + h // 2)])
                        b.act(dm[:, u, :, :], ps[3][:].rearrange("p (h t) -> p h t", h=4), AF.Abs,
                              r=[PS(3)], w=[("dm", u)])
                        b.ts("dve", dm[:, u, :, :], dm[:, u, :, :], 1.0, None, ALU.max, r=[("dm", u)], w=[("dm", u)])
                        b.S.add("dve", lambda e, u=u: e.reciprocal(dm[:, u, :, :], dm[:, u, :, :]), [("dm", u)], [("dm", u)])
                        b.tt("dve", hT[:, u, :, :], ps[2][:].rearrange("p (h t) -> p h t", h=4), dm[:, u, :, :], ALU.mult,
                             r=[PS(2), ("dm", u)], w=[("hT", u)])
                        hp = blk % 2
                        b.tt("pool", hst[:, hp, :, (tt % 4) * 128:(tt % 4 + 1) * 128], hT[:, u, :, :],
                             mo[:, :, c0:c0 + 128], ALU.mult, r=[("hT", u), ("mo", blk)], w=[("hst", hp)])
                        if tt % 4 == 3:
                            for h in H4:
                                b.dma("sp", mixT_d[512 + h * 128:512 + (h + 1) * 128, blk * 512:(blk + 1) * 512],
                                      hst[:, hp, h, :], r=[("hst", hp)])
                        for h in H4:
                            pu = ps[5 + h // 2]
                            o0 = (h % 2) * 256
                            b.stt(Cst[:, h, :], Cst[:, h, :], eg[0:64, tt, h:h + 1], pu[0:64, o0:o0 + 256], ALU.mult, ALU.add,
                                  r=[("C", h), "eg", PS(5 + h // 2)], w=[("C", h)])
                        b.cp("act", Cbf[:], Cst[:], r=[("C", h) for h in H4], w=[("Cbf", h) for h in H4])
                    S.end_phase()

            if "op" in phases:
                with contextlib.ExitStack() as ph:
                    def pb(name, shp, dt):
                        return ph.enter_context(nc.sbuf_tensor(uniq(name), shp, dt))
                    w_o = pb("w_o", [128, KC, D], BF16)
                    for kc in range(KC):
                        b.dma("pool", w_o[:, kc, :], I["w_out"][l, kc * 128:(kc + 1) * 128, :], w=["w_o"])
                    w_r = pb("w_r", [128, KC, NE], F32)
                    b.dma("sp", w_r[:], I["w_router"][l].rearrange("(k p) e -> p k e", p=128), w=["w_r"])
                    br_bc = pb("br_bc", [128, NE], F32)
                    b.dma("sp", br_bc[:], I["b_router"][l:l + 1, :].partition_broadcast(128), w=["w_r"])
                    g_bc = pb("g_bc", [128, D], F32)
                    b_bc = pb("b_bc", [128, D], F32)
                    b.dma("sp", g_bc[:], I["ln1_g"][l:l + 1, :].partition_broadcast(128), w=["lnp"])
                    b.dma("sp", b_bc[:], I["ln1_b"][l:l + 1, :].partition_broadcast(128), w=["lnp"])
                    mixb = pb("mixb", [128, 2, KC, 512], BF16)
                    xt = pb("xt", [128, 2, D], F32)
                    x1t = pb("x1t", [128, 2, D], F32)
                    x1b = pb("x1b", [128, 2, D], BF16)
                    x1T = pb("x1T", [128, KC, 128], F32)
                    tstat = pb("tstat", [128, 2, 6], F32)
                    tmv = pb("tmv", [128, 2], F32)
                    tr_ = pb("tr_", [128, 2], F32)
                    lg = pb("lg", [128, NE], F32)
                    m8 = pb("m8", [128, 8], F32)
                    msk = pb("msk", [128, NE], F32)
                    mskb = pb("mskb", [128, NE], BF16)
                    ex = pb("ex", [128, NE], F32)
                    G = pb("G", [128, NE], F32)
                    gs = pb("gs", [128, 4], F32)
                    cnt = pb("cnt", [128, NE], F32)
                    pos = pb("pos", [128, NE], F32)
                    okm = pb("okm", [128, NE], F32)
                    val = pb("val", [128, NE], F32)
                    v8 = pb("v8", [128, 8], F32)
                    idf = pb("idf", [128, 4], F32)
                    junk = pb("junk", [128, NE], F32)
                    b.memset("dve", cnt[:], 0.0, w=["cnt"])
                    for blk in range(8):
                        mb = blk % 2
                        b.dma("sp", mixb[:, mb, :, :], mixT_d[:, blk * 512:(blk + 1) * 512].rearrange("(k p) t -> p k t", p=128),
                              w=[("mixb", mb)])
                        for j in range(4):
                            tt = blk * 4 + j
                            u = tt % 2
                            b.dma("sp", xt[:, u, :], xcur[tt * 128:(tt + 1) * 128, :], w=[("xt", u)])
                            for half in range(2):
                                pa = ps[half]
                                for kc in range(KC):
                                    b.mm(pa[:], mixb[:, mb, kc, j * 128:(j + 1) * 128], w_o[:, kc, half * 512:(half + 1) * 512],
                                         start=(kc == 0), stop=(kc == KC - 1), r=[("mixb", mb), "w_o"], w=[PS(half)])
                                b.stt(xt[:, u, half * 512:(half + 1) * 512], xt[:, u, half * 512:(half + 1) * 512], DN_ALPHA,
                                      pa[:], ALU.mult, ALU.add, r=[("xt", u), PS(half)], w=[("xt", u)])
                            layer_norm(xt[:, u, :], g_bc[:], b_bc[:], x1t[:, u, :], ("xt", u), ("x1t", u), tstat, tmv, tr_)
                            b.dma("sp", x1_d[tt * 128:(tt + 1) * 128, :], x1t[:, u, :], r=[("x1t", u)])
                            b.cp("act", x1b[:, u, :], x1t[:, u, :], r=[("x1t", u)], w=[("x1b", u)])
                            for kc in range(KC):
                                b.tr(ps[2 + kc // 4][:, (kc % 4) * 128:(kc % 4 + 1) * 128], x1t[:, u, kc * 128:(kc + 1) * 128],
                                     ident_f[:], r=[("x1t", u), "const"], w=[PS(2 + kc // 4)])
                            b.cp("act", x1T[:, 0:4, :], ps[2][:].rearrange("p (k t) -> p k t", k=4), r=[PS(2)], w=["x1T"])
                            b.cp("dve", x1T[:, 4:8, :], ps[3][:].rearrange("p (k t) -> p k t", k=4), r=[PS(3)], w=["x1T"])
                            for kc in range(KC):
                                b.mm(ps[4][:, 0:NE], x1T[:, kc, :], w_r[:, kc, :], start=(kc == 0), stop=(kc == KC - 1),
                                     r=["x1T", "w_r"], w=[PS(4)])
                            b.tt("dve", lg[:], ps[4][:, 0:NE], br_bc[:], ALU.add, r=[PS(4), "w_r"], w=["lg"])
                            b.S.add("dve", lambda e: e.max(m8[:], lg[:]), ["lg"], ["m8"])
                            b.ts("dve", msk[:], lg[:], m8[:, 3:4], None, ALU.is_ge, r=["lg", "m8"], w=["msk"])
                            b.cp("pool", mskb[:], msk[:], r=["msk"], w=["mskb"])
                            b.ts("dve", ex[:], lg[:], m8[:, 0:1], None, ALU.subtract, r=["lg", "m8"], w=["ex"])
                            b.act(ex[:], ex[:], AF.Exp, r=["ex"], w=["ex"])
                            b.stt(G[:], ex[:], 1.0, msk[:], ALU.mult, ALU.mult, r=["ex", "msk"], w=["G"], accum_out=gs[:, 0:1])
                            b.S.add("dve", lambda e: e.reciprocal(gs[:, 1:2], gs[:, 0:1]), ["G"], ["gs"])
                            b.ts("dve", G[:], G[:], gs[:, 1:2], None, ALU.mult, r=["G", "gs"], w=["G"])
                            b.mm(ps[5][:, 0:NE], tris_bf[:], mskb[:], r=["mskb", "const"], w=[PS(5)])
                            b.mm(ps[5][:, NE:2 * NE], ones_bf[:], mskb[:], r=["mskb", "const"], w=[PS(5)])
                            b.tt("dve", pos[:], ps[5][:, 0:NE], cnt[:], ALU.add, r=[PS(5), "cnt"], w=["pos"])
                            b.tt("dve", cnt[:], ps[5][:, NE:2 * NE], cnt[:], ALU.add, r=[PS(5), "cnt", "pos"], w=["cnt"])
                            b.ts("dve", okm[:], pos[:], float(CAP), None, ALU.is_lt, r=["pos"], w=["okm"])
                            b.tt("dve", okm[:], okm[:], msk[:], ALU.mult, r=["okm", "msk"], w=["okm"])
                            b.tt("dve", val[:], ebase[:], pos[:], ALU.subtract, r=["pos", "const"], w=["val"])
                            b.tt("dve", val[:], val[:], okm[:], ALU.mult, r=["val", "okm"], w=["val"])
                            b.S.add("dve", lambda e: e.max(v8[:], val[:]), ["val"], ["v8"])
                            b.ts("dve", idf[:], v8[:, 0:4], -1.0, float(NROWS), ALU.mult, ALU.add, r=["v8"], w=["idf"])
                            b.cp("dve", idx_all[:, tt, :], idf[:], r=["idf"], w=[("idx", tt)])
                            for k in range(4):
                                b.stt(junk[:], val[:], v8[:, k:k + 1], G[:], ALU.is_equal, ALU.mult,
                                      r=["val", "v8", "G"], w=["junk", ("gate", tt)], accum_out=gate_all[:, tt, k:k + 1])
                                b.scatter(xs_d[:, :], idx_all[:, tt, k:k + 1], x1b[:, u, :],
                                          r=[("idx", tt), ("x1b", u)])
                    S.end_phase()

            if "ex" in phases:
                with contextlib.ExitStack() as ph:
                    def pb(name, shp, dt):
                        return ph.enter_context(nc.sbuf_tensor(uniq(name), shp, dt))
                    NJ = CAP // 128
                    wgu = pb("wgu", [128, 2, KC, 2048], BF16)
                    wdn = pb("wdn", [128, 2, KC, D], BF16)
                    bgu = pb("bgu", [128, 2, 16], F32)
                    bdn = pb("bdn", [128, 2, D], F32)
                    xst = pb("xst", [128, 2, D], BF16)
                    xsT = pb("xsT", [128, KC, CAP], BF16)
                    gsb = pb("gsb", [128, 2, CAP], F32)
                    sg = pb("sg", [128, 2, CAP], F32)
                    glu = pb("glu", [128, 8, CAP], BF16)
                    ub = pb("ub", [128, 2, CAP], F32)
                    aT = pb("aT", [128, 8, CAP], BF16)
                    yt = pb("yt", [128, 2, D], F32)

                    def load_w(e):
                        wb = e % 2
                        for kc in range(KC):
                            b.dma("pool", wgu[:, wb, kc, :], I["w_gu"][l, e, kc * 128:(kc + 1) * 128, :], w=[("wgu", wb)])
                        for kc in range(KC):
                            b.dma("pool", wdn[:, wb, kc, :], I["w_down"][l, e, kc * 128:(kc + 1) * 128, :], w=[("wdn", wb)])
                        b.dma("sp", bgu[:, wb, :], I["b_gu"][l, e:e + 1, :].rearrange("o (c p) -> p (o c)", p=128),
                              w=[("bgu", wb)], nc_ok=True)
                        b.dma("sp", bdn[:, wb, :], I["b_down"][l, e:e + 1, :].partition_broadcast(128), w=[("bdn", wb)])

                    load_w(0)
                    halves = [(0, 512), (512, CAP)]
                    for e in range(NE):
                        wb = e % 2
                        if e + 1 < NE:
                            load_w(e + 1)
                        for j in range(NJ):
                            u = j % 2
                            b.dma("sp", xst[:, u, :], xs_d[e * CAP + j * 128:e * CAP + (j + 1) * 128, :],
                                  w=[("xst", u)])
                            pst = ps[7][:].bitcast(BF16)
                            for kc in range(KC):
                                b.tr(pst[:, kc * 128:(kc + 1) * 128], xst[:, u, kc * 128:(kc + 1) * 128], ident_bf[:],
                                     r=[("xst", u), "const"], w=[PS(7)])
                            b.evac(xsT[:, :, j * 128:(j + 1) * 128], pst.rearrange("p (k t) -> p k t", k=KC),
                                   r=[PS(7)], w=["xsT"])
                        for fc in range(16):
                            u = fc % 2
                            for hi, (c0, c1) in enumerate(halves):
                                pa = ps[(fc % 2) * 2 + hi]
                                for kc in range(KC):
                                    b.mm(pa[:, 0:c1 - c0], wgu[:, wb, kc, fc * 128:(fc + 1) * 128], xsT[:, kc, c0:c1],
                                         start=(kc == 0), stop=(kc == KC - 1), r=[("wgu", wb), "xsT"],
                                         w=[PS((fc % 2) * 2 + hi)])
                                if fc < 8:
                                    b.ts("dve", gsb[:, u, c0:c1], pa[:, 0:c1 - c0], bgu[:, wb, fc:fc + 1], 7.0, ALU.add, ALU.min,
                                         r=[PS((fc % 2) * 2 + hi), ("bgu", wb)], w=[("gsb", u)])
                                else:
                                    b.ts("dve", ub[:, u, c0:c1], pa[:, 0:c1 - c0], bgu[:, wb, fc:fc + 1], 7.0, ALU.add, ALU.min,
                                         r=[PS((fc % 2) * 2 + hi), ("bgu", wb)], w=[("ub", u)])
                            if fc < 8:
                                b.act(sg[:, u, :], gsb[:, u, :], AF.Sigmoid, scale=1.702, r=[("gsb", u)], w=[("sg", u)])
                                b.tt("pool", glu[:, fc, :], gsb[:, u, :], sg[:, u, :], ALU.mult,
                                     r=[("gsb", u), ("sg", u)], w=[("glu", fc)])
                            else:
                                b.ts("pool", ub[:, u, :], ub[:, u, :], -7.0, 1.0, ALU.max, ALU.add, r=[("ub", u)], w=[("ub", u)])
                                b.tt("pool", aT[:, fc - 8, :], ub[:, u, :], glu[:, fc - 8, :], ALU.mult,
                                     r=[("ub", u), ("glu", fc - 8)], w=[("aT", fc - 8)])
                        for j in range(NJ):
                            u = j % 2
                            for half in range(2):
                                pa = ps[4 + half]
                                for fc in range(8):
                                    b.mm(pa[:], aT[:, fc, j * 128:(j + 1) * 128], wdn[:, wb, fc, half * 512:(half + 1) * 512],
                                         start=(fc == 0), stop=(fc == 7), r=[("aT", fc), ("wdn", wb)], w=[PS(4 + half)])
                                b.tt("dve", yt[:, u, half * 512:(half + 1) * 512], pa[:], bdn[:, wb, half * 512:(half + 1) * 512],
                                     ALU.add, r=[PS(4 + half), ("bdn", wb)], w=[("yt", u)])
                            b.dma("sp", ys_d[e * CAP + j * 128:e * CAP + (j + 1) * 128, :], yt[:, u, :],
                                  r=[("yt", u)])
                    S.end_phase()

            if "cb" in phases:
                with contextlib.ExitStack() as ph:
                    def pb(name, shp, dt):
                        return ph.enter_context(nc.sbuf_tensor(uniq(name), shp, dt))
                    g_bc = pb("g2_bc", [128, D], F32)
                    b_bc = pb("b2_bc", [128, D], F32)
                    b.dma("sp", g_bc[:], I["ln2_g"][l:l + 1, :].partition_broadcast(128), w=["lnp"])
                    b.dma("sp", b_bc[:], I["ln2_b"][l:l + 1, :].partition_broadcast(128), w=["lnp"])
                    xt = pb("xt2", [128, 2, D], F32)
                    rows = pb("rows", [128, 2, 4, D], F32)
                    xo = pb("xo", [128, 2, D], F32)
                    tstat = pb("tstat2", [128, 2, 6], F32)
                    tmv = pb("tmv2", [128, 2], F32)
                    tr_ = pb("tr2_", [128, 2], F32)
                    for tt in range(NT):
                        u = tt % 2
                        b.dma("sp", xt[:, u, :], x1_d[tt * 128:(tt + 1) * 128, :], w=[("xt", u)])
                        for k in range(4):
                            b.gather(rows[:, u, k, :], ys_d[:, :], idx_all[:, tt, k:k + 1],
                                     r=[("idx", tt)], w=[("rows", u, k)])
                        b.ts("dve", xt[:, u, :], xt[:, u, :], DN_ALPHA, None, ALU.mult, r=[("xt", u)], w=[("xt", u)])
                        for k in range(4):
                            b.stt(xt[:, u, :], rows[:, u, k, :], gate_all[:, tt, k:k + 1], xt[:, u, :], ALU.mult, ALU.add,
                                  r=[("rows", u, k), ("gate", tt), ("xt", u)], w=[("xt", u)])
                        layer_norm(xt[:, u, :], g_bc[:], b_bc[:], xo[:, u, :], ("xt", u), ("xo", u), tstat, tmv, tr_)
                        b.dma("sp", xnext[tt * 128:(tt + 1) * 128, :], xo[:, u, :], r=[("xo", u)])
                    S.end_phase()
            xcur = xnext
        S.end_phase(final=True)
    return nc


_CACHE = {}


def kernel(**inputs):
    if "nc" not in _CACHE:
        _CACHE["nc"] = build()
    nc = _CACHE["nc"]
    consts = host_consts()
    x = np.ascontiguousarray(inputs["x"], dtype=np.float32)
    ncores = x.shape[0]
    shared = {k: np.ascontiguousarray(np.asarray(v, dtype=np.float32)) for k, v in inputs.items() if k != "x"}
    in_maps = []
    for c in range(ncores):
        m = dict(shared)
        m.update(consts)
        m["x"] = x[c]
        in_maps.append(m)
    res = run_bass_kernel_spmd(nc, in_maps, core_ids=list(range(ncores)))
    return np.stack([r["y"] for r in res.results], axis=0).astype(np.float32)
```

```python
import contextlib
import math
import numpy as np
import ml_dtypes
import concourse.bass as bass
import concourse.mybir as mybir
from concourse.bass_utils import run_bass_kernel_spmd

F32 = mybir.dt.float32
BF16 = mybir.dt.bfloat16
I32 = mybir.dt.int32
ALU = mybir.AluOpType
AF = mybir.ActivationFunctionType
AX = mybir.AxisListType

ENGS = ("pe", "act", "dve", "pool", "sp")
NDMASEM = 8
SEM_EPOCH = 30000
DMA_EPOCH = 1800

DEPTH = 4
T = 4096
NT = 32
D = 1024
KC = 8
IN_W = 3080
NE = 32
CAP = 640
NROWS = NE * CAP
DN_ALPHA = (2 * DEPTH) ** 0.25
LN_EPS = 1e-5
RMS_EPS = 1e-5


class Op:
    __slots__ = ("eng", "emit", "deps", "isdma", "sig", "sem", "val", "pre", "idx")


class Sched:
    def __init__(self, nc):
        self.nc = nc
        self.ops = {e: [] for e in ENGS}
        self.track = {}
        self.ndma = {e: 0 for e in ENGS}
        self.fence = []
        self.fenced = set(ENGS)

    def barrier(self):
        f = []
        for e in ENGS:
            last = None
            dm = []
            for op in reversed(self.ops[e]):
                if op.isdma:
                    if len(dm) < NDMASEM:
                        dm.append(op)
                elif last is None:
                    last = op
                if last is not None and len(dm) >= NDMASEM:
                    break
            if last is not None:
                f.append(last)
            f.extend(dm)
        self.fence = f
        self.fenced = set()

    def add(self, eng, emit, reads=(), writes=(), dma=False):
        op = Op()
        op.eng = eng
        op.emit = emit
        op.isdma = dma
        op.sig = False
        op.sem = None
        op.val = 0
        op.pre = None
        op.idx = len(self.ops[eng])
        deps = {}

        def need(p, raw):
            if p is op:
                return
            if p.isdma or p.eng != eng or dma:
                deps[id(p)] = p
            elif raw and eng != "pe":
                deps[id(p)] = p

        if eng not in self.fenced:
            self.fenced.add(eng)
            for p in self.fence:
                if p.isdma or p.eng != eng:
                    deps[id(p)] = p
        for k in reads:
            t = self.track.get(k)
            if t is None:
                t = [None, {}]
                self.track[k] = t
            if t[0] is not None:
                need(t[0], True)
        for k in writes:
            t = self.track.get(k)
            if t is None:
                t = [None, {}]
                self.track[k] = t
            if t[0] is not None:
                need(t[0], False)
            for r in t[1].values():
                need(r, False)
        for k in reads:
            t = self.track[k]
            key = (eng, op.idx) if dma else eng
            t[1][key] = op
        for k in writes:
            t = self.track[k]
            t[0] = op
            t[1] = {}
        op.deps = list(deps.values())
        if dma:
            j = self.ndma[eng]
            self.ndma[eng] = j + 1
            r = j // NDMASEM
            ep = r // DMA_EPOCH
            op.sem = ("dma", eng, j % NDMASEM, ep)
            op.val = 16 * (r % DMA_EPOCH + 1)
            if j >= NDMASEM:
                rp = r - 1
                op.pre = (("dma", eng, j % NDMASEM, rp // DMA_EPOCH), 16 * (rp % DMA_EPOCH + 1))
        self.ops[eng].append(op)
        return op

    def end_phase(self, final=False):
        nc = self.nc
        if not hasattr(self, "upto"):
            self.upto = {e: 0 for e in ENGS}
            self.semcount = {e: 0 for e in ENGS}
            self.sems = {}
            self.waited = {e: {} for e in ENGS}
        self.barrier()
        for p in self.fence:
            if not p.isdma:
                p.sig = True
        for e in ENGS:
            for op in self.ops[e][self.upto[e]:]:
                for p in op.deps:
                    if not p.isdma and p.idx >= self.upto[p.eng]:
                        p.sig = True
        for e in ENGS:
            c = self.semcount[e]
            for op in self.ops[e][self.upto[e]:]:
                if (not op.isdma) and op.sig:
                    op.sem = ("c", e, c // SEM_EPOCH)
                    op.val = c % SEM_EPOCH + 1
                    c += 1
            self.semcount[e] = c

        def getsem(k):
            if k not in self.sems:
                self.sems[k] = self.semstack.enter_context(nc.semaphore("s%d" % len(self.sems)))
            return self.sems[k]

        with nc.Block() as block:
            def replay(e, eng):
                waited = self.waited[e]
                for op in self.ops[e][self.upto[e]:]:
                    ws = [(p.sem, p.val) for p in op.deps if p.sem is not None]
                    if op.pre is not None:
                        ws.append(op.pre)
                    for s_, v in ws:
                        if waited.get(s_, 0) < v:
                            eng.wait_ge(getsem(s_), v)
                            waited[s_] = v
                    ins = op.emit(eng)
                    if op.isdma:
                        ins.then_inc(getsem(op.sem), 16)
                    elif op.sig:
                        ins.then_inc(getsem(op.sem), 1)
                    op.emit = None
                if final and e == "sp":
                    for q in ENGS:
                        dm = [op for op in self.ops[q] if op.isdma][-NDMASEM:]
                        for op in dm:
                            if waited.get(op.sem, 0) < op.val:
                                eng.wait_ge(getsem(op.sem), op.val)
                                waited[op.sem] = op.val
                self.upto[e] = len(self.ops[e])

            @block.tensor
            def _(eng):
                replay("pe", eng)

            @block.scalar
            def _(eng):
                replay("act", eng)

            @block.vector
            def _(eng):
                replay("dve", eng)

            @block.gpsimd
            def _(eng):
                replay("pool", eng)

            @block.sync
            def _(eng):
                replay("sp", eng)


class B:
    def __init__(self, nc):
        self.nc = nc
        self.S = Sched(nc)
        self.rr = 0

    def mm(self, out, lhsT, rhs, start=True, stop=True, r=(), w=()):
        self.S.add("pe", lambda e: e.matmul(out, lhsT, rhs, start=start, stop=stop), r, w)

    def tr(self, out, in_, ident, r=(), w=()):
        self.S.add("pe", lambda e: e.transpose(out, in_, ident), r, w)

    def act(self, out, in_, func, bias=0.0, scale=1.0, r=(), w=(), accum_out=None):
        if accum_out is None:
            self.S.add("act", lambda e: e.activation(out, in_, func, bias=bias, scale=scale), r, w)
        else:
            self.S.add("act", lambda e: e.activation(out, in_, func, bias=bias, scale=scale,
                                                     accum_out=accum_out), r, w)

    def ts(self, eng, out, in0, s1, s2, op0, op1=None, r=(), w=(), accum_out=None):
        if op1 is None:
            self.S.add(eng, lambda e: e.tensor_scalar(out, in0, s1, None, op0), r, w)
        elif accum_out is None:
            self.S.add(eng, lambda e: e.tensor_scalar(out, in0, s1, s2, op0, op1), r, w)
        else:
            self.S.add(eng, lambda e: e.tensor_scalar(out, in0, s1, s2, op0, op1, accum_out), r, w)

    def tt(self, eng, out, in0, in1, op, r=(), w=()):
        self.S.add(eng, lambda e: e.tensor_tensor(out, in0, in1, op), r, w)

    def stt(self, out, in0, scalar, in1, op0, op1, r=(), w=(), accum_out=None):
        if accum_out is None:
            self.S.add("dve", lambda e: e.scalar_tensor_tensor(out, in0, scalar, in1, op0, op1), r, w)
        else:
            self.S.add("dve", lambda e: e.scalar_tensor_tensor(out, in0, scalar, in1, op0, op1,
                                                               accum_out), r, w)

    def cp(self, eng, out, in_, r=(), w=()):
        if eng == "act":
            self.S.add("act", lambda e: e.copy(out, in_), r, w)
        else:
            self.S.add(eng, lambda e: e.tensor_copy(out, in_), r, w)

    def evac(self, out, in_, r=(), w=()):
        self.rr ^= 1
        self.cp("act" if self.rr else "dve", out, in_, r, w)

    def memset(self, eng, ap, val, r=(), w=()):
        self.S.add(eng, lambda e: e.memset(ap, val), r, w)

    def dma(self, eng, out, in_, r=(), w=(), nc_ok=False):
        if nc_ok:
            self.S.add(eng, lambda e: e.dma_start(out=out, in_=in_, allow_slow_non_contiguous=True),
                       r, w, dma=True)
        else:
            self.S.add(eng, lambda e: e.dma_start(out=out, in_=in_), r, w, dma=True)

    def scatter(self, out_dram, idx, in_sb, r=(), w=()):
        self.S.add("pool", lambda e: e.indirect_dma_start(
            out=out_dram, out_offset=bass.IndirectOffsetOnAxis(ap=idx, axis=0),
            in_=in_sb, in_offset=None), r, w, dma=True)

    def gather(self, out_sb, in_dram, idx, r=(), w=()):
        self.S.add("pool", lambda e: e.indirect_dma_start(
            out=out_sb, out_offset=None, in_=in_dram,
            in_offset=bass.IndirectOffsetOnAxis(ap=idx, axis=0),
            ), r, w, dma=True)


def host_consts():
    c = {}
    c["ident_bf"] = np.eye(128, dtype=np.float32).astype(ml_dtypes.bfloat16)
    c["ident_f"] = np.eye(128, dtype=np.float32)
    tri = (np.arange(128)[:, None] <= np.arange(128)[None, :]).astype(np.float32)
    c["tri_bf"] = tri.astype(ml_dtypes.bfloat16)
    c["tri_f"] = tri
    c["tris_bf"] = (np.arange(128)[:, None] < np.arange(128)[None, :]).astype(np.float32).astype(ml_dtypes.bfloat16)
    c["ones_bf"] = np.ones((128, 128), dtype=ml_dtypes.bfloat16)
    c["ones_f"] = np.ones((128, 128), dtype=np.float32)
    c["ebase"] = np.broadcast_to((NROWS - np.arange(NE) * CAP).astype(np.float32)[None, :], (128, NE)).copy()
    return c


CONST_SPECS = [("ident_bf", BF16, [128, 128]), ("ident_f", F32, [128, 128]), ("tri_bf", BF16, [128, 128]),
               ("tri_f", F32, [128, 128]), ("tris_bf", BF16, [128, 128]), ("ones_bf", BF16, [128, 128]),
               ("ones_f", F32, [128, 128]), ("ebase", F32, [128, NE])]

IN_SPECS = [("x", [T, D]), ("w_in", [DEPTH, D, IN_W]), ("conv_w", [DEPTH, 4, 512]), ("conv_b", [DEPTH, 512]),
            ("gate_b", [DEPTH, 8]), ("lam_q1", [DEPTH, 64]), ("lam_k1", [DEPTH, 64]), ("lam_q2", [DEPTH, 64]),
            ("lam_k2", [DEPTH, 64]), ("da_norm_g", [DEPTH, 128]), ("w_out", [DEPTH, D, D]),
            ("ln1_g", [DEPTH, D]), ("ln1_b", [DEPTH, D]), ("w_router", [DEPTH, D, NE]), ("b_router", [DEPTH, NE]),
            ("w_gu", [DEPTH, NE, D, 2048]), ("b_gu", [DEPTH, NE, 2048]), ("w_down", [DEPTH, NE, D, D]),
            ("b_down", [DEPTH, NE, D]), ("ln2_g", [DEPTH, D]), ("ln2_b", [DEPTH, D])]


def build(nlayers=DEPTH, phases=("da", "ml", "op", "ex", "cb"), debug=()):
    nc = bass.Bass("TRN2", target_bir_lowering=False)
    I = {}
    for name, shp in IN_SPECS:
        I[name] = nc.dram_tensor(name, shp, F32, kind="ExternalInput").ap()
    for name, dt, shp in CONST_SPECS:
        I[name] = nc.dram_tensor(name, shp, dt, kind="ExternalInput").ap()
    y_out = nc.dram_tensor("y", [T, D], F32, kind="ExternalOutput").ap()

    def scratch(name, shp, dt):
        kind = "ExternalOutput" if name in debug else "Internal"
        return nc.dram_tensor(name, shp, dt, kind=kind).ap()

    mixT_d = scratch("mixT_d", [D, T], BF16)
    x1_d = scratch("x1_d", [T, D], F32)
    xa_d = scratch("xa_d", [T, D], F32)
    xb_d = scratch("xb_d", [T, D], F32)
    xs_d = scratch("xs_d", [NROWS + 1, D], BF16)
    ys_d = scratch("ys_d", [NROWS + 1, D], F32)

    b = B(nc)
    S = b.S
    _cnt = [0]

    def uniq(name):
        _cnt[0] += 1
        return "sb%d_%s" % (_cnt[0], name)
    st = contextlib.ExitStack()
    with st:
        S.semstack = st
        def sb(name, shp, dt):
            return st.enter_context(nc.sbuf_tensor(uniq(name), shp, dt))

        ident_bf = sb("ident_bf", [128, 128], BF16)
        ident_f = sb("ident_f", [128, 128], F32)
        tri_bf = sb("tri_bf", [128, 128], BF16)
        tri_f = sb("tri_f", [128, 128], F32)
        tris_bf = sb("tris_bf", [128, 128], BF16)
        ones_bf = sb("ones_bf", [128, 128], BF16)
        ones_f = sb("ones_f", [128, 128], F32)
        ebase = sb("ebase", [128, NE], F32)
        for name, t_ in (("ident_bf", ident_bf), ("ident_f", ident_f), ("tri_bf", tri_bf), ("tri_f", tri_f),
                         ("tris_bf", tris_bf), ("ones_bf", ones_bf), ("ones_f", ones_f), ("ebase", ebase)):
            b.dma("sp", t_[:], I[name], w=["const"])
        idx_all = sb("idx_all", [128, NT, 4], I32)
        gate_all = sb("gate_all", [128, NT, 4], F32)
        with nc.sbuf_tensor("sb_zrow", [1, D], F32) as zrow:
            b.memset("dve", zrow[:], 0.0, w=["zrow"])
            b.dma("sp", ys_d[NROWS:NROWS + 1, :], zrow[:], r=["zrow"])

        ps = [st.enter_context(nc.psum_tensor("ps%d" % i, [128, 512], F32)) for i in range(8)]

        def PS(i):
            return ("ps", i)

        def stream_xT(xsrc, blk, xstage, xT_blk, tag, nbuf=2):
            buf = blk % nbuf
            for j in range(4):
                tt = blk * 4 + j
                sbuf_i = tt % 2
                b.dma("pool", xstage[:, sbuf_i, :], xsrc[tt * 128:(tt + 1) * 128, :],
                      w=[("xstage", sbuf_i)])
                pst = ps[7][:].bitcast(BF16)
                for kc in range(KC):
                    b.tr(pst[:, kc * 128:(kc + 1) * 128], xstage[:, sbuf_i, kc * 128:(kc + 1) * 128], ident_bf[:],
                         r=[("xstage", sbuf_i), "const"], w=[PS(7)])
                b.evac(xT_blk[:, buf, :, j * 128:(j + 1) * 128],
                       pst.rearrange("p (k t) -> p k t", k=KC), r=[PS(7)], w=[(tag, buf)])

        def layer_norm(xt, g_bc, b_bc, out, keyin, keyout, tmpstat, tmpmv, tmpr):
            b.S.add("dve", lambda e: e.bn_stats(tmpstat[:, 0, :], xt[:, 0:512]), [keyin], ["lnstat"])
            b.S.add("dve", lambda e: e.bn_stats(tmpstat[:, 1, :], xt[:, 512:1024]), [keyin], ["lnstat"])
            b.S.add("dve", lambda e: e.bn_aggr(tmpmv[:], tmpstat[:].rearrange("p a b -> p (a b)")), ["lnstat"], ["lnmv"])
            b.act(tmpr[:, 0:1], tmpmv[:, 1:2], AF.Sqrt, bias=epsc[:, 0:1], scale=1.0, r=["lnmv", "epsc"], w=["lnr0"])
            b.S.add("dve", lambda e: e.reciprocal(tmpr[:, 1:2], tmpr[:, 0:1]), ["lnr0"], ["lnr1"])
            b.ts("dve", xt, xt, tmpmv[:, 0:1], tmpr[:, 1:2], ALU.subtract, ALU.mult, r=[keyin, "lnmv", "lnr1"], w=[keyin])
            b.tt("pool", xt, xt, g_bc, ALU.mult, r=[keyin, "lnp"], w=[keyin])
            b.tt("pool", out, xt, b_bc, ALU.add, r=[keyin, "lnp"], w=[keyout])

        epsc = sb("epsc", [128, 2], F32)
        b.memset("dve", epsc[:, 0:1], LN_EPS, w=["epsc"])
        b.memset("dve", epsc[:, 1:2], 128.0 * RMS_EPS, w=["epsc"])

        xcur = I["x"]
        for l in range(nlayers):
            lam_init = 0.8 - 0.6 * math.exp(-0.3 * l)
            xnext = y_out if l == nlayers - 1 else (xa_d if l % 2 == 0 else xb_d)
            if "da" in phases:
                with contextlib.ExitStack() as ph:
                    def pb(name, shp, dt):
                        return ph.enter_context(nc.sbuf_tensor(uniq(name), shp, dt))
                    w_da = pb("w_da", [128, KC, 1536], BF16)
                    for kc in range(KC):
                        b.dma("pool", w_da[:, kc, :], I["w_in"][l, kc * 128:(kc + 1) * 128, 0:1536], w=["w_da"])
                    xstage = pb("xstage", [128, 2, D], BF16)
                    xT_blk = pb("xT_blk", [128, 2, KC, 512], BF16)
                    qT = pb("qT", [128, 4, T], BF16)
                    kT = pb("kT", [128, 4, T], BF16)
                    Vd = pb("Vd", [128, NT, 512], BF16)
                    lamt = pb("lamt", [128, 4, 64], F32)
                    lamv = pb("lamv", [128, 8], F32)
                    gcol = pb("gcol", [128, 2], F32)
                    for i, nm in enumerate(("lam_q1", "lam_k1", "lam_q2", "lam_k2")):
                        b.dma("sp", lamt[:, i, :], I[nm][l:l + 1, :].partition_broadcast(128), w=["lamt"])
                    b.dma("sp", gcol[:, 0:1], I["da_norm_g"][l:l + 1, :].rearrange("o p -> p o"), w=["gcol"], nc_ok=True)
                    b.tt("dve", lamt[:, 0, :], lamt[:, 0, :], lamt[:, 1, :], ALU.mult, r=["lamt"], w=["lamt"])
                    b.tt("dve", lamt[:, 2, :], lamt[:, 2, :], lamt[:, 3, :], ALU.mult, r=["lamt"], w=["lamt"])
                    b.S.add("dve", lambda e: e.reduce_sum(lamv[:, 0:1], lamt[:, 0, :], AX.X), ["lamt"], ["lamv"])
                    b.S.add("dve", lambda e: e.reduce_sum(lamv[:, 1:2], lamt[:, 2, :], AX.X), ["lamt"], ["lamv"])
                    b.act(lamv[:, 2:4], lamv[:, 0:2], AF.Exp, r=["lamv"], w=["lamv2"])
                    b.tt("dve", lamv[:, 4:5], lamv[:, 3:4], lamv[:, 2:3], ALU.subtract, r=["lamv2"], w=["lamv3"])
                    b.ts("dve", lamv[:, 5:6], lamv[:, 4:5], -lam_init, None, ALU.add, r=["lamv3"], w=["neglam"])
                    neglam = lamv[:, 5:6]
                    b.ts("dve", gcol[:, 1:2], gcol[:, 0:1], (1.0 - lam_init) * math.sqrt(128.0), None, ALU.mult,
                         r=["gcol"], w=["gcol2"])
                    for blk in range(8):
                        stream_xT(xcur, blk, xstage, xT_blk, "xT")
                        buf = blk % 2
                        for grp in range(8):
                            pa = ps[grp % 2]
                            for kc in range(KC):
                                b.mm(pa[:], w_da[:, kc, grp * 128:(grp + 1) * 128], xT_blk[:, buf, kc, :],
                                     start=(kc == 0), stop=(kc == KC - 1), r=["w_da", ("xT", buf)], w=[PS(grp % 2)])
                            dst = qT if grp < 4 else kT
                            b.evac(dst[:, grp % 4, blk * 512:(blk + 1) * 512], pa[:], r=[PS(grp % 2)],
                                   w=[("q" if grp < 4 else "k", blk)])
                        for j in range(4):
                            tt = blk * 4 + j
                            pa = ps[j % 2]
                            for kc in range(KC):
                                b.mm(pa[:], xT_blk[:, buf, kc, j * 128:(j + 1) * 128], w_da[:, kc, 1024:1536],
                                     start=(kc == 0), stop=(kc == KC - 1), r=["w_da", ("xT", buf)], w=[PS(j % 2)])
                            b.evac(Vd[:, tt, :], pa[:], r=[PS(j % 2)], w=[("v", tt)])
                    Pt = pb("Pt", [128, 3, 512], BF16)
                    Rc = pb("Rc", [128, 2, 512], F32)
                    rec = pb("rec", [128, 512], F32)
                    oc = pb("oc", [128, 512], F32)
                    sq = pb("sq", [128, 512], F32)
                    rs = pb("rs", [128, 512], F32)
                    ob = pb("ob", [128, 2, 512], BF16)
                    items = []
                    for Qb in range(8):
                        for h in range(4):
                            for c in range(2):
                                nk = (Qb + 1) * 4
                                for kt in range(nk):
                                    items.append((Qb, h, c, kt, nk))

                    def emit_S(i):
                        Qb, h, c, kt, nk = items[i]
                        diag = kt >= Qb * 4
                        q0 = (kt - Qb * 4) * 128 if diag else 0
                        n = 512 - q0
                        lo = c * 64
                        sbank = i % 2
                        b.mm(ps[sbank][:, 0:n], kT[lo:lo + 64, h, kt * 128:(kt + 1) * 128],
                             qT[lo:lo + 64, h, Qb * 512 + q0:(Qb + 1) * 512],
                             r=[("k", kt // 4), ("q", Qb)], w=[PS(sbank)])

                    emit_S(0)
                    for i, (Qb, h, c, kt, nk) in enumerate(items):
                        if i + 1 < len(items):
                            emit_S(i + 1)
                        it = (Qb * 4 + h) * 2 + c
                        Ob = 2 + 2 * (it % 2)
                        Lb = Ob + 1
                        diag = kt >= Qb * 4
                        q0 = (kt - Qb * 4) * 128 if diag else 0
                        n = 512 - q0
                        sbank = i % 2
                        pbuf = i % 3
                        b.act(Pt[:, pbuf, 0:n], ps[sbank][:, 0:n], AF.Exp, scale=0.125,
                              r=[PS(sbank)], w=[("Pt", pbuf)])
                        if diag:
                            b.tt("pool", Pt[:, pbuf, 0:128], Pt[:, pbuf, 0:128], tri_bf[:], ALU.mult,
                                 r=[("Pt", pbuf), "const"], w=[("Pt", pbuf)])
                        b.mm(ps[Ob][:, q0:512], Vd[:, kt, h * 128:(h + 1) * 128], Pt[:, pbuf, 0:n],
                             start=(kt == 0), stop=(kt == nk - 1), r=[("v", kt), ("Pt", pbuf)], w=[PS(Ob)])
                        b.mm(ps[Lb][:, q0:512], ones_bf[:], Pt[:, pbuf, 0:n],
                             start=(kt == 0), stop=(kt == nk - 1), r=["const", ("Pt", pbuf)], w=[PS(Lb)])
                        if kt != nk - 1:
                            continue
                        b.S.add("dve", lambda e, Lb=Lb: e.reciprocal(rec[:], ps[Lb][:]), [PS(Lb)], ["rec"])
                        b.tt("dve", Rc[:, c, :], ps[Ob][:], rec[:], ALU.mult, r=[PS(Ob), "rec"], w=[("Rc", c)])
                        if c != 1:
                            continue
                        b.stt(oc[:], Rc[:, 1, :], neglam, Rc[:, 0, :], ALU.mult, ALU.add,
                              r=[("Rc", 0), ("Rc", 1), "neglam"], w=["oc"])
                        b.act(sq[:], oc[:], AF.Square, r=["oc"], w=["sq"])
                        b.mm(ps[6][:], ones_f[:], sq[:], r=["sq", "const"], w=[PS(6)])
                        b.act(rs[:], ps[6][:], AF.Sqrt, bias=epsc[:, 1:2], r=[PS(6), "epsc"], w=["rs"])
                        b.S.add("dve", lambda e: e.reciprocal(rs[:], rs[:]), ["rs"], ["rs"])
                        ob_i = (Qb * 4 + h) % 2
                        b.stt(ob[:, ob_i, :], oc[:], gcol[:, 1:2], rs[:], ALU.mult, ALU.mult,
                              r=["oc", "rs", "gcol2"], w=[("ob", ob_i)])
                        b.dma("sp", mixT_d[h * 128:(h + 1) * 128, Qb * 512:(Qb + 1) * 512], ob[:, ob_i, :],
                              r=[("ob", ob_i)])
                    S.end_phase()

            if "ml" in phases:
                with contextlib.ExitStack() as ph:
                    def pb(name, shp, dt):
                        return ph.enter_context(nc.sbuf_tensor(uniq(name), shp, dt))
                    mq = pb("mq", [64, 4, T], BF16)
                    mk = pb("mk", [64, 4, T], BF16)
                    mv = pb("mv", [128, NT, 512], BF16)
                    mo = pb("mo", [128, 4, T], BF16)
                    gtm = pb("gtm", [128, NT, 8], F32)
                    lf = pb("lf", [128, NT, 4], F32)
                    btm = pb("btm", [128, NT, 4], F32)
                    gbc = pb("gbc", [128, NT, 4], F32)
                    ew = pb("ew", [128, NT, 4], F32)
                    ebias = pb("ebias", [128, NT, 4], F32)
                    eg = pb("eg", [128, NT, 4], F32)
                    nl8 = pb("nl8", [128, 2], F32)
                    b.memset("dve", nl8[:, 0:1], -math.log(8.0), w=["nl8"])
                    b.memset("dve", nl8[:, 1:2], 1.0, w=["nl8"])
                    with contextlib.ExitStack() as ph2:
                        def pb2(name, shp, dt):
                            return ph2.enter_context(nc.sbuf_tensor(uniq(name), shp, dt))
                        w_ml = pb2("w_ml", [128, KC, 1544], BF16)
                        for kc in range(KC):
                            b.dma("pool", w_ml[:, kc, :], I["w_in"][l, kc * 128:(kc + 1) * 128, 1536:3080], w=["w_ml"])
                        xstage = pb2("xstage", [128, 2, D], BF16)
                        xT_blk = pb2("xT_blk", [128, 1, KC, 512], BF16)
                        gb_bc = pb2("gb_bc", [128, 8], F32)
                        b.dma("sp", gb_bc[:], I["gate_b"][l:l + 1, :].partition_broadcast(128), w=["gb_bc"])
                        cw = pb2("cw", [64, 4, 8], F32)
                        cb = pb2("cb", [64, 8], F32)
                        for j in range(4):
                            b.dma("sp", cw[:, j, :], I["conv_w"][l, j:j + 1, :].rearrange("o (g p) -> p (o g)", p=64), w=["cw"], nc_ok=True)
                        b.dma("sp", cb[:], I["conv_b"][l:l + 1, :].rearrange("o (g p) -> p (o g)", p=64), w=["cw"], nc_ok=True)
                        cin = pb2("cin", [64, 8, 515], F32)
                        ctmp = pb2("ctmp", [64, 2, 512], F32)
                        b.memset("dve", cin[:, :, 0:3], 0.0, w=[("cin", g) for g in range(8)])
                        for blk in range(8):
                            stream_xT(xcur, blk, xstage, xT_blk, "xT", nbuf=1)
                            buf = 0
                            for grp in range(8):
                                pa = ps[grp % 2]
                                for kc in range(KC):
                                    b.mm(pa[0:64, :], w_ml[:, kc, grp * 64:(grp + 1) * 64], xT_blk[:, buf, kc, :],
                                         start=(kc == 0), stop=(kc == KC - 1), r=["w_ml", ("xT", buf)], w=[PS(grp % 2)])
                                b.cp("act", cin[:, grp, 3:515], pa[0:64, :], r=[PS(grp % 2)], w=[("cin", grp)])
                                ct = ctmp[:, grp % 2, :]
                                b.ts("dve", ct, cin[:, grp, 0:512], cw[:, 0, grp:grp + 1], cb[:, grp:grp + 1], ALU.mult, ALU.add,
                                     r=[("cin", grp), "cw"], w=[("ctmp", grp % 2)])
                                for j in range(1, 4):
                                    b.stt(ct, cin[:, grp, j:j + 512], cw[:, j, grp:grp + 1], ct, ALU.mult, ALU.add,
                                          r=[("cin", grp), "cw", ("ctmp", grp % 2)], w=[("ctmp", grp % 2)])
                                dst = mq if grp < 4 else mk
                                b.act(dst[:, grp % 4, blk * 512:(blk + 1) * 512], ct, AF.Silu,
                                      r=[("ctmp", grp % 2)], w=[("mqk", blk)])
                                b.cp("pool", cin[:, grp, 0:3], cin[:, grp, 512:515], r=[("cin", grp)], w=[("cin", grp)])
                            for h in range(4):
                                pa = ps[2 + h % 2]
                                for kc in range(KC):
                                    b.mm(pa[:], w_ml[:, kc, 1024 + h * 128:1024 + (h + 1) * 128], xT_blk[:, buf, kc, :],
                                         start=(kc == 0), stop=(kc == KC - 1), r=["w_ml", ("xT", buf)], w=[PS(2 + h % 2)])
                                b.act(mo[:, h, blk * 512:(blk + 1) * 512], pa[:], AF.Sigmoid, r=[PS(2 + h % 2)], w=[("mo", blk)])
                            for j in range(4):
                                tt = blk * 4 + j
                                pa = ps[4 + j % 2]
                                for kc in range(KC):
                                    b.mm(pa[:], xT_blk[:, buf, kc, j * 128:(j + 1) * 128], w_ml[:, kc, 512:1024],
                                         start=(kc == 0), stop=(kc == KC - 1), r=["w_ml", ("xT", buf)], w=[PS(4 + j % 2)])
                                b.evac(mv[:, tt, :], pa[:], r=[PS(4 + j % 2)], w=[("mv", tt)])
                                for kc in range(KC):
                                    b.mm(ps[6][:, 0:8], xT_blk[:, buf, kc, j * 128:(j + 1) * 128], w_ml[:, kc, 1536:1544],
                                         start=(kc == 0), stop=(kc == KC - 1), r=["w_ml", ("xT", buf)], w=[PS(6)])
                                b.tt("dve", gtm[:, tt, :], ps[6][:, 0:8], gb_bc[:], ALU.add, r=[PS(6), "gb_bc"], w=["gtm"])
                    S.barrier()
                    b.act(lf[:], gtm[:, :, 4:8], AF.Exp, scale=-1.0, r=["gtm"], w=["lf"])
                    b.act(lf[:], lf[:], AF.Ln, bias=nl8[:, 1:2], r=["lf", "nl8"], w=["lf"])
                    b.ts("dve", lf[:], lf[:], -1.0, None, ALU.mult, r=["lf"], w=["lf"])
                    lf2 = lf[:].rearrange("p a b -> p (a b)")
                    b.mm(ps[6][:, 0:128], tri_f[:], lf2, r=["lf", "const"], w=[PS(6)])
                    b.cp("dve", btm[:].rearrange("p a b -> p (a b)"), ps[6][:, 0:128], r=[PS(6)], w=["btm"])
                    b.mm(ps[6][:, 128:256], ones_f[:], lf2, r=["lf", "const"], w=[PS(6)])
                    b.cp("dve", gbc[:].rearrange("p a b -> p (a b)"), ps[6][:, 128:256], r=[PS(6)], w=["gbc"])
                    b.tt("dve", ebias[:], gtm[:, :, 0:4], btm[:], ALU.subtract, r=["gtm", "btm"], w=["ebias"])
                    b.tt("dve", ew[:], ebias[:], gbc[:], ALU.add, r=["ebias", "gbc"], w=["ew"])
                    b.act(ew[:], ew[:], AF.Exp, r=["ew"], w=["ew"])
                    b.ts("dve", ebias[:], ebias[:], -math.log(8.0), None, ALU.add, r=["ebias", "ew"], w=["ebias2"])
                    b.act(eg[:], gbc[:], AF.Exp, r=["gbc"], w=["eg"])
                    Cst = pb("Cst", [64, 4, 256], F32)
                    Cbf = pb("Cbf", [64, 4, 256], BF16)
                    b.memset("dve", Cst[:], 0.0, w=[("C", h) for h in range(4)])
                    b.memset("pool", Cbf[:], 0.0, w=[("Cbf", h) for h in range(4)])
                    lfd = pb("lfd", [128, 2, 4, 128], F32)
                    Et = pb("Et", [128, 2, 4, 128], F32)
                    Eq = pb("Eq", [64, 2, 4, 128], F32)
                    qs = pb("qs", [64, 2, 4, 128], BF16)
                    STt = pb("STt", [128, 2, 4, 128], BF16)
                    kh = pb("kh", [128, 2, 4, 64], BF16)
                    dm = pb("dm", [128, 2, 4, 128], F32)
                    hT = pb("hT", [128, 2, 4, 128], F32)
                    hst = pb("hst", [128, 2, 4, 512], BF16)
                    H4 = range(4)

                    def front(tt):
                        c0 = tt * 128
                        u = tt % 2
                        blk = tt // 4
                        sb_ = 1 if u == 0 else 7
                        for h in H4:
                            b.ts("pool", lfd[:, u, h, :], ones_f[:], lf[:, tt, h:h + 1], None, ALU.mult,
                                 r=["lf", "const"], w=[("lfd", u, h)])
                        for h in H4:
                            b.mm(ps[0][:, h * 128:(h + 1) * 128], lfd[:, u, h, :], tri_f[:], r=[("lfd", u, h), "const"], w=[PS(0)])
                        for h in H4:
                            b.mm(ps[sb_][:, h * 128:(h + 1) * 128], mk[:, h, c0:c0 + 128], mq[:, h, c0:c0 + 128],
                                 r=[("mqk", blk)], w=[PS(sb_)])
                        pkt = ps[4][:].bitcast(BF16)
                        for h in H4:
                            b.tr(pkt[:, h * 64:(h + 1) * 64], mk[:, h, c0:c0 + 128], ident_bf[0:64, 0:64],
                                 r=[("mqk", blk), "const"], w=[PS(4)])
                        for h in H4:
                            b.act(Et[:, u, h, :], ps[0][:, h * 128:(h + 1) * 128], AF.Exp, bias=ebias[:, tt, h:h + 1],
                                  r=[PS(0), "ebias2"], w=[("Et", u, h)])
                        b.act(Eq[:, u, :, :], ps[0][0:64, :].rearrange("p (h t) -> p h t", h=4), AF.Exp, bias=nl8[0:64, 0:1],
                              r=[PS(0), "nl8"], w=[("Eq", u)])
                        b.tt("dve", qs[:, u, :, :], mq[:, :, c0:c0 + 128], Eq[:, u, :, :], ALU.mult,
                             r=[("mqk", blk), ("Eq", u)], w=[("qs", u)])
                        for h in H4:
                            b.tt("pool", Et[:, u, h, :], Et[:, u, h, :], tri_f[:], ALU.mult, r=[("Et", u, h), "const"], w=[("Et", u, h)])
                        for h in H4:
                            b.ts("dve", kh[:, u, h, :], pkt[:, h * 64:(h + 1) * 64], ew[:, tt, h:h + 1], None, ALU.mult,
                                 r=[PS(4), "ew"], w=[("kh", u, h)])

                    def back(tt):
                        c0 = tt * 128
                        u = tt % 2
                        blk = tt // 4
                        sb_ = 1 if u == 0 else 7
                        b.tt("dve", STt[:, u, :, :], ps[sb_][:].rearrange("p (h t) -> p h t", h=4), Et[:, u, :, :], ALU.mult,
                             r=[PS(sb_)] + [("Et", u, h) for h in H4], w=[("ST", u)])
                        for h in H4:
                            b.mm(ps[2][:, h * 128:(h + 1) * 128], mv[:, tt, h * 128:(h + 1) * 128], STt[:, u, h, :],
                                 start=True, stop=False, r=[("mv", tt), ("ST", u)], w=[PS(2)])
                            b.mm(ps[2][:, h * 128:(h + 1) * 128], Cbf[:, h, 0:128], qs[:, u, h, :],
                                 start=False, stop=True, r=[("Cbf", h), ("qs", u)], w=[PS(2)])
                        for h in H4:
                            b.mm(ps[3][:, h * 128:(h + 1) * 128], ones_bf[:], STt[:, u, h, :],
                                 start=True, stop=False, r=["const", ("ST", u)], w=[PS(3)])
                            b.mm(ps[3][:, h * 128:(h + 1) * 128], Cbf[:, h, 128:256], qs[:, u, h, :],
                                 start=False, stop=True, r=[("Cbf", h), ("qs", u)], w=[PS(3)])
                        for h in H4:
                            pu = ps[5 + h // 2]
                            o0 = (h % 2) * 256
                            b.mm(pu[0:64, o0:o0 + 128], kh[:, u, h, :], mv[:, tt, h * 128:(h + 1) * 128],
                                 r=[("kh", u, h), ("mv", tt)], w=[PS(5 + h // 2)])
                            b.mm(pu[0:64, o0 + 128:o0 + 256], kh[:, u, h, :], ones_bf[:], r=[("kh", u, h), "const"],
                                 w=[PS(5 + h // 2)])
                        b.act(dm[:, u, :, :], ps[3][:].rearrange("p (h t) -> p h t", h=4), AF.Abs,
                              r=[PS(3)], w=[("dm", u)])
                        b.ts("dve", dm[:, u, :, :], dm[:, u, :, :], 1.0, None, ALU.max, r=[("dm", u)], w=[("dm", u)])
                        b.S.add("dve", lambda e, u=u: e.reciprocal(dm[:, u, :, :], dm[:, u, :, :]), [("dm", u)], [("dm", u)])
                        b.tt("dve", hT[:, u, :, :], ps[2][:].rearrange("p (h t) -> p h t", h=4), dm[:, u, :, :], ALU.mult,
                             r=[PS(2), ("dm", u)], w=[("hT", u)])
                        hp = blk % 2
                        b.tt("pool", hst[:, hp, :, (tt % 4) * 128:(tt % 4 + 1) * 128], hT[:, u, :, :],
                             mo[:, :, c0:c0 + 128], ALU.mult, r=[("hT", u), ("mo", blk)], w=[("hst", hp)])
                        if tt % 4 == 3:
                            for h in H4:
                                b.dma("sp", mixT_d[512 + h * 128:512 + (h + 1) * 128, blk * 512:(blk + 1) * 512],
                                      hst[:, hp, h, :], r=[("hst", hp)])
                        for h in H4:
                            pu = ps[5 + h // 2]
                            o0 = (h % 2) * 256
                            b.stt(Cst[:, h, :], Cst[:, h, :], eg[0:64, tt, h:h + 1], pu[0:64, o0:o0 + 256], ALU.mult, ALU.add,
                                  r=[("C", h), "eg", PS(5 + h // 2)], w=[("C", h)])
                        b.cp("act", Cbf[:], Cst[:], r=[("C", h) for h in H4], w=[("Cbf", h) for h in H4])

                    front(0)
                    for tt in range(NT):
                        if tt + 1 < NT:
                            front(tt + 1)
                        back(tt)
                    S.end_phase()

            if "op" in phases:
                with contextlib.ExitStack() as ph:
                    def pb(name, shp, dt):
                        return ph.enter_context(nc.sbuf_tensor(uniq(name), shp, dt))
                    w_o = pb("w_o", [128, KC, D], BF16)
                    for kc in range(KC):
                        b.dma("pool", w_o[:, kc, :], I["w_out"][l, kc * 128:(kc + 1) * 128, :], w=["w_o"])
                    w_r = pb("w_r", [128, KC, NE], F32)
                    b.dma("sp", w_r[:], I["w_router"][l].rearrange("(k p) e -> p k e", p=128), w=["w_r"])
                    br_bc = pb("br_bc", [128, NE], F32)
                    b.dma("sp", br_bc[:], I["b_router"][l:l + 1, :].partition_broadcast(128), w=["w_r"])
                    g_bc = pb("g_bc", [128, D], F32)
                    b_bc = pb("b_bc", [128, D], F32)
                    b.dma("sp", g_bc[:], I["ln1_g"][l:l + 1, :].partition_broadcast(128), w=["lnp"])
                    b.dma("sp", b_bc[:], I["ln1_b"][l:l + 1, :].partition_broadcast(128), w=["lnp"])
                    mixb = pb("mixb", [128, 2, KC, 512], BF16)
                    xt = pb("xt", [128, 2, D], F32)
                    x1t = pb("x1t", [128, 2, D], F32)
                    x1b = pb("x1b", [128, 2, D], BF16)
                    x1T = pb("x1T", [128, KC, 128], F32)
                    tstat = pb("tstat", [128, 2, 6], F32)
                    tmv = pb("tmv", [128, 2], F32)
                    tr_ = pb("tr_", [128, 2], F32)
                    lg = pb("lg", [128, NE], F32)
                    m8 = pb("m8", [128, 8], F32)
                    msk = pb("msk", [128, NE], F32)
                    mskb = pb("mskb", [128, NE], BF16)
                    ex = pb("ex", [128, NE], F32)
                    G = pb("G", [128, NE], F32)
                    gs = pb("gs", [128, 4], F32)
                    cnt = pb("cnt", [128, NE], F32)
                    pos = pb("pos", [128, NE], F32)
                    okm = pb("okm", [128, NE], F32)
                    val = pb("val", [128, NE], F32)
                    v8 = pb("v8", [128, 8], F32)
                    idf = pb("idf", [128, 4], F32)
                    junk = pb("junk", [128, NE], F32)
                    b.memset("dve", cnt[:], 0.0, w=["cnt"])
                    for blk in range(8):
                        mb = blk % 2
                        b.dma("sp", mixb[:, mb, :, :], mixT_d[:, blk * 512:(blk + 1) * 512].rearrange("(k p) t -> p k t", p=128),
                              w=[("mixb", mb)])
                        for j in range(4):
                            tt = blk * 4 + j
                            u = tt % 2
                            b.dma("sp", xt[:, u, :], xcur[tt * 128:(tt + 1) * 128, :], w=[("xt", u)])
                            for half in range(2):
                                pa = ps[half]
                                for kc in range(KC):
                                    b.mm(pa[:], mixb[:, mb, kc, j * 128:(j + 1) * 128], w_o[:, kc, half * 512:(half + 1) * 512],
                                         start=(kc == 0), stop=(kc == KC - 1), r=[("mixb", mb), "w_o"], w=[PS(half)])
                                b.stt(xt[:, u, half * 512:(half + 1) * 512], xt[:, u, half * 512:(half + 1) * 512], DN_ALPHA,
                                      pa[:], ALU.mult, ALU.add, r=[("xt", u), PS(half)], w=[("xt", u)])
                            layer_norm(xt[:, u, :], g_bc[:], b_bc[:], x1t[:, u, :], ("xt", u), ("x1t", u), tstat, tmv, tr_)
                            b.dma("sp", x1_d[tt * 128:(tt + 1) * 128, :], x1t[:, u, :], r=[("x1t", u)])
                            b.cp("act", x1b[:, u, :], x1t[:, u, :], r=[("x1t", u)], w=[("x1b", u)])
                            for kc in range(KC):
                                b.tr(ps[2 + kc // 4][:, (kc % 4) * 128:(kc % 4 + 1) * 128], x1t[:, u, kc * 128:(kc + 1) * 128],
                                     ident_f[:], r=[("x1t", u), "const"], w=[PS(2 + kc // 4)])
                            b.cp("act", x1T[:, 0:4, :], ps[2][:].rearrange("p (k t) -> p k t", k=4), r=[PS(2)], w=["x1T"])
                            b.cp("dve", x1T[:, 4:8, :], ps[3][:].rearrange("p (k t) -> p k t", k=4), r=[PS(3)], w=["x1T"])
                            for kc in range(KC):
                                b.mm(ps[4][:, 0:NE], x1T[:, kc, :], w_r[:, kc, :], start=(kc == 0), stop=(kc == KC - 1),
                                     r=["x1T", "w_r"], w=[PS(4)])
                            b.tt("dve", lg[:], ps[4][:, 0:NE], br_bc[:], ALU.add, r=[PS(4), "w_r"], w=["lg"])
                            b.S.add("dve", lambda e: e.max(m8[:], lg[:]), ["lg"], ["m8"])
                            b.ts("dve", msk[:], lg[:], m8[:, 3:4], None, ALU.is_ge, r=["lg", "m8"], w=["msk"])
                            b.cp("pool", mskb[:], msk[:], r=["msk"], w=["mskb"])
                            b.ts("dve", ex[:], lg[:], m8[:, 0:1], None, ALU.subtract, r=["lg", "m8"], w=["ex"])
                            b.act(ex[:], ex[:], AF.Exp, r=["ex"], w=["ex"])
                            b.stt(G[:], ex[:], 1.0, msk[:], ALU.mult, ALU.mult, r=["ex", "msk"], w=["G"], accum_out=gs[:, 0:1])
                            b.S.add("dve", lambda e: e.reciprocal(gs[:, 1:2], gs[:, 0:1]), ["G"], ["gs"])
                            b.ts("dve", G[:], G[:], gs[:, 1:2], None, ALU.mult, r=["G", "gs"], w=["G"])
                            b.mm(ps[5][:, 0:NE], tris_bf[:], mskb[:], r=["mskb", "const"], w=[PS(5)])
                            b.mm(ps[5][:, NE:2 * NE], ones_bf[:], mskb[:], r=["mskb", "const"], w=[PS(5)])
                            b.tt("dve", pos[:], ps[5][:, 0:NE], cnt[:], ALU.add, r=[PS(5), "cnt"], w=["pos"])
                            b.tt("dve", cnt[:], ps[5][:, NE:2 * NE], cnt[:], ALU.add, r=[PS(5), "cnt", "pos"], w=["cnt"])
                            b.ts("dve", okm[:], pos[:], float(CAP), None, ALU.is_lt, r=["pos"], w=["okm"])
                            b.tt("dve", okm[:], okm[:], msk[:], ALU.mult, r=["okm", "msk"], w=["okm"])
                            b.tt("dve", val[:], ebase[:], pos[:], ALU.subtract, r=["pos", "const"], w=["val"])
                            b.tt("dve", val[:], val[:], okm[:], ALU.mult, r=["val", "okm"], w=["val"])
                            b.S.add("dve", lambda e: e.max(v8[:], val[:]), ["val"], ["v8"])
                            b.ts("dve", idf[:], v8[:, 0:4], -1.0, float(NROWS), ALU.mult, ALU.add, r=["v8"], w=["idf"])
                            b.cp("dve", idx_all[:, tt, :], idf[:], r=["idf"], w=[("idx", tt)])
                            for k in range(4):
                                b.stt(junk[:], val[:], v8[:, k:k + 1], G[:], ALU.is_equal, ALU.mult,
                                      r=["val", "v8", "G"], w=["junk", ("gate", tt)], accum_out=gate_all[:, tt, k:k + 1])
                                b.scatter(xs_d[:, :], idx_all[:, tt, k:k + 1], x1b[:, u, :],
                                          r=[("idx", tt), ("x1b", u)])
                    S.end_phase()

            if "ex" in phases:
                with contextlib.ExitStack() as ph:
                    def pb(name, shp, dt):
                        return ph.enter_context(nc.sbuf_tensor(uniq(name), shp, dt))
                    NJ = CAP // 128
                    NCH = 12
                    wgu = pb("wgu", [128, 2, KC, 2048], BF16)
                    wdn = pb("wdn", [128, 2, KC, D], BF16)
                    wst = pb("wst", [128, 3, 2048], F32)
                    bgu = pb("bgu", [128, 2, 16], F32)
                    bdn = pb("bdn", [128, 2, D], F32)
                    xst = pb("xst", [128, 2, D], BF16)
                    xsT = pb("xsT", [128, 2, KC, CAP], BF16)
                    gsb = pb("gsb", [128, 2, CAP], F32)
                    sg = pb("sg", [128, 2, CAP], F32)
                    glu = pb("glu", [128, 8, CAP], BF16)
                    ub = pb("ub", [128, 2, CAP], F32)
                    aT = pb("aT", [128, 8, CAP], BF16)
                    yt = pb("yt", [128, 2, D], F32)
                    NG = NE * NCH

                    def dma_chunk(g):
                        if g >= NG:
                            return
                        e, i = divmod(g, NCH)
                        slot = g % 3
                        if i < 8:
                            b.dma("sp", wst[:, slot, :], I["w_gu"][l, e, i * 128:(i + 1) * 128, :], w=[("wst", slot)])
                        else:
                            kp = i - 8
                            b.dma("sp", wst[:, slot, :].rearrange("p (k d) -> p k d", k=2),
                                  I["w_down"][l, e, kp * 256:(kp + 1) * 256, :].rearrange("(k p) d -> p k d", p=128),
                                  w=[("wst", slot)])

                    def cast_chunk(g):
                        if g >= NG:
                            return
                        e, i = divmod(g, NCH)
                        slot = g % 3
                        wb = e % 2
                        eng = "act" if g % 2 == 0 else "dve"
                        if i < 8:
                            b.cp(eng, wgu[:, wb, i, :], wst[:, slot, :], r=[("wst", slot)], w=[("wgu", wb, i)])
                        else:
                            kp = i - 8
                            b.cp(eng, wdn[:, wb, 2 * kp:2 * kp + 2, :], wst[:, slot, :].rearrange("p (k d) -> p k d", k=2),
                                 r=[("wst", slot)], w=[("wdn", wb, kp)])
                        dma_chunk(g + 3)

                    def load_bias(e):
                        wb = e % 2
                        b.dma("sp", bgu[:, wb, :], I["b_gu"][l, e:e + 1, :].rearrange("o (c p) -> p (o c)", p=128),
                              w=[("bgu", wb)], nc_ok=True)
                        b.dma("sp", bdn[:, wb, :], I["b_down"][l, e:e + 1, :].partition_broadcast(128), w=[("bdn", wb)])

                    def load_x(e):
                        xb = e % 2
                        for j in range(NJ):
                            u = j % 2
                            b.dma("sp", xst[:, u, :], xs_d[e * CAP + j * 128:e * CAP + (j + 1) * 128, :],
                                  w=[("xst", u)])
                            pst = ps[6 + u][:].bitcast(BF16)
                            for kc in range(KC):
                                b.tr(pst[:, kc * 128:(kc + 1) * 128], xst[:, u, kc * 128:(kc + 1) * 128], ident_bf[:],
                                     r=[("xst", u), "const"], w=[PS(6 + u)])
                            b.evac(xsT[:, xb, :, j * 128:(j + 1) * 128], pst.rearrange("p (k t) -> p k t", k=KC),
                                   r=[PS(6 + u)], w=[("xsT", xb)])

                    for g in range(3):
                        dma_chunk(g)
                    load_bias(0)
                    load_x(0)
                    for g in range(NCH):
                        cast_chunk(g)
                    halves = [(0, 512), (512, CAP)]
                    for e in range(NE):
                        wb = e % 2
                        xb = e % 2
                        if e + 1 < NE:
                            load_bias(e + 1)
                        for fc in range(16):
                            u = fc % 2
                            for hi, (c0, c1) in enumerate(halves):
                                bank = (fc % 2) * 2 + hi
                                pa = ps[bank]
                                for kc in range(KC):
                                    b.mm(pa[:, 0:c1 - c0], wgu[:, wb, kc, fc * 128:(fc + 1) * 128], xsT[:, xb, kc, c0:c1],
                                         start=(kc == 0), stop=(kc == KC - 1), r=[("wgu", wb, kc), ("xsT", xb)],
                                         w=[PS(bank)])
                                if fc < 8:
                                    b.ts("dve", gsb[:, u, c0:c1], pa[:, 0:c1 - c0], bgu[:, wb, fc:fc + 1], 7.0, ALU.add, ALU.min,
                                         r=[PS(bank), ("bgu", wb)], w=[("gsb", u)])
                                else:
                                    b.act(ub[:, u, c0:c1], pa[:, 0:c1 - c0], AF.Identity, bias=bgu[:, wb, fc:fc + 1],
                                          r=[PS(bank), ("bgu", wb)], w=[("ub", u)])
                            if fc < 8:
                                b.act(sg[:, u, :], gsb[:, u, :], AF.Sigmoid, scale=1.702, r=[("gsb", u)], w=[("sg", u)])
                                b.tt("dve", glu[:, fc, :], gsb[:, u, :], sg[:, u, :], ALU.mult,
                                     r=[("gsb", u), ("sg", u)], w=[("glu", fc)])
                            else:
                                b.ts("dve", ub[:, u, :], ub[:, u, :], 7.0, -7.0, ALU.min, ALU.max, r=[("ub", u)], w=[("ub", u)])
                                b.stt(aT[:, fc - 8, :], ub[:, u, :], 1.0, glu[:, fc - 8, :], ALU.add, ALU.mult,
                                      r=[("ub", u), ("glu", fc - 8)], w=[("aT", fc - 8)])
                            if e + 1 < NE and fc < NCH:
                                cast_chunk((e + 1) * NCH + fc)
                            if e + 1 < NE and fc == 7:
                                load_x(e + 1)
                        for j in range(NJ):
                            u = j % 2
                            for half in range(2):
                                pa = ps[4 + half]
                                for fc in range(8):
                                    b.mm(pa[:], aT[:, fc, j * 128:(j + 1) * 128], wdn[:, wb, fc, half * 512:(half + 1) * 512],
                                         start=(fc == 0), stop=(fc == 7), r=[("aT", fc), ("wdn", wb, fc // 2)], w=[PS(4 + half)])
                                b.tt("dve", yt[:, u, half * 512:(half + 1) * 512], pa[:], bdn[:, wb, half * 512:(half + 1) * 512],
                                     ALU.add, r=[PS(4 + half), ("bdn", wb)], w=[("yt", u)])
                            b.dma("pool", ys_d[e * CAP + j * 128:e * CAP + (j + 1) * 128, :], yt[:, u, :],
                                  r=[("yt", u)])
                    S.end_phase()

            if "cb" in phases:
                with contextlib.ExitStack() as ph:
                    def pb(name, shp, dt):
                        return ph.enter_context(nc.sbuf_tensor(uniq(name), shp, dt))
                    g_bc = pb("g2_bc", [128, D], F32)
                    b_bc = pb("b2_bc", [128, D], F32)
                    b.dma("sp", g_bc[:], I["ln2_g"][l:l + 1, :].partition_broadcast(128), w=["lnp"])
                    b.dma("sp", b_bc[:], I["ln2_b"][l:l + 1, :].partition_broadcast(128), w=["lnp"])
                    xt = pb("xt2", [128, 2, D], F32)
                    rows = pb("rows", [128, 2, 4, D], F32)
                    xo = pb("xo", [128, 2, D], F32)
                    tstat = pb("tstat2", [128, 2, 6], F32)
                    tmv = pb("tmv2", [128, 2], F32)
                    tr_ = pb("tr2_", [128, 2], F32)
                    for tt in range(NT):
                        u = tt % 2
                        b.dma("sp", xt[:, u, :], x1_d[tt * 128:(tt + 1) * 128, :], w=[("xt", u)])
                        for k in range(4):
                            b.gather(rows[:, u, k, :], ys_d[:, :], idx_all[:, tt, k:k + 1],
                                     r=[("idx", tt)], w=[("rows", u, k)])
                        b.ts("dve", xt[:, u, :], xt[:, u, :], DN_ALPHA, None, ALU.mult, r=[("xt", u)], w=[("xt", u)])
                        for k in range(4):
                            b.stt(xt[:, u, :], rows[:, u, k, :], gate_all[:, tt, k:k + 1], xt[:, u, :], ALU.mult, ALU.add,
                                  r=[("rows", u, k), ("gate", tt), ("xt", u)], w=[("xt", u)])
                        layer_norm(xt[:, u, :], g_bc[:], b_bc[:], xo[:, u, :], ("xt", u), ("xo", u), tstat, tmv, tr_)
                        b.dma("sp", xnext[tt * 128:(tt + 1) * 128, :], xo[:, u, :], r=[("xo", u)])
                    S.end_phase()
            xcur = xnext
        S.end_phase(final=True)
    return nc


_CACHE = {}


def kernel(**inputs):
    if "nc" not in _CACHE:
        _CACHE["nc"] = build()
    nc = _CACHE["nc"]
    consts = host_consts()
    x = np.ascontiguousarray(inputs["x"], dtype=np.float32)
    ncores = x.shape[0]
    shared = {k: np.ascontiguousarray(np.asarray(v, dtype=np.float32)) for k, v in inputs.items() if k != "x"}
    in_maps = []
    for c in range(ncores):
        m = dict(shared)
        m.update(consts)
        m["x"] = x[c]
        in_maps.append(m)
    res = run_bass_kernel_spmd(nc, in_maps, core_ids=list(range(ncores)))
    return np.stack([r["y"] for r in res.results], axis=0).astype(np.float32)
```

```python
import contextlib
import math
import numpy as np
import ml_dtypes
import concourse.bass as bass
import concourse.mybir as mybir
from concourse.bass_utils import run_bass_kernel_spmd

F32 = mybir.dt.float32
BF16 = mybir.dt.bfloat16
I32 = mybir.dt.int32
ALU = mybir.AluOpType
AF = mybir.ActivationFunctionType
AX = mybir.AxisListType

ENGS = ("pe", "act", "dve", "pool", "sp")
NDMASEM = 8
SEM_EPOCH = 30000
DMA_EPOCH = 1800

DEPTH = 4
T = 4096
NT = 32
D = 1024
KC = 8
IN_W = 3080
NE = 32
CAP = 640
NROWS = NE * CAP
DN_ALPHA = (2 * DEPTH) ** 0.25
LN_EPS = 1e-5
RMS_EPS = 1e-5


class Op:
    __slots__ = ("eng", "emit", "deps", "isdma", "sig", "sem", "val", "pre", "idx")


class Sched:
    def __init__(self, nc):
        self.nc = nc
        self.ops = {e: [] for e in ENGS}
        self.track = {}
        self.ndma = {e: 0 for e in ENGS}
        self.fence = []
        self.fenced = set(ENGS)

    def barrier(self):
        f = []
        for e in ENGS:
            last = None
            dm = []
            for op in reversed(self.ops[e]):
                if op.isdma:
                    if len(dm) < NDMASEM:
                        dm.append(op)
                elif last is None:
                    last = op
                if last is not None and len(dm) >= NDMASEM:
                    break
            if last is not None:
                f.append(last)
            f.extend(dm)
        self.fence = f
        self.fenced = set()

    def add(self, eng, emit, reads=(), writes=(), dma=False):
        op = Op()
        op.eng = eng
        op.emit = emit
        op.isdma = dma
        op.sig = False
        op.sem = None
        op.val = 0
        op.pre = None
        op.idx = len(self.ops[eng])
        deps = {}

        def need(p, raw):
            if p is op:
                return
            if p.isdma or p.eng != eng or dma:
                deps[id(p)] = p
            elif raw and eng != "pe":
                deps[id(p)] = p

        if eng not in self.fenced:
            self.fenced.add(eng)
            for p in self.fence:
                if p.isdma or p.eng != eng:
                    deps[id(p)] = p
        for k in reads:
            t = self.track.get(k)
            if t is None:
                t = [None, {}]
                self.track[k] = t
            if t[0] is not None:
                need(t[0], True)
        for k in writes:
            t = self.track.get(k)
            if t is None:
                t = [None, {}]
                self.track[k] = t
            if t[0] is not None:
                need(t[0], False)
            for r in t[1].values():
                need(r, False)
        for k in reads:
            t = self.track[k]
            key = (eng, op.idx) if dma else eng
            t[1][key] = op
        for k in writes:
            t = self.track[k]
            t[0] = op
            t[1] = {}
        op.deps = list(deps.values())
        if dma:
            j = self.ndma[eng]
            self.ndma[eng] = j + 1
            r = j // NDMASEM
            ep = r // DMA_EPOCH
            op.sem = ("dma", eng, j % NDMASEM, ep)
            op.val = 16 * (r % DMA_EPOCH + 1)
            if j >= NDMASEM:
                rp = r - 1
                op.pre = (("dma", eng, j % NDMASEM, rp // DMA_EPOCH), 16 * (rp % DMA_EPOCH + 1))
        self.ops[eng].append(op)
        return op

    def end_phase(self, final=False):
        nc = self.nc
        if not hasattr(self, "upto"):
            self.upto = {e: 0 for e in ENGS}
            self.semcount = {e: 0 for e in ENGS}
            self.sems = {}
            self.waited = {e: {} for e in ENGS}
        self.barrier()
        for p in self.fence:
            if not p.isdma:
                p.sig = True
        for e in ENGS:
            for op in self.ops[e][self.upto[e]:]:
                for p in op.deps:
                    if not p.isdma and p.idx >= self.upto[p.eng]:
                        p.sig = True
        for e in ENGS:
            c = self.semcount[e]
            for op in self.ops[e][self.upto[e]:]:
                if (not op.isdma) and op.sig:
                    op.sem = ("c", e, c // SEM_EPOCH)
                    op.val = c % SEM_EPOCH + 1
                    c += 1
            self.semcount[e] = c

        def getsem(k):
            if k not in self.sems:
                self.sems[k] = self.semstack.enter_context(nc.semaphore("s%d" % len(self.sems)))
            return self.sems[k]

        with nc.Block() as block:
            def replay(e, eng):
                waited = self.waited[e]
                for op in self.ops[e][self.upto[e]:]:
                    ws = [(p.sem, p.val) for p in op.deps if p.sem is not None]
                    if op.pre is not None:
                        ws.append(op.pre)
                    for s_, v in ws:
                        if waited.get(s_, 0) < v:
                            eng.wait_ge(getsem(s_), v)
                            waited[s_] = v
                    ins = op.emit(eng)
                    if op.isdma:
                        ins.then_inc(getsem(op.sem), 16)
                    elif op.sig:
                        ins.then_inc(getsem(op.sem), 1)
                    op.emit = None
                if final and e == "sp":
                    for q in ENGS:
                        dm = [op for op in self.ops[q] if op.isdma][-NDMASEM:]
                        for op in dm:
                            if waited.get(op.sem, 0) < op.val:
                                eng.wait_ge(getsem(op.sem), op.val)
                                waited[op.sem] = op.val
                self.upto[e] = len(self.ops[e])

            @block.tensor
            def _(eng):
                replay("pe", eng)

            @block.scalar
            def _(eng):
                replay("act", eng)

            @block.vector
            def _(eng):
                replay("dve", eng)

            @block.gpsimd
            def _(eng):
                replay("pool", eng)

            @block.sync
            def _(eng):
                replay("sp", eng)


class B:
    def __init__(self, nc):
        self.nc = nc
        self.S = Sched(nc)
        self.rr = 0

    def mm(self, out, lhsT, rhs, start=True, stop=True, r=(), w=()):
        self.S.add("pe", lambda e: e.matmul(out, lhsT, rhs, start=start, stop=stop), r, w)

    def tr(self, out, in_, ident, r=(), w=()):
        self.S.add("pe", lambda e: e.transpose(out, in_, ident), r, w)

    def act(self, out, in_, func, bias=0.0, scale=1.0, r=(), w=(), accum_out=None):
        if accum_out is None:
            self.S.add("act", lambda e: e.activation(out, in_, func, bias=bias, scale=scale), r, w)
        else:
            self.S.add("act", lambda e: e.activation(out, in_, func, bias=bias, scale=scale,
                                                     accum_out=accum_out), r, w)

    def ts(self, eng, out, in0, s1, s2, op0, op1=None, r=(), w=(), accum_out=None):
        if op1 is None:
            self.S.add(eng, lambda e: e.tensor_scalar(out, in0, s1, None, op0), r, w)
        elif accum_out is None:
            self.S.add(eng, lambda e: e.tensor_scalar(out, in0, s1, s2, op0, op1), r, w)
        else:
            self.S.add(eng, lambda e: e.tensor_scalar(out, in0, s1, s2, op0, op1, accum_out), r, w)

    def tt(self, eng, out, in0, in1, op, r=(), w=()):
        self.S.add(eng, lambda e: e.tensor_tensor(out, in0, in1, op), r, w)

    def stt(self, out, in0, scalar, in1, op0, op1, r=(), w=(), accum_out=None):
        if accum_out is None:
            self.S.add("dve", lambda e: e.scalar_tensor_tensor(out, in0, scalar, in1, op0, op1), r, w)
        else:
            self.S.add("dve", lambda e: e.scalar_tensor_tensor(out, in0, scalar, in1, op0, op1,
                                                               accum_out), r, w)

    def cp(self, eng, out, in_, r=(), w=()):
        if eng == "act":
            self.S.add("act", lambda e: e.copy(out, in_), r, w)
        else:
            self.S.add(eng, lambda e: e.tensor_copy(out, in_), r, w)

    def evac(self, out, in_, r=(), w=()):
        self.rr ^= 1
        self.cp("act" if self.rr else "dve", out, in_, r, w)

    def memset(self, eng, ap, val, r=(), w=()):
        self.S.add(eng, lambda e: e.memset(ap, val), r, w)

    def dma(self, eng, out, in_, r=(), w=(), nc_ok=False):
        if nc_ok:
            self.S.add(eng, lambda e: e.dma_start(out=out, in_=in_, allow_slow_non_contiguous=True),
                       r, w, dma=True)
        else:
            self.S.add(eng, lambda e: e.dma_start(out=out, in_=in_), r, w, dma=True)

    def scatter(self, out_dram, idx, in_sb, r=(), w=()):
        self.S.add("pool", lambda e: e.indirect_dma_start(
            out=out_dram, out_offset=bass.IndirectOffsetOnAxis(ap=idx, axis=0),
            in_=in_sb, in_offset=None), r, w, dma=True)

    def gather(self, out_sb, in_dram, idx, r=(), w=()):
        self.S.add("pool", lambda e: e.indirect_dma_start(
            out=out_sb, out_offset=None, in_=in_dram,
            in_offset=bass.IndirectOffsetOnAxis(ap=idx, axis=0),
            ), r, w, dma=True)


def host_consts():
    c = {}
    c["ident_bf"] = np.eye(128, dtype=np.float32).astype(ml_dtypes.bfloat16)
    c["ident_f"] = np.eye(128, dtype=np.float32)
    tri = (np.arange(128)[:, None] <= np.arange(128)[None, :]).astype(np.float32)
    c["tri_bf"] = tri.astype(ml_dtypes.bfloat16)
    c["tri_f"] = tri
    c["tris_bf"] = (np.arange(128)[:, None] < np.arange(128)[None, :]).astype(np.float32).astype(ml_dtypes.bfloat16)
    c["ones_bf"] = np.ones((128, 128), dtype=ml_dtypes.bfloat16)
    c["ones_f"] = np.ones((128, 128), dtype=np.float32)
    c["ebase"] = np.broadcast_to((NROWS - np.arange(NE) * CAP).astype(np.float32)[None, :], (128, NE)).copy()
    return c


CONST_SPECS = [("ident_bf", BF16, [128, 128]), ("ident_f", F32, [128, 128]), ("tri_bf", BF16, [128, 128]),
               ("tri_f", F32, [128, 128]), ("tris_bf", BF16, [128, 128]), ("ones_bf", BF16, [128, 128]),
               ("ones_f", F32, [128, 128]), ("ebase", F32, [128, NE])]

IN_SPECS = [("x", [T, D]), ("w_in", [DEPTH, D, IN_W]), ("conv_w", [DEPTH, 4, 512]), ("conv_b", [DEPTH, 512]),
            ("gate_b", [DEPTH, 8]), ("lam_q1", [DEPTH, 64]), ("lam_k1", [DEPTH, 64]), ("lam_q2", [DEPTH, 64]),
            ("lam_k2", [DEPTH, 64]), ("da_norm_g", [DEPTH, 128]), ("w_out", [DEPTH, D, D]),
            ("ln1_g", [DEPTH, D]), ("ln1_b", [DEPTH, D]), ("w_router", [DEPTH, D, NE]), ("b_router", [DEPTH, NE]),
            ("w_gu", [DEPTH, NE, D, 2048]), ("b_gu", [DEPTH, NE, 2048]), ("w_down", [DEPTH, NE, D, D]),
            ("b_down", [DEPTH, NE, D]), ("ln2_g", [DEPTH, D]), ("ln2_b", [DEPTH, D])]


def build(nlayers=DEPTH, phases=("da", "ml", "op", "ex", "cb"), debug=()):
    nc = bass.Bass("TRN2", target_bir_lowering=False)
    I = {}
    for name, shp in IN_SPECS:
        I[name] = nc.dram_tensor(name, shp, F32, kind="ExternalInput").ap()
    for name, dt, shp in CONST_SPECS:
        I[name] = nc.dram_tensor(name, shp, dt, kind="ExternalInput").ap()
    y_out = nc.dram_tensor("y", [T, D], F32, kind="ExternalOutput").ap()

    def scratch(name, shp, dt):
        kind = "ExternalOutput" if name in debug else "Internal"
        return nc.dram_tensor(name, shp, dt, kind=kind).ap()

    mixT_d = scratch("mixT_d", [D, T], BF16)
    x1_d = scratch("x1_d", [T, D], F32)
    xa_d = scratch("xa_d", [T, D], F32)
    xb_d = scratch("xb_d", [T, D], F32)
    xs_d = scratch("xs_d", [NROWS + 1, D], BF16)
    ys_d = scratch("ys_d", [NROWS + 1, D], F32)

    b = B(nc)
    S = b.S
    _cnt = [0]

    def uniq(name):
        _cnt[0] += 1
        return "sb%d_%s" % (_cnt[0], name)
    st = contextlib.ExitStack()
    with st:
        S.semstack = st
        def sb(name, shp, dt):
            return st.enter_context(nc.sbuf_tensor(uniq(name), shp, dt))

        ident_bf = sb("ident_bf", [128, 128], BF16)
        ident_f = sb("ident_f", [128, 128], F32)
        tri_bf = sb("tri_bf", [128, 128], BF16)
        tri_f = sb("tri_f", [128, 128], F32)
        tris_bf = sb("tris_bf", [128, 128], BF16)
        ones_bf = sb("ones_bf", [128, 128], BF16)
        ones_f = sb("ones_f", [128, 128], F32)
        ebase = sb("ebase", [128, NE], F32)
        for name, t_ in (("ident_bf", ident_bf), ("ident_f", ident_f), ("tri_bf", tri_bf), ("tri_f", tri_f),
                         ("tris_bf", tris_bf), ("ones_bf", ones_bf), ("ones_f", ones_f), ("ebase", ebase)):
            b.dma("sp", t_[:], I[name], w=["const"])
        idx_all = sb("idx_all", [128, NT, 4], I32)
        gate_all = sb("gate_all", [128, NT, 4], F32)
        with nc.sbuf_tensor("sb_zrow", [1, D], F32) as zrow:
            b.memset("dve", zrow[:], 0.0, w=["zrow"])
            b.dma("sp", ys_d[NROWS:NROWS + 1, :], zrow[:], r=["zrow"])

        ps_big = [st.enter_context(nc.psum_tensor("psb%d" % i, [128, 1024], F32)) for i in range(4)]
        ps = [ps_big[i // 2][:, (i % 2) * 512:(i % 2 + 1) * 512] for i in range(8)]

        def PS(i):
            return ("ps", i)

        def stream_xT(xsrc, blk, xstage, xT_blk, tag, nbuf=2):
            buf = blk % nbuf
            for j in range(4):
                tt = blk * 4 + j
                sbuf_i = tt % 2
                b.dma("pool", xstage[:, sbuf_i, :], xsrc[tt * 128:(tt + 1) * 128, :],
                      w=[("xstage", sbuf_i)])
                pst = ps[7][:].bitcast(BF16)
                for kc in range(KC):
                    b.tr(pst[:, kc * 128:(kc + 1) * 128], xstage[:, sbuf_i, kc * 128:(kc + 1) * 128], ident_bf[:],
                         r=[("xstage", sbuf_i), "const"], w=[PS(7)])
                b.evac(xT_blk[:, buf, :, j * 128:(j + 1) * 128],
                       pst.rearrange("p (k t) -> p k t", k=KC), r=[PS(7)], w=[(tag, buf)])

        def layer_norm(xt, g_bc, b_bc, out, keyin, keyout, tmpstat, tmpmv, tmpr, aff="pool"):
            b.S.add("dve", lambda e: e.bn_stats(tmpstat[:, 0, :], xt[:, 0:512]), [keyin], ["lnstat"])
            b.S.add("dve", lambda e: e.bn_stats(tmpstat[:, 1, :], xt[:, 512:1024]), [keyin], ["lnstat"])
            b.S.add("dve", lambda e: e.bn_aggr(tmpmv[:], tmpstat[:].rearrange("p a b -> p (a b)")), ["lnstat"], ["lnmv"])
            b.act(tmpr[:, 0:1], tmpmv[:, 1:2], AF.Ln, bias=epsc[:, 0:1], scale=1.0, r=["lnmv", "epsc"], w=["lnr0"])
            b.act(tmpr[:, 1:2], tmpr[:, 0:1], AF.Exp, scale=-0.5, r=["lnr0"], w=["lnr1"])
            b.ts("dve", xt, xt, tmpmv[:, 0:1], tmpr[:, 1:2], ALU.subtract, ALU.mult, r=[keyin, "lnmv", "lnr1"], w=[keyin])
            b.tt(aff, xt, xt, g_bc, ALU.mult, r=[keyin, "lnp"], w=[keyin])
            b.tt(aff, out, xt, b_bc, ALU.add, r=[keyin, "lnp"], w=[keyout])

        epsc = sb("epsc", [128, 2], F32)
        b.memset("dve", epsc[:, 0:1], LN_EPS, w=["epsc"])
        b.memset("dve", epsc[:, 1:2], 128.0 * RMS_EPS, w=["epsc"])

        xcur = I["x"]
        for l in range(nlayers):
            lam_init = 0.8 - 0.6 * math.exp(-0.3 * l)
            xnext = y_out if l == nlayers - 1 else (xa_d if l % 2 == 0 else xb_d)
            if "da" in phases:
                with contextlib.ExitStack() as ph:
                    def pb(name, shp, dt):
                        return ph.enter_context(nc.sbuf_tensor(uniq(name), shp, dt))
                    w_da = pb("w_da", [128, KC, 1536], BF16)
                    for kc in range(KC):
                        b.dma("pool", w_da[:, kc, :], I["w_in"][l, kc * 128:(kc + 1) * 128, 0:1536], w=["w_da"])
                    xstage = pb("xstage", [128, 2, D], BF16)
                    xT_blk = pb("xT_blk", [128, 2, KC, 512], BF16)
                    qT = pb("qT", [128, 4, T], BF16)
                    kT = pb("kT", [128, 4, T], BF16)
                    Vd = pb("Vd", [128, NT, 512], BF16)
                    lamt = pb("lamt", [128, 4, 64], F32)
                    lamv = pb("lamv", [128, 8], F32)
                    gcol = pb("gcol", [128, 2], F32)
                    for i, nm in enumerate(("lam_q1", "lam_k1", "lam_q2", "lam_k2")):
                        b.dma("sp", lamt[:, i, :], I[nm][l:l + 1, :].partition_broadcast(128), w=["lamt"])
                    b.dma("sp", gcol[:, 0:1], I["da_norm_g"][l:l + 1, :].rearrange("o p -> p o"), w=["gcol"], nc_ok=True)
                    b.tt("dve", lamt[:, 0, :], lamt[:, 0, :], lamt[:, 1, :], ALU.mult, r=["lamt"], w=["lamt"])
                    b.tt("dve", lamt[:, 2, :], lamt[:, 2, :], lamt[:, 3, :], ALU.mult, r=["lamt"], w=["lamt"])
                    b.S.add("dve", lambda e: e.reduce_sum(lamv[:, 0:1], lamt[:, 0, :], AX.X), ["lamt"], ["lamv"])
                    b.S.add("dve", lambda e: e.reduce_sum(lamv[:, 1:2], lamt[:, 2, :], AX.X), ["lamt"], ["lamv"])
                    b.act(lamv[:, 2:4], lamv[:, 0:2], AF.Exp, r=["lamv"], w=["lamv2"])
                    b.tt("dve", lamv[:, 4:5], lamv[:, 3:4], lamv[:, 2:3], ALU.subtract, r=["lamv2"], w=["lamv3"])
                    b.ts("dve", lamv[:, 5:6], lamv[:, 4:5], -lam_init, None, ALU.add, r=["lamv3"], w=["neglam"])
                    neglam = lamv[:, 5:6]
                    b.ts("dve", gcol[:, 1:2], gcol[:, 0:1], (1.0 - lam_init) * math.sqrt(128.0), None, ALU.mult,
                         r=["gcol"], w=["gcol2"])
                    for blk in range(8):
                        stream_xT(xcur, blk, xstage, xT_blk, "xT")
                        buf = blk % 2
                        for grp in range(8):
                            pa = ps[grp % 2]
                            for kc in range(KC):
                                b.mm(pa[:], w_da[:, kc, grp * 128:(grp + 1) * 128], xT_blk[:, buf, kc, :],
                                     start=(kc == 0), stop=(kc == KC - 1), r=["w_da", ("xT", buf)], w=[PS(grp % 2)])
                            dst = qT if grp < 4 else kT
                            b.evac(dst[:, grp % 4, blk * 512:(blk + 1) * 512], pa[:], r=[PS(grp % 2)],
                                   w=[("q" if grp < 4 else "k", blk)])
                        for j in range(4):
                            tt = blk * 4 + j
                            pa = ps[j % 2]
                            for kc in range(KC):
                                b.mm(pa[:], xT_blk[:, buf, kc, j * 128:(j + 1) * 128], w_da[:, kc, 1024:1536],
                                     start=(kc == 0), stop=(kc == KC - 1), r=["w_da", ("xT", buf)], w=[PS(j % 2)])
                            b.evac(Vd[:, tt, :], pa[:], r=[PS(j % 2)], w=[("v", tt)])
                    Pt = pb("Pt", [128, 3, 512], BF16)
                    Rc = pb("Rc", [128, 2, 512], F32)
                    rec = pb("rec", [128, 512], F32)
                    oc = pb("oc", [128, 512], F32)
                    sq = pb("sq", [128, 512], F32)
                    rs = pb("rs", [128, 512], F32)
                    ob = pb("ob", [128, 2, 512], BF16)
                    items = []
                    for Qb in range(8):
                        for h in range(4):
                            for c in range(2):
                                nk = (Qb + 1) * 4
                                for kt in range(nk):
                                    items.append((Qb, h, c, kt, nk))

                    def emit_S(i):
                        Qb, h, c, kt, nk = items[i]
                        diag = kt >= Qb * 4
                        q0 = (kt - Qb * 4) * 128 if diag else 0
                        n = 512 - q0
                        lo = c * 64
                        sbank = i % 2
                        b.mm(ps[sbank][:, 0:n], kT[lo:lo + 64, h, kt * 128:(kt + 1) * 128],
                             qT[lo:lo + 64, h, Qb * 512 + q0:(Qb + 1) * 512],
                             r=[("k", kt // 4), ("q", Qb)], w=[PS(sbank)])

                    emit_S(0)
                    for i, (Qb, h, c, kt, nk) in enumerate(items):
                        if i + 1 < len(items):
                            emit_S(i + 1)
                        it = (Qb * 4 + h) * 2 + c
                        Ob = 2 + 2 * (it % 2)
                        Lb = Ob + 1
                        diag = kt >= Qb * 4
                        q0 = (kt - Qb * 4) * 128 if diag else 0
                        n = 512 - q0
                        sbank = i % 2
                        pbuf = i % 3
                        b.act(Pt[:, pbuf, 0:n], ps[sbank][:, 0:n], AF.Exp, scale=0.125,
                              r=[PS(sbank)], w=[("Pt", pbuf)])
                        if diag:
                            b.tt("pool", Pt[:, pbuf, 0:128], Pt[:, pbuf, 0:128], tri_bf[:], ALU.mult,
                                 r=[("Pt", pbuf), "const"], w=[("Pt", pbuf)])
                        b.mm(ps[Ob][:, q0:512], Vd[:, kt, h * 128:(h + 1) * 128], Pt[:, pbuf, 0:n],
                             start=(kt == 0), stop=(kt == nk - 1), r=[("v", kt), ("Pt", pbuf)], w=[PS(Ob)])
                        b.mm(ps[Lb][:, q0:512], ones_bf[:], Pt[:, pbuf, 0:n],
                             start=(kt == 0), stop=(kt == nk - 1), r=["const", ("Pt", pbuf)], w=[PS(Lb)])
                        if kt != nk - 1:
                            continue
                        b.S.add("dve", lambda e, Lb=Lb: e.reciprocal(rec[:], ps[Lb][:]), [PS(Lb)], ["rec"])
                        b.tt("dve", Rc[:, c, :], ps[Ob][:], rec[:], ALU.mult, r=[PS(Ob), "rec"], w=[("Rc", c)])
                        if c != 1:
                            continue
                        b.stt(oc[:], Rc[:, 1, :], neglam, Rc[:, 0, :], ALU.mult, ALU.add,
                              r=[("Rc", 0), ("Rc", 1), "neglam"], w=["oc"])
                        b.act(sq[:], oc[:], AF.Square, r=["oc"], w=["sq"])
                        b.mm(ps[6][:], ones_f[:], sq[:], r=["sq", "const"], w=[PS(6)])
                        b.act(rs[:], ps[6][:], AF.Sqrt, bias=epsc[:, 1:2], r=[PS(6), "epsc"], w=["rs"])
                        b.S.add("dve", lambda e: e.reciprocal(rs[:], rs[:]), ["rs"], ["rs"])
                        ob_i = (Qb * 4 + h) % 2
                        b.stt(ob[:, ob_i, :], oc[:], gcol[:, 1:2], rs[:], ALU.mult, ALU.mult,
                              r=["oc", "rs", "gcol2"], w=[("ob", ob_i)])
                        b.dma("sp", mixT_d[h * 128:(h + 1) * 128, Qb * 512:(Qb + 1) * 512], ob[:, ob_i, :],
                              r=[("ob", ob_i)])
                    S.end_phase()

            if "ml" in phases:
                with contextlib.ExitStack() as ph:
                    def pb(name, shp, dt):
                        return ph.enter_context(nc.sbuf_tensor(uniq(name), shp, dt))
                    mq = pb("mq", [64, 4, T], BF16)
                    mk = pb("mk", [64, 4, T], BF16)
                    mv = pb("mv", [128, NT, 512], BF16)
                    mo = pb("mo", [128, 4, T], BF16)
                    gtm = pb("gtm", [128, NT, 8], F32)
                    lf = pb("lf", [128, NT, 4], F32)
                    btm = pb("btm", [128, NT, 4], F32)
                    gbc = pb("gbc", [128, NT, 4], F32)
                    ew = pb("ew", [128, NT, 4], F32)
                    ebias = pb("ebias", [128, NT, 4], F32)
                    eg = pb("eg", [128, NT, 4], F32)
                    nl8 = pb("nl8", [128, 2], F32)
                    b.memset("dve", nl8[:, 0:1], -math.log(8.0), w=["nl8"])
                    b.memset("dve", nl8[:, 1:2], 1.0, w=["nl8"])
                    with contextlib.ExitStack() as ph2:
                        def pb2(name, shp, dt):
                            return ph2.enter_context(nc.sbuf_tensor(uniq(name), shp, dt))
                        w_ml = pb2("w_ml", [128, KC, 1544], BF16)
                        for kc in range(KC):
                            b.dma("pool", w_ml[:, kc, :], I["w_in"][l, kc * 128:(kc + 1) * 128, 1536:3080], w=["w_ml"])
                        xstage = pb2("xstage", [128, 2, D], BF16)
                        xT_blk = pb2("xT_blk", [128, 1, KC, 512], BF16)
                        gb_bc = pb2("gb_bc", [128, 8], F32)
                        b.dma("sp", gb_bc[:], I["gate_b"][l:l + 1, :].partition_broadcast(128), w=["gb_bc"])
                        cw = pb2("cw", [64, 4, 8], F32)
                        cb = pb2("cb", [64, 8], F32)
                        for j in range(4):
                            b.dma("sp", cw[:, j, :], I["conv_w"][l, j:j + 1, :].rearrange("o (g p) -> p (o g)", p=64), w=["cw"], nc_ok=True)
                        b.dma("sp", cb[:], I["conv_b"][l:l + 1, :].rearrange("o (g p) -> p (o g)", p=64), w=["cw"], nc_ok=True)
                        cin = pb2("cin", [64, 8, 515], F32)
                        ctmp = pb2("ctmp", [64, 2, 512], F32)
                        b.memset("dve", cin[:, :, 0:3], 0.0, w=[("cin", g) for g in range(8)])
                        for blk in range(8):
                            stream_xT(xcur, blk, xstage, xT_blk, "xT", nbuf=1)
                            buf = 0
                            for grp in range(8):
                                pa = ps[grp % 2]
                                for kc in range(KC):
                                    b.mm(pa[0:64, :], w_ml[:, kc, grp * 64:(grp + 1) * 64], xT_blk[:, buf, kc, :],
                                         start=(kc == 0), stop=(kc == KC - 1), r=["w_ml", ("xT", buf)], w=[PS(grp % 2)])
                                b.cp("act", cin[:, grp, 3:515], pa[0:64, :], r=[PS(grp % 2)], w=[("cin", grp)])
                                ct = ctmp[:, grp % 2, :]
                                b.ts("dve", ct, cin[:, grp, 0:512], cw[:, 0, grp:grp + 1], cb[:, grp:grp + 1], ALU.mult, ALU.add,
                                     r=[("cin", grp), "cw"], w=[("ctmp", grp % 2)])
                                for j in range(1, 4):
                                    b.stt(ct, cin[:, grp, j:j + 512], cw[:, j, grp:grp + 1], ct, ALU.mult, ALU.add,
                                          r=[("cin", grp), "cw", ("ctmp", grp % 2)], w=[("ctmp", grp % 2)])
                                dst = mq if grp < 4 else mk
                                b.act(dst[:, grp % 4, blk * 512:(blk + 1) * 512], ct, AF.Silu,
                                      r=[("ctmp", grp % 2)], w=[("mqk", blk)])
                                b.cp("pool", cin[:, grp, 0:3], cin[:, grp, 512:515], r=[("cin", grp)], w=[("cin", grp)])
                            for h in range(4):
                                pa = ps[2 + h % 2]
                                for kc in range(KC):
                                    b.mm(pa[:], w_ml[:, kc, 1024 + h * 128:1024 + (h + 1) * 128], xT_blk[:, buf, kc, :],
                                         start=(kc == 0), stop=(kc == KC - 1), r=["w_ml", ("xT", buf)], w=[PS(2 + h % 2)])
                                b.act(mo[:, h, blk * 512:(blk + 1) * 512], pa[:], AF.Sigmoid, r=[PS(2 + h % 2)], w=[("mo", blk)])
                            for j in range(4):
                                tt = blk * 4 + j
                                pa = ps[4 + j % 2]
                                for kc in range(KC):
                                    b.mm(pa[:], xT_blk[:, buf, kc, j * 128:(j + 1) * 128], w_ml[:, kc, 512:1024],
                                         start=(kc == 0), stop=(kc == KC - 1), r=["w_ml", ("xT", buf)], w=[PS(4 + j % 2)])
                                b.evac(mv[:, tt, :], pa[:], r=[PS(4 + j % 2)], w=[("mv", tt)])
                                for kc in range(KC):
                                    b.mm(ps[6][:, 0:8], xT_blk[:, buf, kc, j * 128:(j + 1) * 128], w_ml[:, kc, 1536:1544],
                                         start=(kc == 0), stop=(kc == KC - 1), r=["w_ml", ("xT", buf)], w=[PS(6)])
                                b.tt("dve", gtm[:, tt, :], ps[6][:, 0:8], gb_bc[:], ALU.add, r=[PS(6), "gb_bc"], w=["gtm"])
                    S.barrier()
                    b.act(lf[:], gtm[:, :, 4:8], AF.Exp, scale=-1.0, r=["gtm"], w=["lf"])
                    b.act(lf[:], lf[:], AF.Ln, bias=nl8[:, 1:2], r=["lf", "nl8"], w=["lf"])
                    b.ts("dve", lf[:], lf[:], -1.0, None, ALU.mult, r=["lf"], w=["lf"])
                    lf2 = lf[:].rearrange("p a b -> p (a b)")
                    b.mm(ps[6][:, 0:128], tri_f[:], lf2, r=["lf", "const"], w=[PS(6)])
                    b.cp("dve", btm[:].rearrange("p a b -> p (a b)"), ps[6][:, 0:128], r=[PS(6)], w=["btm"])
                    b.mm(ps[6][:, 128:256], ones_f[:], lf2, r=["lf", "const"], w=[PS(6)])
                    b.cp("dve", gbc[:].rearrange("p a b -> p (a b)"), ps[6][:, 128:256], r=[PS(6)], w=["gbc"])
                    b.tt("dve", ebias[:], gtm[:, :, 0:4], btm[:], ALU.subtract, r=["gtm", "btm"], w=["ebias"])
                    b.tt("dve", ew[:], ebias[:], gbc[:], ALU.add, r=["ebias", "gbc"], w=["ew"])
                    b.act(ew[:], ew[:], AF.Exp, r=["ew"], w=["ew"])
                    b.ts("dve", ebias[:], ebias[:], -math.log(8.0), None, ALU.add, r=["ebias", "ew"], w=["ebias2"])
                    b.act(eg[:], gbc[:], AF.Exp, r=["gbc"], w=["eg"])
                    Cst = pb("Cst", [64, 4, 256], F32)
                    Cbf = pb("Cbf", [64, 4, 256], BF16)
                    b.memset("dve", Cst[:], 0.0, w=[("C", h) for h in range(4)])
                    b.memset("pool", Cbf[:], 0.0, w=[("Cbf", h) for h in range(4)])
                    lfd = pb("lfd", [128, 2, 4, 128], F32)
                    Et = pb("Et", [128, 2, 4, 128], F32)
                    Eq = pb("Eq", [64, 2, 4, 128], F32)
                    qs = pb("qs", [64, 2, 4, 128], BF16)
                    STt = pb("STt", [128, 2, 4, 128], BF16)
                    kh = pb("kh", [128, 2, 4, 64], BF16)
                    dm = pb("dm", [128, 2, 4, 128], F32)
                    hT = pb("hT", [128, 2, 4, 128], F32)
                    hst = pb("hst", [128, 2, 4, 512], BF16)
                    H4 = range(4)

                    def front(tt):
                        c0 = tt * 128
                        u = tt % 2
                        blk = tt // 4
                        sb_ = 1 if u == 0 else 7
                        for h in H4:
                            b.ts("dve", lfd[:, u, h, :], ones_f[:], lf[:, tt, h:h + 1], None, ALU.mult,
                                 r=["lf", "const"], w=[("lfd", u, h)])
                        for h in H4:
                            b.mm(ps[0][:, h * 128:(h + 1) * 128], lfd[:, u, h, :], tri_f[:], r=[("lfd", u, h), "const"], w=[PS(0)])
                        for h in H4:
                            b.mm(ps[sb_][:, h * 128:(h + 1) * 128], mk[:, h, c0:c0 + 128], mq[:, h, c0:c0 + 128],
                                 r=[("mqk", blk)], w=[PS(sb_)])
                        pkt = ps[4][:].bitcast(BF16)
                        for h in H4:
                            b.tr(pkt[:, h * 64:(h + 1) * 64], mk[:, h, c0:c0 + 128], ident_bf[0:64, 0:64],
                                 r=[("mqk", blk), "const"], w=[PS(4)])
                        for h in H4:
                            b.act(Et[:, u, h, :], ps[0][:, h * 128:(h + 1) * 128], AF.Exp, bias=ebias[:, tt, h:h + 1],
                                  r=[PS(0), "ebias2"], w=[("Et", u, h)])
                        b.act(Eq[:, u, :, :], ps[0][0:64, :].rearrange("p (h t) -> p h t", h=4), AF.Exp, bias=nl8[0:64, 0:1],
                              r=[PS(0), "nl8"], w=[("Eq", u)])
                        for h in H4:
                            b.tt("pool", Et[:, u, h, :], Et[:, u, h, :], tri_f[:], ALU.mult, r=[("Et", u, h), "const"], w=[("Et", u, h)])

                    def front_b(tt):
                        c0 = tt * 128
                        u = tt % 2
                        blk = tt // 4
                        pkt = ps[4][:].bitcast(BF16)
                        b.tt("dve", qs[:, u, :, :], mq[:, :, c0:c0 + 128], Eq[:, u, :, :], ALU.mult,
                             r=[("mqk", blk), ("Eq", u)], w=[("qs", u)])
                        for h in H4:
                            b.ts("dve", kh[:, u, h, :], pkt[:, h * 64:(h + 1) * 64], ew[:, tt, h:h + 1], None, ALU.mult,
                                 r=[PS(4), "ew"], w=[("kh", u, h)])

                    def back(tt):
                        c0 = tt * 128
                        u = tt % 2
                        blk = tt // 4
                        sb_ = 1 if u == 0 else 7
                        b.tt("dve", STt[:, u, :, :], ps[sb_][:].rearrange("p (h t) -> p h t", h=4), Et[:, u, :, :], ALU.mult,
                             r=[PS(sb_)] + [("Et", u, h) for h in H4], w=[("ST", u)])
                        for h in H4:
                            b.mm(ps[2][:, h * 128:(h + 1) * 128], mv[:, tt, h * 128:(h + 1) * 128], STt[:, u, h, :],
                                 start=True, stop=False, r=[("mv", tt), ("ST", u)], w=[PS(2)])
                            b.mm(ps[2][:, h * 128:(h + 1) * 128], Cbf[:, h, 0:128], qs[:, u, h, :],
                                 start=False, stop=True, r=[("Cbf", h), ("qs", u)], w=[PS(2)])
                        for h in H4:
                            b.mm(ps[3][:, h * 128:(h + 1) * 128], ones_bf[:], STt[:, u, h, :],
                                 start=True, stop=False, r=["const", ("ST", u)], w=[PS(3)])
                            b.mm(ps[3][:, h * 128:(h + 1) * 128], Cbf[:, h, 128:256], qs[:, u, h, :],
                                 start=False, stop=True, r=[("Cbf", h), ("qs", u)], w=[PS(3)])
                        for h in H4:
                            pu = ps[5 + h // 2]
                            o0 = (h % 2) * 256
                            b.mm(pu[0:64, o0:o0 + 128], kh[:, u, h, :], mv[:, tt, h * 128:(h + 1) * 128],
                                 r=[("kh", u, h), ("mv", tt)], w=[PS(5 + h // 2)])
                            b.mm(pu[0:64, o0 + 128:o0 + 256], kh[:, u, h, :], ones_bf[:], r=[("kh", u, h), "const"],
                                 w=[PS(5 + h // 2)])

                    def back_2(tt):
                        c0 = tt * 128
                        u = tt % 2
                        blk = tt // 4
                        b.act(dm[:, u, :, :], ps[3][:].rearrange("p (h t) -> p h t", h=4), AF.Abs,
                              r=[PS(3)], w=[("dm", u)])
                        b.ts("dve", dm[:, u, :, :], dm[:, u, :, :], 1.0, None, ALU.max, r=[("dm", u)], w=[("dm", u)])
                        b.S.add("dve", lambda e, u=u: e.reciprocal(dm[:, u, :, :], dm[:, u, :, :]), [("dm", u)], [("dm", u)])
                        b.tt("dve", hT[:, u, :, :], ps[2][:].rearrange("p (h t) -> p h t", h=4), dm[:, u, :, :], ALU.mult,
                             r=[PS(2), ("dm", u)], w=[("hT", u)])
                        hp = blk % 2
                        b.tt("dve", hst[:, hp, :, (tt % 4) * 128:(tt % 4 + 1) * 128], hT[:, u, :, :],
                             mo[:, :, c0:c0 + 128], ALU.mult, r=[("hT", u), ("mo", blk)], w=[("hst", hp)])
                        if tt % 4 == 3:
                            for h in H4:
                                b.dma("sp", mixT_d[512 + h * 128:512 + (h + 1) * 128, blk * 512:(blk + 1) * 512],
                                      hst[:, hp, h, :], r=[("hst", hp)])
                        for h in H4:
                            pu = ps[5 + h // 2]
                            o0 = (h % 2) * 256
                            b.stt(Cst[:, h, :], Cst[:, h, :], eg[0:64, tt, h:h + 1], pu[0:64, o0:o0 + 256], ALU.mult, ALU.add,
                                  r=[("C", h), "eg", PS(5 + h // 2)], w=[("C", h)])
                        b.cp("act", Cbf[:], Cst[:], r=[("C", h) for h in H4], w=[("Cbf", h) for h in H4])

                    front(0)
                    front_b(0)
                    for tt in range(NT):
                        if tt + 1 < NT:
                            front(tt + 1)
                        back(tt)
                        if tt + 1 < NT:
                            front_b(tt + 1)
                        back_2(tt)
                    S.end_phase()

            if "op" in phases:
                with contextlib.ExitStack() as ph:
                    def pb(name, shp, dt):
                        return ph.enter_context(nc.sbuf_tensor(uniq(name), shp, dt))
                    w_o = pb("w_o", [128, KC, D], BF16)
                    for kc in range(KC):
                        b.dma("pool", w_o[:, kc, :], I["w_out"][l, kc * 128:(kc + 1) * 128, :], w=["w_o"])
                    w_r = pb("w_r", [128, KC, NE], F32)
                    b.dma("sp", w_r[:], I["w_router"][l].rearrange("(k p) e -> p k e", p=128), w=["w_r"])
                    br_bc = pb("br_bc", [128, NE], F32)
                    b.dma("sp", br_bc[:], I["b_router"][l:l + 1, :].partition_broadcast(128), w=["w_r"])
                    g_bc = pb("g_bc", [128, D], F32)
                    b_bc = pb("b_bc", [128, D], F32)
                    b.dma("sp", g_bc[:], I["ln1_g"][l:l + 1, :].partition_broadcast(128), w=["lnp"])
                    b.dma("sp", b_bc[:], I["ln1_b"][l:l + 1, :].partition_broadcast(128), w=["lnp"])
                    mixb = pb("mixb", [128, 2, KC, 512], BF16)
                    xt = pb("xt", [128, 2, D], F32)
                    x1t = pb("x1t", [128, 2, D], F32)
                    x1b = pb("x1b", [128, 2, D], BF16)
                    x1T = pb("x1T", [128, KC, 128], F32)
                    tstat = pb("tstat", [128, 2, 6], F32)
                    tmv = pb("tmv", [128, 2], F32)
                    tr_ = pb("tr_", [128, 2], F32)
                    lg = pb("lg", [128, NE], F32)
                    m8 = pb("m8", [128, 8], F32)
                    msk = pb("msk", [128, NE], F32)
                    mskb = pb("mskb", [128, NE], BF16)
                    ex = pb("ex", [128, NE], F32)
                    G = pb("G", [128, NE], F32)
                    gs = pb("gs", [128, 4], F32)
                    cnt = pb("cnt", [128, NE], F32)
                    pos = pb("pos", [128, NE], F32)
                    okm = pb("okm", [128, NE], F32)
                    val = pb("val", [128, NE], F32)
                    v8 = pb("v8", [128, 8], F32)
                    idf = pb("idf", [128, 4], F32)
                    junk = pb("junk", [128, NE], F32)
                    b.memset("dve", cnt[:], 0.0, w=["cnt"])
                    for blk in range(8):
                        mb = blk % 2
                        b.dma("sp", mixb[:, mb, :, :], mixT_d[:, blk * 512:(blk + 1) * 512].rearrange("(k p) t -> p k t", p=128),
                              w=[("mixb", mb)])
                        for j in range(4):
                            tt = blk * 4 + j
                            u = tt % 2
                            b.dma("sp", xt[:, u, :], xcur[tt * 128:(tt + 1) * 128, :], w=[("xt", u)])
                            for half in range(2):
                                pa = ps[half]
                                for kc in range(KC):
                                    b.mm(pa[:], mixb[:, mb, kc, j * 128:(j + 1) * 128], w_o[:, kc, half * 512:(half + 1) * 512],
                                         start=(kc == 0), stop=(kc == KC - 1), r=[("mixb", mb), "w_o"], w=[PS(half)])
                                b.stt(xt[:, u, half * 512:(half + 1) * 512], xt[:, u, half * 512:(half + 1) * 512], DN_ALPHA,
                                      pa[:], ALU.mult, ALU.add, r=[("xt", u), PS(half)], w=[("xt", u)])
                            layer_norm(xt[:, u, :], g_bc[:], b_bc[:], x1t[:, u, :], ("xt", u), ("x1t", u), tstat, tmv, tr_)
                            b.dma("sp", x1_d[tt * 128:(tt + 1) * 128, :], x1t[:, u, :], r=[("x1t", u)])
                            b.cp("act", x1b[:, u, :], x1t[:, u, :], r=[("x1t", u)], w=[("x1b", u)])
                            for kc in range(KC):
                                b.tr(ps[2 + kc // 4][:, (kc % 4) * 128:(kc % 4 + 1) * 128], x1t[:, u, kc * 128:(kc + 1) * 128],
                                     ident_f[:], r=[("x1t", u), "const"], w=[PS(2 + kc // 4)])
                            b.cp("act", x1T[:, 0:4, :], ps[2][:].rearrange("p (k t) -> p k t", k=4), r=[PS(2)], w=["x1T"])
                            b.cp("dve", x1T[:, 4:8, :], ps[3][:].rearrange("p (k t) -> p k t", k=4), r=[PS(3)], w=["x1T"])
                            for kc in range(KC):
                                b.mm(ps[4][:, 0:NE], x1T[:, kc, :], w_r[:, kc, :], start=(kc == 0), stop=(kc == KC - 1),
                                     r=["x1T", "w_r"], w=[PS(4)])
                            b.tt("dve", lg[:], ps[4][:, 0:NE], br_bc[:], ALU.add, r=[PS(4), "w_r"], w=["lg"])
                            b.S.add("dve", lambda e: e.max(m8[:], lg[:]), ["lg"], ["m8"])
                            b.ts("dve", msk[:], lg[:], m8[:, 3:4], None, ALU.is_ge, r=["lg", "m8"], w=["msk"])
                            b.cp("pool", mskb[:], msk[:], r=["msk"], w=["mskb"])
                            b.ts("dve", ex[:], lg[:], m8[:, 0:1], None, ALU.subtract, r=["lg", "m8"], w=["ex"])
                            b.act(ex[:], ex[:], AF.Exp, r=["ex"], w=["ex"])
                            b.stt(G[:], ex[:], 1.0, msk[:], ALU.mult, ALU.mult, r=["ex", "msk"], w=["G"], accum_out=gs[:, 0:1])
                            b.S.add("dve", lambda e: e.reciprocal(gs[:, 1:2], gs[:, 0:1]), ["G"], ["gs"])
                            b.ts("dve", G[:], G[:], gs[:, 1:2], None, ALU.mult, r=["G", "gs"], w=["G"])
                            b.mm(ps[5][:, 0:NE], tris_bf[:], mskb[:], r=["mskb", "const"], w=[PS(5)])
                            b.mm(ps[5][:, NE:2 * NE], ones_bf[:], mskb[:], r=["mskb", "const"], w=[PS(5)])
                            b.tt("dve", pos[:], ps[5][:, 0:NE], cnt[:], ALU.add, r=[PS(5), "cnt"], w=["pos"])
                            b.tt("dve", cnt[:], ps[5][:, NE:2 * NE], cnt[:], ALU.add, r=[PS(5), "cnt", "pos"], w=["cnt"])
                            b.ts("dve", okm[:], pos[:], float(CAP), None, ALU.is_lt, r=["pos"], w=["okm"])
                            b.tt("dve", okm[:], okm[:], msk[:], ALU.mult, r=["okm", "msk"], w=["okm"])
                            b.tt("dve", val[:], ebase[:], pos[:], ALU.subtract, r=["pos", "const"], w=["val"])
                            b.tt("dve", val[:], val[:], okm[:], ALU.mult, r=["val", "okm"], w=["val"])
                            b.S.add("dve", lambda e: e.max(v8[:], val[:]), ["val"], ["v8"])
                            b.ts("dve", idf[:], v8[:, 0:4], -1.0, float(NROWS), ALU.mult, ALU.add, r=["v8"], w=["idf"])
                            b.cp("dve", idx_all[:, tt, :], idf[:], r=["idf"], w=[("idx", tt)])
                            for k in range(4):
                                b.stt(junk[:], val[:], v8[:, k:k + 1], G[:], ALU.is_equal, ALU.mult,
                                      r=["val", "v8", "G"], w=["junk", ("gate", tt)], accum_out=gate_all[:, tt, k:k + 1])
                                b.scatter(xs_d[:, :], idx_all[:, tt, k:k + 1], x1b[:, u, :],
                                          r=[("idx", tt), ("x1b", u)])
                    S.end_phase()

            if "ex" in phases:
                with contextlib.ExitStack() as ph:
                    def pb(name, shp, dt):
                        return ph.enter_context(nc.sbuf_tensor(uniq(name), shp, dt))
                    NJ = CAP // 128
                    NCH = 12
                    wgu = pb("wgu", [128, 2, KC, 2048], BF16)
                    wdn = pb("wdn", [128, 2, KC, D], BF16)
                    wst = pb("wst", [128, 3, 2048], F32)
                    bgu = pb("bgu", [128, 2, 16], F32)
                    bgu1 = pb("bgu1", [128, 2, 8], F32)
                    bdn = pb("bdn", [128, 2, D], F32)
                    xst = pb("xst", [128, 2, D], BF16)
                    xsT = pb("xsT", [128, 2, KC, CAP], BF16)
                    gsb = pb("gsb", [128, 2, CAP], F32)
                    sg = pb("sg", [128, 2, CAP], F32)
                    glu = pb("glu", [128, 8, CAP], BF16)
                    ub = pb("ub", [128, 2, CAP], F32)
                    aT = pb("aT", [128, 8, CAP], BF16)
                    yt = pb("yt", [128, 2, D], F32)
                    NG = NE * NCH

                    def dma_chunk(g):
                        if g >= NG:
                            return
                        e, i = divmod(g, NCH)
                        slot = g % 3
                        if i < 8:
                            b.dma("sp", wst[:, slot, :], I["w_gu"][l, e, i * 128:(i + 1) * 128, :], w=[("wst", slot)])
                        else:
                            kp = i - 8
                            b.dma("sp", wst[:, slot, :].rearrange("p (k d) -> p k d", k=2),
                                  I["w_down"][l, e, kp * 256:(kp + 1) * 256, :].rearrange("(k p) d -> p k d", p=128),
                                  w=[("wst", slot)])

                    def cast_chunk(g):
                        if g >= NG:
                            return
                        e, i = divmod(g, NCH)
                        slot = g % 3
                        wb = e % 2
                        eng = "act" if g % 2 == 0 else "dve"
                        if i < 8:
                            b.cp(eng, wgu[:, wb, i, :], wst[:, slot, :], r=[("wst", slot)], w=[("wgu", wb, i)])
                        else:
                            kp = i - 8
                            b.cp(eng, wdn[:, wb, 2 * kp:2 * kp + 2, :], wst[:, slot, :].rearrange("p (k d) -> p k d", k=2),
                                 r=[("wst", slot)], w=[("wdn", wb, kp)])
                        dma_chunk(g + 3)

                    def load_bias(e):
                        wb = e % 2
                        b.dma("sp", bgu[:, wb, :], I["b_gu"][l, e:e + 1, :].rearrange("o (c p) -> p (o c)", p=128),
                              w=[("bgu", wb)], nc_ok=True)
                        b.dma("sp", bdn[:, wb, :], I["b_down"][l, e:e + 1, :].partition_broadcast(128), w=[("bdn", wb)])
                        b.ts("pool", bgu1[:, wb, :], bgu[:, wb, 8:16], 1.0, None, ALU.add, r=[("bgu", wb)], w=[("bgu1", wb)])

                    def load_x(e):
                        xb = e % 2
                        for j in range(NJ):
                            u = j % 2
                            b.dma("sp", xst[:, u, :], xs_d[e * CAP + j * 128:e * CAP + (j + 1) * 128, :],
                                  w=[("xst", u)])
                            pst = ps[6 + u][:].bitcast(BF16)
                            for kc in range(KC):
                                b.tr(pst[:, kc * 128:(kc + 1) * 128], xst[:, u, kc * 128:(kc + 1) * 128], ident_bf[:],
                                     r=[("xst", u), "const"], w=[PS(6 + u)])
                            b.evac(xsT[:, xb, :, j * 128:(j + 1) * 128], pst.rearrange("p (k t) -> p k t", k=KC),
                                   r=[PS(6 + u)], w=[("xsT", xb)])

                    for g in range(3):
                        dma_chunk(g)
                    load_bias(0)
                    load_x(0)
                    for g in range(NCH):
                        cast_chunk(g)
                    halves = [(0, 512), (512, CAP)]
                    for e in range(NE):
                        wb = e % 2
                        xb = e % 2
                        if e + 1 < NE:
                            load_bias(e + 1)
                        for fc in range(16):
                            u = fc % 2
                            pbig = ps_big[fc % 2]
                            bk = [PS((fc % 2) * 2), PS((fc % 2) * 2 + 1)]
                            for hi, (c0, c1) in enumerate(halves):
                                for kc in range(KC):
                                    b.mm(pbig[:, c0:c1], wgu[:, wb, kc, fc * 128:(fc + 1) * 128], xsT[:, xb, kc, c0:c1],
                                         start=(kc == 0), stop=(kc == KC - 1), r=[("wgu", wb, kc), ("xsT", xb)],
                                         w=[bk[hi]])
                            if fc < 8:
                                b.ts("dve", gsb[:, u, :], pbig[:, 0:CAP], bgu[:, wb, fc:fc + 1], 7.0, ALU.add, ALU.min,
                                     r=bk + [("bgu", wb)], w=[("gsb", u)])
                                b.act(sg[:, u, :], gsb[:, u, :], AF.Sigmoid, scale=1.702, r=[("gsb", u)], w=[("sg", u)])
                                b.tt("pool", glu[:, fc, :], gsb[:, u, :], sg[:, u, :], ALU.mult,
                                     r=[("gsb", u), ("sg", u)], w=[("glu", fc)])
                            else:
                                b.act(ub[:, u, :], pbig[:, 0:CAP], AF.Identity, bias=bgu1[:, wb, fc - 8:fc - 7],
                                      r=bk + [("bgu1", wb)], w=[("ub", u)])
                                b.ts("dve", ub[:, u, :], ub[:, u, :], 8.0, -6.0, ALU.min, ALU.max, r=[("ub", u)], w=[("ub", u)])
                                b.tt("pool", aT[:, fc - 8, :], ub[:, u, :], glu[:, fc - 8, :], ALU.mult,
                                     r=[("ub", u), ("glu", fc - 8)], w=[("aT", fc - 8)])
                            if e + 1 < NE and fc < NCH:
                                cast_chunk((e + 1) * NCH + fc)
                        if e + 1 < NE:
                            load_x(e + 1)
                        for j in range(NJ):
                            u = j % 2
                            for half in range(2):
                                pa = ps[4 + half]
                                for fc in range(8):
                                    b.mm(pa[:], aT[:, fc, j * 128:(j + 1) * 128], wdn[:, wb, fc, half * 512:(half + 1) * 512],
                                         start=(fc == 0), stop=(fc == 7), r=[("aT", fc), ("wdn", wb, fc // 2)], w=[PS(4 + half)])
                                b.tt("dve", yt[:, u, half * 512:(half + 1) * 512], pa[:], bdn[:, wb, half * 512:(half + 1) * 512],
                                     ALU.add, r=[PS(4 + half), ("bdn", wb)], w=[("yt", u)])
                            b.dma("pool", ys_d[e * CAP + j * 128:e * CAP + (j + 1) * 128, :], yt[:, u, :],
                                  r=[("yt", u)])
                    S.end_phase()

            if "cb" in phases:
                with contextlib.ExitStack() as ph:
                    def pb(name, shp, dt):
                        return ph.enter_context(nc.sbuf_tensor(uniq(name), shp, dt))
                    g_bc = pb("g2_bc", [128, D], F32)
                    b_bc = pb("b2_bc", [128, D], F32)
                    b.dma("sp", g_bc[:], I["ln2_g"][l:l + 1, :].partition_broadcast(128), w=["lnp"])
                    b.dma("sp", b_bc[:], I["ln2_b"][l:l + 1, :].partition_broadcast(128), w=["lnp"])
                    xt = pb("xt2", [128, 2, D], F32)
                    rows = pb("rows", [128, 2, 4, D], F32)
                    xo = pb("xo", [128, 2, D], F32)
                    tstat = pb("tstat2", [128, 2, 6], F32)
                    tmv = pb("tmv2", [128, 2], F32)
                    tr_ = pb("tr2_", [128, 2], F32)
                    def cb_load(tt):
                        u = tt % 2
                        b.dma("sp", xt[:, u, :], x1_d[tt * 128:(tt + 1) * 128, :], w=[("xt", u)])
                        for k in range(4):
                            b.gather(rows[:, u, k, :], ys_d[:, :], idx_all[:, tt, k:k + 1],
                                     r=[("idx", tt)], w=[("rows", u, k)])

                    cb_load(0)
                    for tt in range(NT):
                        u = tt % 2
                        if tt + 1 < NT:
                            cb_load(tt + 1)
                        b.ts("dve", xt[:, u, :], xt[:, u, :], DN_ALPHA, None, ALU.mult, r=[("xt", u)], w=[("xt", u)])
                        for k in range(4):
                            b.stt(xt[:, u, :], rows[:, u, k, :], gate_all[:, tt, k:k + 1], xt[:, u, :], ALU.mult, ALU.add,
                                  r=[("rows", u, k), ("gate", tt), ("xt", u)], w=[("xt", u)])
                        layer_norm(xt[:, u, :], g_bc[:], b_bc[:], xo[:, u, :], ("xt", u), ("xo", u), tstat, tmv, tr_, aff="dve")
                        b.dma("sp", xnext[tt * 128:(tt + 1) * 128, :], xo[:, u, :], r=[("xo", u)])
                    S.end_phase()
            xcur = xnext
        S.end_phase(final=True)
    return nc


_CACHE = {}


def kernel(**inputs):
    if "nc" not in _CACHE:
        _CACHE["nc"] = build()
    nc = _CACHE["nc"]
    consts = host_consts()
    x = np.ascontiguousarray(inputs["x"], dtype=np.float32)
    ncores = x.shape[0]
    shared = {k: np.ascontiguousarray(np.asarray(v, dtype=np.float32)) for k, v in inputs.items() if k != "x"}
    in_maps = []
    for c in range(ncores):
        m = dict(shared)
        m.update(consts)
        m["x"] = x[c]
        in_maps.append(m)
    res = run_bass_kernel_spmd(nc, in_maps, core_ids=list(range(ncores)))
    return np.stack([r["y"] for r in res.results], axis=0).astype(np.float32)
```

```python
import contextlib
import math
import numpy as np
import ml_dtypes
import concourse.bass as bass
import concourse.mybir as mybir
from concourse.bass_utils import run_bass_kernel_spmd

F32 = mybir.dt.float32
BF16 = mybir.dt.bfloat16
I32 = mybir.dt.int32
ALU = mybir.AluOpType
AF = mybir.ActivationFunctionType
AX = mybir.AxisListType

ENGS = ("pe", "act", "dve", "pool", "sp")
NDMASEM = 8
SEM_EPOCH = 30000
DMA_EPOCH = 1800

DEPTH = 4
T = 4096
NT = 32
D = 1024
KC = 8
IN_W = 3080
NE = 32
CAP = 640
NROWS = NE * CAP
DN_ALPHA = (2 * DEPTH) ** 0.25
LN_EPS = 1e-5
RMS_EPS = 1e-5


class Op:
    __slots__ = ("eng", "emit", "deps", "isdma", "sig", "sem", "val", "pre", "idx")


class Sched:
    def __init__(self, nc):
        self.nc = nc
        self.ops = {e: [] for e in ENGS}
        self.track = {}
        self.ndma = {e: 0 for e in ENGS}
        self.fence = []
        self.fenced = set(ENGS)

    def barrier(self):
        f = []
        for e in ENGS:
            last = None
            dm = []
            for op in reversed(self.ops[e]):
                if op.isdma:
                    if len(dm) < NDMASEM:
                        dm.append(op)
                elif last is None:
                    last = op
                if last is not None and len(dm) >= NDMASEM:
                    break
            if last is not None:
                f.append(last)
            f.extend(dm)
        self.fence = f
        self.fenced = set()

    def add(self, eng, emit, reads=(), writes=(), dma=False):
        op = Op()
        op.eng = eng
        op.emit = emit
        op.isdma = dma
        op.sig = False
        op.sem = None
        op.val = 0
        op.pre = None
        op.idx = len(self.ops[eng])
        deps = {}

        def need(p, raw):
            if p is op:
                return
            if p.isdma or p.eng != eng or dma:
                deps[id(p)] = p
            elif raw and eng != "pe":
                deps[id(p)] = p

        if eng not in self.fenced:
            self.fenced.add(eng)
            for p in self.fence:
                if p.isdma or p.eng != eng:
                    deps[id(p)] = p
        for k in reads:
            t = self.track.get(k)
            if t is None:
                t = [None, {}]
                self.track[k] = t
            if t[0] is not None:
                need(t[0], True)
        for k in writes:
            t = self.track.get(k)
            if t is None:
                t = [None, {}]
                self.track[k] = t
            if t[0] is not None:
                need(t[0], False)
            for r in t[1].values():
                need(r, False)
        for k in reads:
            t = self.track[k]
            key = (eng, op.idx) if dma else eng
            t[1][key] = op
        for k in writes:
            t = self.track[k]
            t[0] = op
            t[1] = {}
        op.deps = list(deps.values())
        if dma:
            j = self.ndma[eng]
            self.ndma[eng] = j + 1
            r = j // NDMASEM
            ep = r // DMA_EPOCH
            op.sem = ("dma", eng, j % NDMASEM, ep)
            op.val = 16 * (r % DMA_EPOCH + 1)
            if j >= NDMASEM:
                rp = r - 1
                op.pre = (("dma", eng, j % NDMASEM, rp // DMA_EPOCH), 16 * (rp % DMA_EPOCH + 1))
        self.ops[eng].append(op)
        return op

    def end_phase(self, final=False):
        nc = self.nc
        if not hasattr(self, "upto"):
            self.upto = {e: 0 for e in ENGS}
            self.semcount = {e: 0 for e in ENGS}
            self.sems = {}
            self.waited = {e: {} for e in ENGS}
        self.barrier()
        for p in self.fence:
            if not p.isdma:
                p.sig = True
        for e in ENGS:
            for op in self.ops[e][self.upto[e]:]:
                for p in op.deps:
                    if not p.isdma and p.idx >= self.upto[p.eng]:
                        p.sig = True
        for e in ENGS:
            c = self.semcount[e]
            for op in self.ops[e][self.upto[e]:]:
                if (not op.isdma) and op.sig:
                    op.sem = ("c", e, c // SEM_EPOCH)
                    op.val = c % SEM_EPOCH + 1
                    c += 1
            self.semcount[e] = c

        def getsem(k):
            if k not in self.sems:
                self.sems[k] = self.semstack.enter_context(nc.semaphore("s%d" % len(self.sems)))
            return self.sems[k]

        with nc.Block() as block:
            def replay(e, eng):
                waited = self.waited[e]
                for op in self.ops[e][self.upto[e]:]:
                    ws = [(p.sem, p.val) for p in op.deps if p.sem is not None]
                    if op.pre is not None:
                        ws.append(op.pre)
                    for s_, v in ws:
                        if waited.get(s_, 0) < v:
                            eng.wait_ge(getsem(s_), v)
                            waited[s_] = v
                    ins = op.emit(eng)
                    if op.isdma:
                        ins.then_inc(getsem(op.sem), 16)
                    elif op.sig:
                        ins.then_inc(getsem(op.sem), 1)
                    op.emit = None
                if final and e == "sp":
                    for q in ENGS:
                        dm = [op for op in self.ops[q] if op.isdma][-NDMASEM:]
                        for op in dm:
                            if waited.get(op.sem, 0) < op.val:
                                eng.wait_ge(getsem(op.sem), op.val)
                                waited[op.sem] = op.val
                self.upto[e] = len(self.ops[e])

            @block.tensor
            def _(eng):
                replay("pe", eng)

            @block.scalar
            def _(eng):
                replay("act", eng)

            @block.vector
            def _(eng):
                replay("dve", eng)

            @block.gpsimd
            def _(eng):
                replay("pool", eng)

            @block.sync
            def _(eng):
                replay("sp", eng)


class B:
    def __init__(self, nc):
        self.nc = nc
        self.S = Sched(nc)
        self.rr = 0

    def mm(self, out, lhsT, rhs, start=True, stop=True, r=(), w=()):
        self.S.add("pe", lambda e: e.matmul(out, lhsT, rhs, start=start, stop=stop), r, w)

    def tr(self, out, in_, ident, r=(), w=()):
        self.S.add("pe", lambda e: e.transpose(out, in_, ident), r, w)

    def act(self, out, in_, func, bias=0.0, scale=1.0, r=(), w=(), accum_out=None):
        if accum_out is None:
            self.S.add("act", lambda e: e.activation(out, in_, func, bias=bias, scale=scale), r, w)
        else:
            self.S.add("act", lambda e: e.activation(out, in_, func, bias=bias, scale=scale,
                                                     accum_out=accum_out), r, w)

    def ts(self, eng, out, in0, s1, s2, op0, op1=None, r=(), w=(), accum_out=None):
        if op1 is None:
            self.S.add(eng, lambda e: e.tensor_scalar(out, in0, s1, None, op0), r, w)
        elif accum_out is None:
            self.S.add(eng, lambda e: e.tensor_scalar(out, in0, s1, s2, op0, op1), r, w)
        else:
            self.S.add(eng, lambda e: e.tensor_scalar(out, in0, s1, s2, op0, op1, accum_out), r, w)

    def tt(self, eng, out, in0, in1, op, r=(), w=()):
        self.S.add(eng, lambda e: e.tensor_tensor(out, in0, in1, op), r, w)

    def stt(self, out, in0, scalar, in1, op0, op1, r=(), w=(), accum_out=None):
        if accum_out is None:
            self.S.add("dve", lambda e: e.scalar_tensor_tensor(out, in0, scalar, in1, op0, op1), r, w)
        else:
            self.S.add("dve", lambda e: e.scalar_tensor_tensor(out, in0, scalar, in1, op0, op1,
                                                               accum_out), r, w)

    def cp(self, eng, out, in_, r=(), w=()):
        if eng == "act":
            self.S.add("act", lambda e: e.copy(out, in_), r, w)
        else:
            self.S.add(eng, lambda e: e.tensor_copy(out, in_), r, w)

    def evac(self, out, in_, r=(), w=()):
        self.rr ^= 1
        self.cp("act" if self.rr else "dve", out, in_, r, w)

    def memset(self, eng, ap, val, r=(), w=()):
        self.S.add(eng, lambda e: e.memset(ap, val), r, w)

    def dma(self, eng, out, in_, r=(), w=(), nc_ok=False):
        if nc_ok:
            self.S.add(eng, lambda e: e.dma_start(out=out, in_=in_, allow_slow_non_contiguous=True),
                       r, w, dma=True)
        else:
            self.S.add(eng, lambda e: e.dma_start(out=out, in_=in_), r, w, dma=True)

    def scatter(self, out_dram, idx, in_sb, r=(), w=()):
        self.S.add("pool", lambda e: e.indirect_dma_start(
            out=out_dram, out_offset=bass.IndirectOffsetOnAxis(ap=idx, axis=0),
            in_=in_sb, in_offset=None), r, w, dma=True)

    def gather(self, out_sb, in_dram, idx, r=(), w=()):
        self.S.add("pool", lambda e: e.indirect_dma_start(
            out=out_sb, out_offset=None, in_=in_dram,
            in_offset=bass.IndirectOffsetOnAxis(ap=idx, axis=0),
            ), r, w, dma=True)


def host_consts():
    c = {}
    c["ident_bf"] = np.eye(128, dtype=np.float32).astype(ml_dtypes.bfloat16)
    c["ident_f"] = np.eye(128, dtype=np.float32)
    tri = (np.arange(128)[:, None] <= np.arange(128)[None, :]).astype(np.float32)
    c["tri_bf"] = tri.astype(ml_dtypes.bfloat16)
    c["tri_f"] = tri
    c["tris_bf"] = (np.arange(128)[:, None] < np.arange(128)[None, :]).astype(np.float32).astype(ml_dtypes.bfloat16)
    c["ones_bf"] = np.ones((128, 128), dtype=ml_dtypes.bfloat16)
    c["ones_f"] = np.ones((128, 128), dtype=np.float32)
    c["ebase"] = np.broadcast_to((NROWS - np.arange(NE) * CAP).astype(np.float32)[None, :], (128, NE)).copy()
    return c


CONST_SPECS = [("ident_bf", BF16, [128, 128]), ("ident_f", F32, [128, 128]), ("tri_bf", BF16, [128, 128]),
               ("tri_f", F32, [128, 128]), ("tris_bf", BF16, [128, 128]), ("ones_bf", BF16, [128, 128]),
               ("ones_f", F32, [128, 128]), ("ebase", F32, [128, NE])]

IN_SPECS = [("x", [T, D]), ("w_in", [DEPTH, D, IN_W]), ("conv_w", [DEPTH, 4, 512]), ("conv_b", [DEPTH, 512]),
            ("gate_b", [DEPTH, 8]), ("lam_q1", [DEPTH, 64]), ("lam_k1", [DEPTH, 64]), ("lam_q2", [DEPTH, 64]),
            ("lam_k2", [DEPTH, 64]), ("da_norm_g", [DEPTH, 128]), ("w_out", [DEPTH, D, D]),
            ("ln1_g", [DEPTH, D]), ("ln1_b", [DEPTH, D]), ("w_router", [DEPTH, D, NE]), ("b_router", [DEPTH, NE]),
            ("w_gu", [DEPTH, NE, D, 2048]), ("b_gu", [DEPTH, NE, 2048]), ("w_down", [DEPTH, NE, D, D]),
            ("b_down", [DEPTH, NE, D]), ("ln2_g", [DEPTH, D]), ("ln2_b", [DEPTH, D])]


def build(nlayers=DEPTH, phases=("da", "ml", "op", "ex", "cb"), debug=()):
    nc = bass.Bass("TRN2", target_bir_lowering=False)
    I = {}
    for name, shp in IN_SPECS:
        I[name] = nc.dram_tensor(name, shp, F32, kind="ExternalInput").ap()
    for name, dt, shp in CONST_SPECS:
        I[name] = nc.dram_tensor(name, shp, dt, kind="ExternalInput").ap()
    y_out = nc.dram_tensor("y", [T, D], F32, kind="ExternalOutput").ap()

    def scratch(name, shp, dt):
        kind = "ExternalOutput" if name in debug else "Internal"
        return nc.dram_tensor(name, shp, dt, kind=kind).ap()

    mixT_d = scratch("mixT_d", [D, T], BF16)
    x1_d = scratch("x1_d", [T, D], F32)
    xa_d = scratch("xa_d", [T, D], F32)
    xb_d = scratch("xb_d", [T, D], F32)
    xs_d = scratch("xs_d", [NROWS + 1, D], BF16)
    ys_d = scratch("ys_d", [NROWS + 1, D], F32)

    b = B(nc)
    S = b.S
    _cnt = [0]

    def uniq(name):
        _cnt[0] += 1
        return "sb%d_%s" % (_cnt[0], name)
    st = contextlib.ExitStack()
    with st:
        S.semstack = st
        def sb(name, shp, dt):
            return st.enter_context(nc.sbuf_tensor(uniq(name), shp, dt))

        ident_bf = sb("ident_bf", [128, 128], BF16)
        ident_f = sb("ident_f", [128, 128], F32)
        tri_bf = sb("tri_bf", [128, 128], BF16)
        tri_f = sb("tri_f", [128, 128], F32)
        tris_bf = sb("tris_bf", [128, 128], BF16)
        ones_bf = sb("ones_bf", [128, 128], BF16)
        ones_f = sb("ones_f", [128, 128], F32)
        ebase = sb("ebase", [128, NE], F32)
        for name, t_ in (("ident_bf", ident_bf), ("ident_f", ident_f), ("tri_bf", tri_bf), ("tri_f", tri_f),
                         ("tris_bf", tris_bf), ("ones_bf", ones_bf), ("ones_f", ones_f), ("ebase", ebase)):
            b.dma("sp", t_[:], I[name], w=["const"])
        idx_all = sb("idx_all", [128, NT, 4], I32)
        gate_all = sb("gate_all", [128, NT, 4], F32)
        with nc.sbuf_tensor("sb_zrow", [1, D], F32) as zrow:
            b.memset("dve", zrow[:], 0.0, w=["zrow"])
            b.dma("sp", ys_d[NROWS:NROWS + 1, :], zrow[:], r=["zrow"])

        ps_big = [st.enter_context(nc.psum_tensor("psb%d" % i, [128, 1024], F32)) for i in range(4)]
        ps = [ps_big[i // 2][:, (i % 2) * 512:(i % 2 + 1) * 512] for i in range(8)]

        def PS(i):
            return ("ps", i)

        def stream_xT(xsrc, blk, xstage, xT_blk, tag, nbuf=2):
            buf = blk % nbuf
            for j in range(4):
                tt = blk * 4 + j
                sbuf_i = tt % 2
                b.dma("pool", xstage[:, sbuf_i, :], xsrc[tt * 128:(tt + 1) * 128, :],
                      w=[("xstage", sbuf_i)])
                pst = ps[7][:].bitcast(BF16)
                for kc in range(KC):
                    b.tr(pst[:, kc * 128:(kc + 1) * 128], xstage[:, sbuf_i, kc * 128:(kc + 1) * 128], ident_bf[:],
                         r=[("xstage", sbuf_i), "const"], w=[PS(7)])
                b.evac(xT_blk[:, buf, :, j * 128:(j + 1) * 128],
                       pst.rearrange("p (k t) -> p k t", k=KC), r=[PS(7)], w=[(tag, buf)])

        def layer_norm(xt, g_bc, b_bc, out, keyin, keyout, tmpstat, tmpmv, tmpr, aff="pool"):
            b.S.add("dve", lambda e: e.bn_stats(tmpstat[:, 0, :], xt[:, 0:512]), [keyin], ["lnstat"])
            b.S.add("dve", lambda e: e.bn_stats(tmpstat[:, 1, :], xt[:, 512:1024]), [keyin], ["lnstat"])
            b.S.add("dve", lambda e: e.bn_aggr(tmpmv[:], tmpstat[:].rearrange("p a b -> p (a b)")), ["lnstat"], ["lnmv"])
            b.act(tmpr[:, 0:1], tmpmv[:, 1:2], AF.Ln, bias=epsc[:, 0:1], scale=1.0, r=["lnmv", "epsc"], w=["lnr0"])
            b.act(tmpr[:, 1:2], tmpr[:, 0:1], AF.Exp, scale=-0.5, r=["lnr0"], w=["lnr1"])
            b.ts("dve", xt, xt, tmpmv[:, 0:1], tmpr[:, 1:2], ALU.subtract, ALU.mult, r=[keyin, "lnmv", "lnr1"], w=[keyin])
            b.tt(aff, xt, xt, g_bc, ALU.mult, r=[keyin, "lnp"], w=[keyin])
            b.tt(aff, out, xt, b_bc, ALU.add, r=[keyin, "lnp"], w=[keyout])

        epsc = sb("epsc", [128, 2], F32)
        b.memset("dve", epsc[:, 0:1], LN_EPS, w=["epsc"])
        b.memset("dve", epsc[:, 1:2], 128.0 * RMS_EPS, w=["epsc"])

        xcur = I["x"]
        for l in range(nlayers):
            lam_init = 0.8 - 0.6 * math.exp(-0.3 * l)
            xnext = y_out if l == nlayers - 1 else (xa_d if l % 2 == 0 else xb_d)
            if "da" in phases:
                with contextlib.ExitStack() as ph:
                    def pb(name, shp, dt):
                        return ph.enter_context(nc.sbuf_tensor(uniq(name), shp, dt))
                    w_da = pb("w_da", [128, KC, 1536], BF16)
                    for kc in range(KC):
                        b.dma("pool", w_da[:, kc, :], I["w_in"][l, kc * 128:(kc + 1) * 128, 0:1536], w=["w_da"])
                    xstage = pb("xstage", [128, 2, D], BF16)
                    xT_blk = pb("xT_blk", [128, 2, KC, 512], BF16)
                    qT = pb("qT", [128, 4, T], BF16)
                    kT = pb("kT", [128, 4, T], BF16)
                    Vd = pb("Vd", [128, NT, 512], BF16)
                    lamt = pb("lamt", [128, 4, 64], F32)
                    lamv = pb("lamv", [128, 8], F32)
                    gcol = pb("gcol", [128, 2], F32)
                    for i, nm in enumerate(("lam_q1", "lam_k1", "lam_q2", "lam_k2")):
                        b.dma("sp", lamt[:, i, :], I[nm][l:l + 1, :].partition_broadcast(128), w=["lamt"])
                    b.dma("sp", gcol[:, 0:1], I["da_norm_g"][l:l + 1, :].rearrange("o p -> p o"), w=["gcol"], nc_ok=True)
                    b.tt("dve", lamt[:, 0, :], lamt[:, 0, :], lamt[:, 1, :], ALU.mult, r=["lamt"], w=["lamt"])
                    b.tt("dve", lamt[:, 2, :], lamt[:, 2, :], lamt[:, 3, :], ALU.mult, r=["lamt"], w=["lamt"])
                    b.S.add("dve", lambda e: e.reduce_sum(lamv[:, 0:1], lamt[:, 0, :], AX.X), ["lamt"], ["lamv"])
                    b.S.add("dve", lambda e: e.reduce_sum(lamv[:, 1:2], lamt[:, 2, :], AX.X), ["lamt"], ["lamv"])
                    b.act(lamv[:, 2:4], lamv[:, 0:2], AF.Exp, r=["lamv"], w=["lamv2"])
                    b.tt("dve", lamv[:, 4:5], lamv[:, 3:4], lamv[:, 2:3], ALU.subtract, r=["lamv2"], w=["lamv3"])
                    b.ts("dve", lamv[:, 5:6], lamv[:, 4:5], -lam_init, None, ALU.add, r=["lamv3"], w=["neglam"])
                    neglam = lamv[:, 5:6]
                    b.ts("dve", gcol[:, 1:2], gcol[:, 0:1], (1.0 - lam_init) * math.sqrt(128.0), None, ALU.mult,
                         r=["gcol"], w=["gcol2"])
                    for blk in range(8):
                        stream_xT(xcur, blk, xstage, xT_blk, "xT")
                        buf = blk % 2
                        for grp in range(8):
                            pa = ps[grp % 2]
                            for kc in range(KC):
                                b.mm(pa[:], w_da[:, kc, grp * 128:(grp + 1) * 128], xT_blk[:, buf, kc, :],
                                     start=(kc == 0), stop=(kc == KC - 1), r=["w_da", ("xT", buf)], w=[PS(grp % 2)])
                            dst = qT if grp < 4 else kT
                            b.evac(dst[:, grp % 4, blk * 512:(blk + 1) * 512], pa[:], r=[PS(grp % 2)],
                                   w=[("q" if grp < 4 else "k", blk)])
                        for j in range(4):
                            tt = blk * 4 + j
                            pa = ps[j % 2]
                            for kc in range(KC):
                                b.mm(pa[:], xT_blk[:, buf, kc, j * 128:(j + 1) * 128], w_da[:, kc, 1024:1536],
                                     start=(kc == 0), stop=(kc == KC - 1), r=["w_da", ("xT", buf)], w=[PS(j % 2)])
                            b.evac(Vd[:, tt, :], pa[:], r=[PS(j % 2)], w=[("v", tt)])
                    Pt = pb("Pt", [128, 2, 3, 512], BF16)
                    acc = pb("acc", [128, 2, 2, 512], F32)
                    Osb = pb("Osb", [128, 2, 512], F32)
                    Lsb = pb("Lsb", [128, 2, 512], F32)
                    Rc = pb("Rc", [128, 2, 512], F32)
                    oc = pb("oc", [128, 2, 512], F32)
                    sq = pb("sq", [128, 2, 512], F32)
                    rs = pb("rs", [128, 512], F32)
                    ob = pb("ob", [128, 2, 512], BF16)
                    items = []
                    for Qb in range(8):
                        for h in range(4):
                            nk = (Qb + 1) * 4
                            for kt in range(nk):
                                items.append((Qb, h, kt, nk))
                    ACC_ENG = ("pool", "dve")

                    def emit_S(i):
                        Qb, h, kt, nk = items[i]
                        diag = kt >= Qb * 4
                        q0 = (kt - Qb * 4) * 128 if diag else 0
                        n = 512 - q0
                        for c in range(2):
                            lo = c * 64
                            sbank = 2 * (i % 2) + c
                            b.mm(ps[sbank][:, 0:n], kT[lo:lo + 64, h, kt * 128:(kt + 1) * 128],
                                 qT[lo:lo + 64, h, Qb * 512 + q0:(Qb + 1) * 512],
                                 r=[("k", kt // 4), ("q", Qb)], w=[PS(sbank)])

                    def epi_a(Qb, h):
                        gp = (Qb * 4 + h) % 2
                        for c in range(2):
                            b.cp("act", Osb[:, c, :], ps[4 + c][:], r=[PS(4 + c)], w=[("Osb", c)])
                            b.mm(ps[6 + c][:], ones_f[:], acc[:, c, gp, :], r=["const", ("acc", c, gp)], w=[PS(6 + c)])
                            b.cp("act", Lsb[:, c, :], ps[6 + c][:], r=[PS(6 + c)], w=[("Lsb", c)])
                            b.S.add("dve", lambda e, c=c: e.reciprocal(Lsb[:, c, :], Lsb[:, c, :]), [("Lsb", c)], [("Lsb", c)])
                            b.tt("dve", Rc[:, c, :], Osb[:, c, :], Lsb[:, c, :], ALU.mult, r=[("Osb", c), ("Lsb", c)], w=[("Rc", c)])
                        b.stt(oc[:, gp, :], Rc[:, 1, :], neglam, Rc[:, 0, :], ALU.mult, ALU.add,
                              r=[("Rc", 0), ("Rc", 1), "neglam"], w=[("oc", gp)])
                        b.act(sq[:, gp, :], oc[:, gp, :], AF.Square, r=[("oc", gp)], w=[("sq", gp)])

                    def epi_b(Qb, h):
                        gp = (Qb * 4 + h) % 2
                        b.mm(ps[6][:], ones_f[:], sq[:, gp, :], r=[("sq", gp), "const"], w=[PS(6)])
                        b.act(rs[:], ps[6][:], AF.Sqrt, bias=epsc[:, 1:2], r=[PS(6), "epsc"], w=["rs"])
                        b.S.add("dve", lambda e: e.reciprocal(rs[:], rs[:]), ["rs"], ["rs"])
                        b.stt(ob[:, gp, :], oc[:, gp, :], gcol[:, 1:2], rs[:], ALU.mult, ALU.mult,
                              r=[("oc", gp), "rs", "gcol2"], w=[("ob", gp)])
                        b.dma("sp", mixT_d[h * 128:(h + 1) * 128, Qb * 512:(Qb + 1) * 512], ob[:, gp, :],
                              r=[("ob", gp)])

                    pending = None
                    emit_S(0)
                    for i, (Qb, h, kt, nk) in enumerate(items):
                        if i + 1 < len(items):
                            emit_S(i + 1)
                        gp = (Qb * 4 + h) % 2
                        diag = kt >= Qb * 4
                        q0 = (kt - Qb * 4) * 128 if diag else 0
                        n = 512 - q0
                        pbuf = i % 3
                        if kt == 0:
                            b.memset("pool", acc[:, 0, gp, :], 0.0, w=[("acc", 0, gp)])
                            b.memset("dve", acc[:, 1, gp, :], 0.0, w=[("acc", 1, gp)])
                        for c in range(2):
                            sbank = 2 * (i % 2) + c
                            b.act(Pt[:, c, pbuf, 0:n], ps[sbank][:, 0:n], AF.Exp, scale=0.125,
                                  r=[PS(sbank)], w=[("Pt", c, pbuf)])
                            if diag:
                                b.tt("pool", Pt[:, c, pbuf, 0:128], Pt[:, c, pbuf, 0:128], tri_bf[:], ALU.mult,
                                     r=[("Pt", c, pbuf), "const"], w=[("Pt", c, pbuf)])
                            b.mm(ps[4 + c][:, q0:512], Vd[:, kt, h * 128:(h + 1) * 128], Pt[:, c, pbuf, 0:n],
                                 start=(kt == 0), stop=(kt == nk - 1), r=[("v", kt), ("Pt", c, pbuf)], w=[PS(4 + c)])
                            b.tt(ACC_ENG[c], acc[:, c, gp, q0:512], acc[:, c, gp, q0:512], Pt[:, c, pbuf, 0:n], ALU.add,
                                 r=[("acc", c, gp), ("Pt", c, pbuf)], w=[("acc", c, gp)])
                        if kt == min(7, nk - 2) and pending is not None:
                            epi_b(*pending)
                            pending = None
                        if kt == nk - 1:
                            if pending is not None:
                                epi_b(*pending)
                            epi_a(Qb, h)
                            pending = (Qb, h)
                    if pending is not None:
                        epi_b(*pending)
                    S.end_phase()

            if "ml" in phases:
                with contextlib.ExitStack() as ph:
                    def pb(name, shp, dt):
                        return ph.enter_context(nc.sbuf_tensor(uniq(name), shp, dt))
                    mq = pb("mq", [64, 4, T], BF16)
                    mk = pb("mk", [64, 4, T], BF16)
                    mv = pb("mv", [128, NT, 512], BF16)
                    mo = pb("mo", [128, 4, T], BF16)
                    gtm = pb("gtm", [128, NT, 8], F32)
                    lf = pb("lf", [128, NT, 4], F32)
                    btm = pb("btm", [128, NT, 4], F32)
                    gbc = pb("gbc", [128, NT, 4], F32)
                    ew = pb("ew", [128, NT, 4], F32)
                    ebias = pb("ebias", [128, NT, 4], F32)
                    eg = pb("eg", [128, NT, 4], F32)
                    nl8 = pb("nl8", [128, 2], F32)
                    b.memset("dve", nl8[:, 0:1], -math.log(8.0), w=["nl8"])
                    b.memset("dve", nl8[:, 1:2], 1.0, w=["nl8"])
                    with contextlib.ExitStack() as ph2:
                        def pb2(name, shp, dt):
                            return ph2.enter_context(nc.sbuf_tensor(uniq(name), shp, dt))
                        w_ml = pb2("w_ml", [128, KC, 1544], BF16)
                        for kc in range(KC):
                            b.dma("pool", w_ml[:, kc, :], I["w_in"][l, kc * 128:(kc + 1) * 128, 1536:3080], w=["w_ml"])
                        xstage = pb2("xstage", [128, 2, D], BF16)
                        xT_blk = pb2("xT_blk", [128, 1, KC, 512], BF16)
                        gb_bc = pb2("gb_bc", [128, 8], F32)
                        b.dma("sp", gb_bc[:], I["gate_b"][l:l + 1, :].partition_broadcast(128), w=["gb_bc"])
                        cw = pb2("cw", [64, 4, 8], F32)
                        cb = pb2("cb", [64, 8], F32)
                        for j in range(4):
                            b.dma("sp", cw[:, j, :], I["conv_w"][l, j:j + 1, :].rearrange("o (g p) -> p (o g)", p=64), w=["cw"], nc_ok=True)
                        b.dma("sp", cb[:], I["conv_b"][l:l + 1, :].rearrange("o (g p) -> p (o g)", p=64), w=["cw"], nc_ok=True)
                        cin = pb2("cin", [64, 8, 515], F32)
                        ctmp = pb2("ctmp", [64, 2, 512], F32)
                        b.memset("dve", cin[:, :, 0:3], 0.0, w=[("cin", g) for g in range(8)])
                        for blk in range(8):
                            stream_xT(xcur, blk, xstage, xT_blk, "xT", nbuf=1)
                            buf = 0
                            for grp in range(8):
                                pa = ps[grp % 2]
                                for kc in range(KC):
                                    b.mm(pa[0:64, :], w_ml[:, kc, grp * 64:(grp + 1) * 64], xT_blk[:, buf, kc, :],
                                         start=(kc == 0), stop=(kc == KC - 1), r=["w_ml", ("xT", buf)], w=[PS(grp % 2)])
                                b.cp("act", cin[:, grp, 3:515], pa[0:64, :], r=[PS(grp % 2)], w=[("cin", grp)])
                                ct = ctmp[:, grp % 2, :]
                                b.ts("dve", ct, cin[:, grp, 0:512], cw[:, 0, grp:grp + 1], cb[:, grp:grp + 1], ALU.mult, ALU.add,
                                     r=[("cin", grp), "cw"], w=[("ctmp", grp % 2)])
                                for j in range(1, 4):
                                    b.stt(ct, cin[:, grp, j:j + 512], cw[:, j, grp:grp + 1], ct, ALU.mult, ALU.add,
                                          r=[("cin", grp), "cw", ("ctmp", grp % 2)], w=[("ctmp", grp % 2)])
                                dst = mq if grp < 4 else mk
                                b.act(dst[:, grp % 4, blk * 512:(blk + 1) * 512], ct, AF.Silu,
                                      r=[("ctmp", grp % 2)], w=[("mqk", blk)])
                                b.cp("pool", cin[:, grp, 0:3], cin[:, grp, 512:515], r=[("cin", grp)], w=[("cin", grp)])
                            for h in range(4):
                                pa = ps[2 + h % 2]
                                for kc in range(KC):
                                    b.mm(pa[:], w_ml[:, kc, 1024 + h * 128:1024 + (h + 1) * 128], xT_blk[:, buf, kc, :],
                                         start=(kc == 0), stop=(kc == KC - 1), r=["w_ml", ("xT", buf)], w=[PS(2 + h % 2)])
                                b.act(mo[:, h, blk * 512:(blk + 1) * 512], pa[:], AF.Sigmoid, r=[PS(2 + h % 2)], w=[("mo", blk)])
                            for j in range(4):
                                tt = blk * 4 + j
                                pa = ps[4 + j % 2]
                                for kc in range(KC):
                                    b.mm(pa[:], xT_blk[:, buf, kc, j * 128:(j + 1) * 128], w_ml[:, kc, 512:1024],
                                         start=(kc == 0), stop=(kc == KC - 1), r=["w_ml", ("xT", buf)], w=[PS(4 + j % 2)])
                                b.evac(mv[:, tt, :], pa[:], r=[PS(4 + j % 2)], w=[("mv", tt)])
                                for kc in range(KC):
                                    b.mm(ps[6][:, 0:8], xT_blk[:, buf, kc, j * 128:(j + 1) * 128], w_ml[:, kc, 1536:1544],
                                         start=(kc == 0), stop=(kc == KC - 1), r=["w_ml", ("xT", buf)], w=[PS(6)])
                                b.tt("dve", gtm[:, tt, :], ps[6][:, 0:8], gb_bc[:], ALU.add, r=[PS(6), "gb_bc"], w=["gtm"])
                    S.barrier()
                    b.act(lf[:], gtm[:, :, 4:8], AF.Exp, scale=-1.0, r=["gtm"], w=["lf"])
                    b.act(lf[:], lf[:], AF.Ln, bias=nl8[:, 1:2], r=["lf", "nl8"], w=["lf"])
                    b.ts("dve", lf[:], lf[:], -1.0, None, ALU.mult, r=["lf"], w=["lf"])
                    lf2 = lf[:].rearrange("p a b -> p (a b)")
                    b.mm(ps[6][:, 0:128], tri_f[:], lf2, r=["lf", "const"], w=[PS(6)])
                    b.cp("dve", btm[:].rearrange("p a b -> p (a b)"), ps[6][:, 0:128], r=[PS(6)], w=["btm"])
                    b.mm(ps[6][:, 128:256], ones_f[:], lf2, r=["lf", "const"], w=[PS(6)])
                    b.cp("dve", gbc[:].rearrange("p a b -> p (a b)"), ps[6][:, 128:256], r=[PS(6)], w=["gbc"])
                    b.tt("dve", ebias[:], gtm[:, :, 0:4], btm[:], ALU.subtract, r=["gtm", "btm"], w=["ebias"])
                    b.tt("dve", ew[:], ebias[:], gbc[:], ALU.add, r=["ebias", "gbc"], w=["ew"])
                    b.act(ew[:], ew[:], AF.Exp, r=["ew"], w=["ew"])
                    b.ts("dve", ebias[:], ebias[:], -math.log(8.0), None, ALU.add, r=["ebias", "ew"], w=["ebias2"])
                    b.act(eg[:], gbc[:], AF.Exp, r=["gbc"], w=["eg"])
                    Cst = pb("Cst", [64, 4, 256], F32)
                    Cbf = pb("Cbf", [64, 4, 256], BF16)
                    b.memset("dve", Cst[:], 0.0, w=[("C", h) for h in range(4)])
                    b.memset("pool", Cbf[:], 0.0, w=[("Cbf", h) for h in range(4)])
                    lfd = pb("lfd", [128, 2, 4, 128], F32)
                    Et = pb("Et", [128, 2, 4, 128], F32)
                    Eq = pb("Eq", [64, 2, 4, 128], F32)
                    qs = pb("qs", [64, 2, 4, 128], BF16)
                    STt = pb("STt", [128, 2, 4, 128], BF16)
                    kh = pb("kh", [128, 2, 4, 64], BF16)
                    dm = pb("dm", [128, 2, 4, 128], F32)
                    hT = pb("hT", [128, 2, 4, 128], F32)
                    hst = pb("hst", [128, 2, 4, 512], BF16)
                    H4 = range(4)

                    def front(tt):
                        c0 = tt * 128
                        u = tt % 2
                        blk = tt // 4
                        sb_ = 1 if u == 0 else 7
                        for h in H4:
                            b.ts("dve", lfd[:, u, h, :], ones_f[:], lf[:, tt, h:h + 1], None, ALU.mult,
                                 r=["lf", "const"], w=[("lfd", u, h)])
                        for h in H4:
                            b.mm(ps[0][:, h * 128:(h + 1) * 128], lfd[:, u, h, :], tri_f[:], r=[("lfd", u, h), "const"], w=[PS(0)])
                        for h in H4:
                            b.mm(ps[sb_][:, h * 128:(h + 1) * 128], mk[:, h, c0:c0 + 128], mq[:, h, c0:c0 + 128],
                                 r=[("mqk", blk)], w=[PS(sb_)])
                        pkt = ps[4][:].bitcast(BF16)
                        for h in H4:
                            b.tr(pkt[:, h * 64:(h + 1) * 64], mk[:, h, c0:c0 + 128], ident_bf[0:64, 0:64],
                                 r=[("mqk", blk), "const"], w=[PS(4)])
                        for h in H4:
                            b.act(Et[:, u, h, :], ps[0][:, h * 128:(h + 1) * 128], AF.Exp, bias=ebias[:, tt, h:h + 1],
                                  r=[PS(0), "ebias2"], w=[("Et", u, h)])
                        b.act(Eq[:, u, :, :], ps[0][0:64, :].rearrange("p (h t) -> p h t", h=4), AF.Exp, bias=nl8[0:64, 0:1],
                              r=[PS(0), "nl8"], w=[("Eq", u)])
                        for h in H4:
                            b.tt("pool", Et[:, u, h, :], Et[:, u, h, :], tri_f[:], ALU.mult, r=[("Et", u, h), "const"], w=[("Et", u, h)])

                    def front_b(tt):
                        c0 = tt * 128
                        u = tt % 2
                        blk = tt // 4
                        pkt = ps[4][:].bitcast(BF16)
                        b.tt("dve", qs[:, u, :, :], mq[:, :, c0:c0 + 128], Eq[:, u, :, :], ALU.mult,
                             r=[("mqk", blk), ("Eq", u)], w=[("qs", u)])
                        for h in H4:
                            b.ts("dve", kh[:, u, h, :], pkt[:, h * 64:(h + 1) * 64], ew[:, tt, h:h + 1], None, ALU.mult,
                                 r=[PS(4), "ew"], w=[("kh", u, h)])

                    def back(tt):
                        c0 = tt * 128
                        u = tt % 2
                        blk = tt // 4
                        sb_ = 1 if u == 0 else 7
                        b.tt("dve", STt[:, u, :, :], ps[sb_][:].rearrange("p (h t) -> p h t", h=4), Et[:, u, :, :], ALU.mult,
                             r=[PS(sb_)] + [("Et", u, h) for h in H4], w=[("ST", u)])
                        for h in H4:
                            b.mm(ps[2][:, h * 128:(h + 1) * 128], mv[:, tt, h * 128:(h + 1) * 128], STt[:, u, h, :],
                                 start=True, stop=False, r=[("mv", tt), ("ST", u)], w=[PS(2)])
                            b.mm(ps[2][:, h * 128:(h + 1) * 128], Cbf[:, h, 0:128], qs[:, u, h, :],
                                 start=False, stop=True, r=[("Cbf", h), ("qs", u)], w=[PS(2)])
                        for h in H4:
                            b.mm(ps[3][:, h * 128:(h + 1) * 128], ones_bf[:], STt[:, u, h, :],
                                 start=True, stop=False, r=["const", ("ST", u)], w=[PS(3)])
                            b.mm(ps[3][:, h * 128:(h + 1) * 128], Cbf[:, h, 128:256], qs[:, u, h, :],
                                 start=False, stop=True, r=[("Cbf", h), ("qs", u)], w=[PS(3)])
                        for h in H4:
                            pu = ps[5 + h // 2]
                            o0 = (h % 2) * 256
                            b.mm(pu[0:64, o0:o0 + 128], kh[:, u, h, :], mv[:, tt, h * 128:(h + 1) * 128],
                                 r=[("kh", u, h), ("mv", tt)], w=[PS(5 + h // 2)])
                            b.mm(pu[0:64, o0 + 128:o0 + 256], kh[:, u, h, :], ones_bf[:], r=[("kh", u, h), "const"],
                                 w=[PS(5 + h // 2)])

                    def back_2(tt):
                        c0 = tt * 128
                        u = tt % 2
                        blk = tt // 4
                        b.act(dm[:, u, :, :], ps[3][:].rearrange("p (h t) -> p h t", h=4), AF.Abs,
                              r=[PS(3)], w=[("dm", u)])
                        b.ts("dve", dm[:, u, :, :], dm[:, u, :, :], 1.0, None, ALU.max, r=[("dm", u)], w=[("dm", u)])
                        b.S.add("dve", lambda e, u=u: e.reciprocal(dm[:, u, :, :], dm[:, u, :, :]), [("dm", u)], [("dm", u)])
                        b.tt("dve", hT[:, u, :, :], ps[2][:].rearrange("p (h t) -> p h t", h=4), dm[:, u, :, :], ALU.mult,
                             r=[PS(2), ("dm", u)], w=[("hT", u)])
                        hp = blk % 2
                        b.tt("dve", hst[:, hp, :, (tt % 4) * 128:(tt % 4 + 1) * 128], hT[:, u, :, :],
                             mo[:, :, c0:c0 + 128], ALU.mult, r=[("hT", u), ("mo", blk)], w=[("hst", hp)])
                        if tt % 4 == 3:
                            for h in H4:
                                b.dma("sp", mixT_d[512 + h * 128:512 + (h + 1) * 128, blk * 512:(blk + 1) * 512],
                                      hst[:, hp, h, :], r=[("hst", hp)])
                        for h in H4:
                            pu = ps[5 + h // 2]
                            o0 = (h % 2) * 256
                            b.stt(Cst[:, h, :], Cst[:, h, :], eg[0:64, tt, h:h + 1], pu[0:64, o0:o0 + 256], ALU.mult, ALU.add,
                                  r=[("C", h), "eg", PS(5 + h // 2)], w=[("C", h)])
                        b.cp("act", Cbf[:], Cst[:], r=[("C", h) for h in H4], w=[("Cbf", h) for h in H4])

                    front(0)
                    front_b(0)
                    for tt in range(NT):
                        if tt + 1 < NT:
                            front(tt + 1)
                        back(tt)
                        if tt + 1 < NT:
                            front_b(tt + 1)
                        back_2(tt)
                    S.end_phase()

            if "op" in phases:
                with contextlib.ExitStack() as ph:
                    def pb(name, shp, dt):
                        return ph.enter_context(nc.sbuf_tensor(uniq(name), shp, dt))
                    w_o = pb("w_o", [128, KC, D], BF16)
                    for kc in range(KC):
                        b.dma("pool", w_o[:, kc, :], I["w_out"][l, kc * 128:(kc + 1) * 128, :], w=["w_o"])
                    w_r = pb("w_r", [128, KC, NE], F32)
                    b.dma("sp", w_r[:], I["w_router"][l].rearrange("(k p) e -> p k e", p=128), w=["w_r"])
                    br_bc = pb("br_bc", [128, NE], F32)
                    b.dma("sp", br_bc[:], I["b_router"][l:l + 1, :].partition_broadcast(128), w=["w_r"])
                    g_bc = pb("g_bc", [128, D], F32)
                    b_bc = pb("b_bc", [128, D], F32)
                    b.dma("sp", g_bc[:], I["ln1_g"][l:l + 1, :].partition_broadcast(128), w=["lnp"])
                    b.dma("sp", b_bc[:], I["ln1_b"][l:l + 1, :].partition_broadcast(128), w=["lnp"])
                    mixb = pb("mixb", [128, 2, KC, 512], BF16)
                    xt = pb("xt", [128, 2, D], F32)
                    x1t = pb("x1t", [128, 2, D], F32)
                    x1b = pb("x1b", [128, 2, D], BF16)
                    x1T = pb("x1T", [128, KC, 128], F32)
                    tstat = pb("tstat", [128, 2, 6], F32)
                    tmv = pb("tmv", [128, 2], F32)
                    tr_ = pb("tr_", [128, 2], F32)
                    lg = pb("lg", [128, NE], F32)
                    m8 = pb("m8", [128, 8], F32)
                    msk = pb("msk", [128, NE], F32)
                    mskb = pb("mskb", [128, NE], BF16)
                    ex = pb("ex", [128, NE], F32)
                    G = pb("G", [128, NE], F32)
                    gs = pb("gs", [128, 4], F32)
                    cnt = pb("cnt", [128, NE], F32)
                    pos = pb("pos", [128, NE], F32)
                    okm = pb("okm", [128, NE], F32)
                    val = pb("val", [128, NE], F32)
                    v8 = pb("v8", [128, 8], F32)
                    idf = pb("idf", [128, 4], F32)
                    junk = pb("junk", [128, NE], F32)
                    b.memset("dve", cnt[:], 0.0, w=["cnt"])
                    for blk in range(8):
                        mb = blk % 2
                        b.dma("sp", mixb[:, mb, :, :], mixT_d[:, blk * 512:(blk + 1) * 512].rearrange("(k p) t -> p k t", p=128),
                              w=[("mixb", mb)])
                        for j in range(4):
                            tt = blk * 4 + j
                            u = tt % 2
                            b.dma("sp", xt[:, u, :], xcur[tt * 128:(tt + 1) * 128, :], w=[("xt", u)])
                            for half in range(2):
                                pa = ps[half]
                                for kc in range(KC):
                                    b.mm(pa[:], mixb[:, mb, kc, j * 128:(j + 1) * 128], w_o[:, kc, half * 512:(half + 1) * 512],
                                         start=(kc == 0), stop=(kc == KC - 1), r=[("mixb", mb), "w_o"], w=[PS(half)])
                                b.stt(xt[:, u, half * 512:(half + 1) * 512], xt[:, u, half * 512:(half + 1) * 512], DN_ALPHA,
                                      pa[:], ALU.mult, ALU.add, r=[("xt", u), PS(half)], w=[("xt", u)])
                            layer_norm(xt[:, u, :], g_bc[:], b_bc[:], x1t[:, u, :], ("xt", u), ("x1t", u), tstat, tmv, tr_)
                            b.dma("sp", x1_d[tt * 128:(tt + 1) * 128, :], x1t[:, u, :], r=[("x1t", u)])
                            b.cp("act", x1b[:, u, :], x1t[:, u, :], r=[("x1t", u)], w=[("x1b", u)])
                            for kc in range(KC):
                                b.tr(ps[2 + kc // 4][:, (kc % 4) * 128:(kc % 4 + 1) * 128], x1t[:, u, kc * 128:(kc + 1) * 128],
                                     ident_f[:], r=[("x1t", u), "const"], w=[PS(2 + kc // 4)])
                            b.cp("act", x1T[:, 0:4, :], ps[2][:].rearrange("p (k t) -> p k t", k=4), r=[PS(2)], w=["x1T"])
                            b.cp("dve", x1T[:, 4:8, :], ps[3][:].rearrange("p (k t) -> p k t", k=4), r=[PS(3)], w=["x1T"])
                            for kc in range(KC):
                                b.mm(ps[4][:, 0:NE], x1T[:, kc, :], w_r[:, kc, :], start=(kc == 0), stop=(kc == KC - 1),
                                     r=["x1T", "w_r"], w=[PS(4)])
                            b.tt("dve", lg[:], ps[4][:, 0:NE], br_bc[:], ALU.add, r=[PS(4), "w_r"], w=["lg"])
                            b.S.add("dve", lambda e: e.max(m8[:], lg[:]), ["lg"], ["m8"])
                            b.ts("dve", msk[:], lg[:], m8[:, 3:4], None, ALU.is_ge, r=["lg", "m8"], w=["msk"])
                            b.cp("pool", mskb[:], msk[:], r=["msk"], w=["mskb"])
                            b.ts("dve", ex[:], lg[:], m8[:, 0:1], None, ALU.subtract, r=["lg", "m8"], w=["ex"])
                            b.act(ex[:], ex[:], AF.Exp, r=["ex"], w=["ex"])
                            b.stt(G[:], ex[:], 1.0, msk[:], ALU.mult, ALU.mult, r=["ex", "msk"], w=["G"], accum_out=gs[:, 0:1])
                            b.S.add("dve", lambda e: e.reciprocal(gs[:, 1:2], gs[:, 0:1]), ["G"], ["gs"])
                            b.ts("dve", G[:], G[:], gs[:, 1:2], None, ALU.mult, r=["G", "gs"], w=["G"])
                            b.mm(ps[5][:, 0:NE], tris_bf[:], mskb[:], r=["mskb", "const"], w=[PS(5)])
                            b.mm(ps[5][:, NE:2 * NE], ones_bf[:], mskb[:], r=["mskb", "const"], w=[PS(5)])
                            b.tt("dve", pos[:], ps[5][:, 0:NE], cnt[:], ALU.add, r=[PS(5), "cnt"], w=["pos"])
                            b.tt("dve", cnt[:], ps[5][:, NE:2 * NE], cnt[:], ALU.add, r=[PS(5), "cnt", "pos"], w=["cnt"])
                            b.ts("dve", okm[:], pos[:], float(CAP), None, ALU.is_lt, r=["pos"], w=["okm"])
                            b.tt("dve", okm[:], okm[:], msk[:], ALU.mult, r=["okm", "msk"], w=["okm"])
                            b.tt("dve", val[:], ebase[:], pos[:], ALU.subtract, r=["pos", "const"], w=["val"])
                            b.tt("dve", val[:], val[:], okm[:], ALU.mult, r=["val", "okm"], w=["val"])
                            b.S.add("dve", lambda e: e.max(v8[:], val[:]), ["val"], ["v8"])
                            b.ts("dve", idf[:], v8[:, 0:4], -1.0, float(NROWS), ALU.mult, ALU.add, r=["v8"], w=["idf"])
                            b.cp("dve", idx_all[:, tt, :], idf[:], r=["idf"], w=[("idx", tt)])
                            for k in range(4):
                                b.stt(junk[:], val[:], v8[:, k:k + 1], G[:], ALU.is_equal, ALU.mult,
                                      r=["val", "v8", "G"], w=["junk", ("gate", tt)], accum_out=gate_all[:, tt, k:k + 1])
                                b.scatter(xs_d[:, :], idx_all[:, tt, k:k + 1], x1b[:, u, :],
                                          r=[("idx", tt), ("x1b", u)])
                    S.end_phase()

            if "ex" in phases:
                with contextlib.ExitStack() as ph:
                    def pb(name, shp, dt):
                        return ph.enter_context(nc.sbuf_tensor(uniq(name), shp, dt))
                    NJ = CAP // 128
                    NCH = 12
                    wgu = pb("wgu", [128, 2, KC, 2048], BF16)
                    wdn = pb("wdn", [128, 2, KC, D], BF16)
                    wst = pb("wst", [128, 3, 2048], F32)
                    bgu = pb("bgu", [128, 2, 16], F32)
                    bgu1 = pb("bgu1", [128, 2, 8], F32)
                    bdn = pb("bdn", [128, 2, D], F32)
                    xst = pb("xst", [128, 2, D], BF16)
                    xsT = pb("xsT", [128, 2, KC, CAP], BF16)
                    gsb = pb("gsb", [128, 2, CAP], F32)
                    sg = pb("sg", [128, 2, CAP], F32)
                    glu = pb("glu", [128, 8, CAP], BF16)
                    ub = pb("ub", [128, 2, CAP], F32)
                    aT = pb("aT", [128, 8, CAP], BF16)
                    yt = pb("yt", [128, 2, D], F32)
                    NG = NE * NCH

                    def dma_chunk(g):
                        if g >= NG:
                            return
                        e, i = divmod(g, NCH)
                        slot = g % 3
                        if i < 8:
                            b.dma("sp", wst[:, slot, :], I["w_gu"][l, e, i * 128:(i + 1) * 128, :], w=[("wst", slot)])
                        else:
                            kp = i - 8
                            b.dma("sp", wst[:, slot, :].rearrange("p (k d) -> p k d", k=2),
                                  I["w_down"][l, e, kp * 256:(kp + 1) * 256, :].rearrange("(k p) d -> p k d", p=128),
                                  w=[("wst", slot)])

                    def cast_chunk(g):
                        if g >= NG:
                            return
                        e, i = divmod(g, NCH)
                        slot = g % 3
                        wb = e % 2
                        eng = "act" if g % 2 == 0 else "dve"
                        if i < 8:
                            b.cp(eng, wgu[:, wb, i, :], wst[:, slot, :], r=[("wst", slot)], w=[("wgu", wb, i)])
                        else:
                            kp = i - 8
                            b.cp(eng, wdn[:, wb, 2 * kp:2 * kp + 2, :], wst[:, slot, :].rearrange("p (k d) -> p k d", k=2),
                                 r=[("wst", slot)], w=[("wdn", wb, kp)])
                        dma_chunk(g + 3)

                    def load_bias(e):
                        wb = e % 2
                        b.dma("sp", bgu[:, wb, :], I["b_gu"][l, e:e + 1, :].rearrange("o (c p) -> p (o c)", p=128),
                              w=[("bgu", wb)], nc_ok=True)
                        b.dma("sp", bdn[:, wb, :], I["b_down"][l, e:e + 1, :].partition_broadcast(128), w=[("bdn", wb)])
                        b.ts("pool", bgu1[:, wb, :], bgu[:, wb, 8:16], 1.0, None, ALU.add, r=[("bgu", wb)], w=[("bgu1", wb)])

                    def load_x(e):
                        xb = e % 2
                        for j in range(NJ):
                            u = j % 2
                            b.dma("sp", xst[:, u, :], xs_d[e * CAP + j * 128:e * CAP + (j + 1) * 128, :],
                                  w=[("xst", u)])
                            pst = ps[6 + u][:].bitcast(BF16)
                            for kc in range(KC):
                                b.tr(pst[:, kc * 128:(kc + 1) * 128], xst[:, u, kc * 128:(kc + 1) * 128], ident_bf[:],
                                     r=[("xst", u), "const"], w=[PS(6 + u)])
                            b.evac(xsT[:, xb, :, j * 128:(j + 1) * 128], pst.rearrange("p (k t) -> p k t", k=KC),
                                   r=[PS(6 + u)], w=[("xsT", xb)])

                    for g in range(3):
                        dma_chunk(g)
                    load_bias(0)
                    load_x(0)
                    for g in range(NCH):
                        cast_chunk(g)
                    halves = [(0, 512), (512, CAP)]
                    for e in range(NE):
                        wb = e % 2
                        xb = e % 2
                        if e + 1 < NE:
                            load_bias(e + 1)
                        for fc in range(16):
                            u = fc % 2
                            pbig = ps_big[fc % 2]
                            bk = [PS((fc % 2) * 2), PS((fc % 2) * 2 + 1)]
                            for hi, (c0, c1) in enumerate(halves):
                                for kc in range(KC):
                                    b.mm(pbig[:, c0:c1], wgu[:, wb, kc, fc * 128:(fc + 1) * 128], xsT[:, xb, kc, c0:c1],
                                         start=(kc == 0), stop=(kc == KC - 1), r=[("wgu", wb, kc), ("xsT", xb)],
                                         w=[bk[hi]])
                            if fc < 8:
                                b.ts("dve", gsb[:, u, :], pbig[:, 0:CAP], bgu[:, wb, fc:fc + 1], 7.0, ALU.add, ALU.min,
                                     r=bk + [("bgu", wb)], w=[("gsb", u)])
                                b.act(sg[:, u, :], gsb[:, u, :], AF.Sigmoid, scale=1.702, r=[("gsb", u)], w=[("sg", u)])
                                b.tt("pool", glu[:, fc, :], gsb[:, u, :], sg[:, u, :], ALU.mult,
                                     r=[("gsb", u), ("sg", u)], w=[("glu", fc)])
                            else:
                                b.act(ub[:, u, :], pbig[:, 0:CAP], AF.Identity, bias=bgu1[:, wb, fc - 8:fc - 7],
                                      r=bk + [("bgu1", wb)], w=[("ub", u)])
                                b.ts("dve", ub[:, u, :], ub[:, u, :], 8.0, -6.0, ALU.min, ALU.max, r=[("ub", u)], w=[("ub", u)])
                                b.tt("pool", aT[:, fc - 8, :], ub[:, u, :], glu[:, fc - 8, :], ALU.mult,
                                     r=[("ub", u), ("glu", fc - 8)], w=[("aT", fc - 8)])
                            if e + 1 < NE and fc < NCH:
                                cast_chunk((e + 1) * NCH + fc)
                        if e + 1 < NE:
                            load_x(e + 1)
                        for j in range(NJ):
                            u = j % 2
                            for half in range(2):
                                pa = ps[4 + half]
                                for fc in range(8):
                                    b.mm(pa[:], aT[:, fc, j * 128:(j + 1) * 128], wdn[:, wb, fc, half * 512:(half + 1) * 512],
                                         start=(fc == 0), stop=(fc == 7), r=[("aT", fc), ("wdn", wb, fc // 2)], w=[PS(4 + half)])
                                b.tt("dve", yt[:, u, half * 512:(half + 1) * 512], pa[:], bdn[:, wb, half * 512:(half + 1) * 512],
                                     ALU.add, r=[PS(4 + half), ("bdn", wb)], w=[("yt", u)])
                            b.dma("pool", ys_d[e * CAP + j * 128:e * CAP + (j + 1) * 128, :], yt[:, u, :],
                                  r=[("yt", u)])
                    S.end_phase()

            if "cb" in phases:
                with contextlib.ExitStack() as ph:
                    def pb(name, shp, dt):
                        return ph.enter_context(nc.sbuf_tensor(uniq(name), shp, dt))
                    g_bc = pb("g2_bc", [128, D], F32)
                    b_bc = pb("b2_bc", [128, D], F32)
                    b.dma("sp", g_bc[:], I["ln2_g"][l:l + 1, :].partition_broadcast(128), w=["lnp"])
                    b.dma("sp", b_bc[:], I["ln2_b"][l:l + 1, :].partition_broadcast(128), w=["lnp"])
                    xt = pb("xt2", [128, 2, D], F32)
                    rows = pb("rows", [128, 2, 4, D], F32)
                    xo = pb("xo", [128, 2, D], F32)
                    tstat = pb("tstat2", [128, 2, 6], F32)
                    tmv = pb("tmv2", [128, 2], F32)
                    tr_ = pb("tr2_", [128, 2], F32)
                    def cb_load(tt):
                        u = tt % 2
                        b.dma("sp", xt[:, u, :], x1_d[tt * 128:(tt + 1) * 128, :], w=[("xt", u)])
                        for k in range(4):
                            b.gather(rows[:, u, k, :], ys_d[:, :], idx_all[:, tt, k:k + 1],
                                     r=[("idx", tt)], w=[("rows", u, k)])

                    cb_load(0)
                    for tt in range(NT):
                        u = tt % 2
                        if tt + 1 < NT:
                            cb_load(tt + 1)
                        b.ts("dve", xt[:, u, :], xt[:, u, :], DN_ALPHA, None, ALU.mult, r=[("xt", u)], w=[("xt", u)])
                        for k in range(4):
                            b.stt(xt[:, u, :], rows[:, u, k, :], gate_all[:, tt, k:k + 1], xt[:, u, :], ALU.mult, ALU.add,
                                  r=[("rows", u, k), ("gate", tt), ("xt", u)], w=[("xt", u)])
                        layer_norm(xt[:, u, :], g_bc[:], b_bc[:], xo[:, u, :], ("xt", u), ("xo", u), tstat, tmv, tr_, aff="dve")
                        b.dma("sp", xnext[tt * 128:(tt + 1) * 128, :], xo[:, u, :], r=[("xo", u)])
                    S.end_phase()
            xcur = xnext
        S.end_phase(final=True)
    return nc


_CACHE = {}


def kernel(**inputs):
    if "nc" not in _CACHE:
        _CACHE["nc"] = build()
    nc = _CACHE["nc"]
    consts = host_consts()
    x = np.ascontiguousarray(inputs["x"], dtype=np.float32)
    ncores = x.shape[0]
    shared = {k: np.ascontiguousarray(np.asarray(v, dtype=np.float32)) for k, v in inputs.items() if k != "x"}
    in_maps = []
    for c in range(ncores):
        m = dict(shared)
        m.update(consts)
        m["x"] = x[c]
        in_maps.append(m)
    res = run_bass_kernel_spmd(nc, in_maps, core_ids=list(range(ncores)))
    return np.stack([r["y"] for r in res.results], axis=0).astype(np.float32)
```

```python
import contextlib
import math
import numpy as np
import ml_dtypes
import concourse.bass as bass
import concourse.mybir as mybir
from concourse.bass_utils import run_bass_kernel_spmd

F32 = mybir.dt.float32
BF16 = mybir.dt.bfloat16
I32 = mybir.dt.int32
ALU = mybir.AluOpType
AF = mybir.ActivationFunctionType
AX = mybir.AxisListType

ENGS = ("pe", "act", "dve", "pool", "sp")
NDMASEM = 8
SEM_EPOCH = 30000
DMA_EPOCH = 1800

DEPTH = 4
T = 4096
NT = 32
D = 1024
KC = 8
IN_W = 3080
NE = 32
CAP = 640
NROWS = NE * CAP
DN_ALPHA = (2 * DEPTH) ** 0.25
LN_EPS = 1e-5
RMS_EPS = 1e-5


class Op:
    __slots__ = ("eng", "emit", "deps", "isdma", "sig", "sem", "val", "pre", "idx")


class Sched:
    def __init__(self, nc):
        self.nc = nc
        self.ops = {e: [] for e in ENGS}
        self.track = {}
        self.ndma = {e: 0 for e in ENGS}
        self.fence = []
        self.fenced = set(ENGS)

    def barrier(self):
        f = []
        for e in ENGS:
            last = None
            dm = []
            for op in reversed(self.ops[e]):
                if op.isdma:
                    if len(dm) < NDMASEM:
                        dm.append(op)
                elif last is None:
                    last = op
                if last is not None and len(dm) >= NDMASEM:
                    break
            if last is not None:
                f.append(last)
            f.extend(dm)
        self.fence = f
        self.fenced = set()

    def add(self, eng, emit, reads=(), writes=(), dma=False):
        op = Op()
        op.eng = eng
        op.emit = emit
        op.isdma = dma
        op.sig = False
        op.sem = None
        op.val = 0
        op.pre = None
        op.idx = len(self.ops[eng])
        deps = {}

        def need(p, raw):
            if p is op:
                return
            if p.isdma or p.eng != eng or dma:
                deps[id(p)] = p
            elif raw and eng != "pe":
                deps[id(p)] = p

        if eng not in self.fenced:
            self.fenced.add(eng)
            for p in self.fence:
                if p.isdma or p.eng != eng:
                    deps[id(p)] = p
        for k in reads:
            t = self.track.get(k)
            if t is None:
                t = [None, {}]
                self.track[k] = t
            if t[0] is not None:
                need(t[0], True)
        for k in writes:
            t = self.track.get(k)
            if t is None:
                t = [None, {}]
                self.track[k] = t
            if t[0] is not None:
                need(t[0], False)
            for r in t[1].values():
                need(r, False)
        for k in reads:
            t = self.track[k]
            key = (eng, op.idx) if dma else eng
            t[1][key] = op
        for k in writes:
            t = self.track[k]
            t[0] = op
            t[1] = {}
        op.deps = list(deps.values())
        if dma:
            j = self.ndma[eng]
            self.ndma[eng] = j + 1
            r = j // NDMASEM
            ep = r // DMA_EPOCH
            op.sem = ("dma", eng, j % NDMASEM, ep)
            op.val = 16 * (r % DMA_EPOCH + 1)
            if j >= NDMASEM:
                rp = r - 1
                op.pre = (("dma", eng, j % NDMASEM, rp // DMA_EPOCH), 16 * (rp % DMA_EPOCH + 1))
        self.ops[eng].append(op)
        return op

    def end_phase(self, final=False):
        nc = self.nc
        if not hasattr(self, "upto"):
            self.upto = {e: 0 for e in ENGS}
            self.semcount = {e: 0 for e in ENGS}
            self.sems = {}
            self.waited = {e: {} for e in ENGS}
        self.barrier()
        for p in self.fence:
            if not p.isdma:
                p.sig = True
        for e in ENGS:
            for op in self.ops[e][self.upto[e]:]:
                for p in op.deps:
                    if not p.isdma and p.idx >= self.upto[p.eng]:
                        p.sig = True
        for e in ENGS:
            c = self.semcount[e]
            for op in self.ops[e][self.upto[e]:]:
                if (not op.isdma) and op.sig:
                    op.sem = ("c", e, c // SEM_EPOCH)
                    op.val = c % SEM_EPOCH + 1
                    c += 1
            self.semcount[e] = c

        def getsem(k):
            if k not in self.sems:
                self.sems[k] = self.semstack.enter_context(nc.semaphore("s%d" % len(self.sems)))
            return self.sems[k]

        with nc.Block() as block:
            def replay(e, eng):
                waited = self.waited[e]
                for op in self.ops[e][self.upto[e]:]:
                    ws = [(p.sem, p.val) for p in op.deps if p.sem is not None]
                    if op.pre is not None:
                        ws.append(op.pre)
                    for s_, v in ws:
                        if waited.get(s_, 0) < v:
                            eng.wait_ge(getsem(s_), v)
                            waited[s_] = v
                    ins = op.emit(eng)
                    if op.isdma:
                        ins.then_inc(getsem(op.sem), 16)
                    elif op.sig:
                        ins.then_inc(getsem(op.sem), 1)
                    op.emit = None
                if final and e == "sp":
                    for q in ENGS:
                        dm = [op for op in self.ops[q] if op.isdma][-NDMASEM:]
                        for op in dm:
                            if waited.get(op.sem, 0) < op.val:
                                eng.wait_ge(getsem(op.sem), op.val)
                                waited[op.sem] = op.val
                self.upto[e] = len(self.ops[e])

            @block.tensor
            def _(eng):
                replay("pe", eng)

            @block.scalar
            def _(eng):
                replay("act", eng)

            @block.vector
            def _(eng):
                replay("dve", eng)

            @block.gpsimd
            def _(eng):
                replay("pool", eng)

            @block.sync
            def _(eng):
                replay("sp", eng)


class B:
    def __init__(self, nc):
        self.nc = nc
        self.S = Sched(nc)
        self.rr = 0

    def mm(self, out, lhsT, rhs, start=True, stop=True, r=(), w=()):
        self.S.add("pe", lambda e: e.matmul(out, lhsT, rhs, start=start, stop=stop), r, w)

    def tr(self, out, in_, ident, r=(), w=()):
        self.S.add("pe", lambda e: e.transpose(out, in_, ident), r, w)

    def act(self, out, in_, func, bias=0.0, scale=1.0, r=(), w=(), accum_out=None):
        if accum_out is None:
            self.S.add("act", lambda e: e.activation(out, in_, func, bias=bias, scale=scale), r, w)
        else:
            self.S.add("act", lambda e: e.activation(out, in_, func, bias=bias, scale=scale,
                                                     accum_out=accum_out), r, w)

    def ts(self, eng, out, in0, s1, s2, op0, op1=None, r=(), w=(), accum_out=None):
        if op1 is None:
            self.S.add(eng, lambda e: e.tensor_scalar(out, in0, s1, None, op0), r, w)
        elif accum_out is None:
            self.S.add(eng, lambda e: e.tensor_scalar(out, in0, s1, s2, op0, op1), r, w)
        else:
            self.S.add(eng, lambda e: e.tensor_scalar(out, in0, s1, s2, op0, op1, accum_out), r, w)

    def tt(self, eng, out, in0, in1, op, r=(), w=()):
        self.S.add(eng, lambda e: e.tensor_tensor(out, in0, in1, op), r, w)

    def stt(self, out, in0, scalar, in1, op0, op1, r=(), w=(), accum_out=None):
        if accum_out is None:
            self.S.add("dve", lambda e: e.scalar_tensor_tensor(out, in0, scalar, in1, op0, op1), r, w)
        else:
            self.S.add("dve", lambda e: e.scalar_tensor_tensor(out, in0, scalar, in1, op0, op1,
                                                               accum_out), r, w)

    def cp(self, eng, out, in_, r=(), w=()):
        if eng == "act":
            self.S.add("act", lambda e: e.copy(out, in_), r, w)
        else:
            self.S.add(eng, lambda e: e.tensor_copy(out, in_), r, w)

    def evac(self, out, in_, r=(), w=()):
        self.rr ^= 1
        self.cp("act" if self.rr else "dve", out, in_, r, w)

    def memset(self, eng, ap, val, r=(), w=()):
        self.S.add(eng, lambda e: e.memset(ap, val), r, w)

    def dma(self, eng, out, in_, r=(), w=(), nc_ok=False):
        if nc_ok:
            self.S.add(eng, lambda e: e.dma_start(out=out, in_=in_, allow_slow_non_contiguous=True),
                       r, w, dma=True)
        else:
            self.S.add(eng, lambda e: e.dma_start(out=out, in_=in_), r, w, dma=True)

    def scatter(self, out_dram, idx, in_sb, r=(), w=()):
        self.S.add("pool", lambda e: e.indirect_dma_start(
            out=out_dram, out_offset=bass.IndirectOffsetOnAxis(ap=idx, axis=0),
            in_=in_sb, in_offset=None), r, w, dma=True)

    def gather(self, out_sb, in_dram, idx, r=(), w=()):
        self.S.add("pool", lambda e: e.indirect_dma_start(
            out=out_sb, out_offset=None, in_=in_dram,
            in_offset=bass.IndirectOffsetOnAxis(ap=idx, axis=0),
            ), r, w, dma=True)


def host_consts():
    c = {}
    c["ident_bf"] = np.eye(128, dtype=np.float32).astype(ml_dtypes.bfloat16)
    c["ident_f"] = np.eye(128, dtype=np.float32)
    tri = (np.arange(128)[:, None] <= np.arange(128)[None, :]).astype(np.float32)
    c["tri_bf"] = tri.astype(ml_dtypes.bfloat16)
    c["tri_f"] = tri
    c["tris_bf"] = (np.arange(128)[:, None] < np.arange(128)[None, :]).astype(np.float32).astype(ml_dtypes.bfloat16)
    c["ones_bf"] = np.ones((128, 128), dtype=ml_dtypes.bfloat16)
    c["ones_f"] = np.ones((128, 128), dtype=np.float32)
    c["ebase"] = np.broadcast_to((NROWS - np.arange(NE) * CAP).astype(np.float32)[None, :], (128, NE)).copy()
    return c


CONST_SPECS = [("ident_bf", BF16, [128, 128]), ("ident_f", F32, [128, 128]), ("tri_bf", BF16, [128, 128]),
               ("tri_f", F32, [128, 128]), ("tris_bf", BF16, [128, 128]), ("ones_bf", BF16, [128, 128]),
               ("ones_f", F32, [128, 128]), ("ebase", F32, [128, NE])]

IN_SPECS = [("x", [T, D]), ("w_in", [DEPTH, D, IN_W]), ("conv_w", [DEPTH, 4, 512]), ("conv_b", [DEPTH, 512]),
            ("gate_b", [DEPTH, 8]), ("lam_q1", [DEPTH, 64]), ("lam_k1", [DEPTH, 64]), ("lam_q2", [DEPTH, 64]),
            ("lam_k2", [DEPTH, 64]), ("da_norm_g", [DEPTH, 128]), ("w_out", [DEPTH, D, D]),
            ("ln1_g", [DEPTH, D]), ("ln1_b", [DEPTH, D]), ("w_router", [DEPTH, D, NE]), ("b_router", [DEPTH, NE]),
            ("w_gu", [DEPTH, NE, D, 2048]), ("b_gu", [DEPTH, NE, 2048]), ("w_down", [DEPTH, NE, D, D]),
            ("b_down", [DEPTH, NE, D]), ("ln2_g", [DEPTH, D]), ("ln2_b", [DEPTH, D])]


def build(nlayers=DEPTH, phases=("da", "ml", "op", "ex", "cb"), debug=()):
    nc = bass.Bass("TRN2", target_bir_lowering=False)
    I = {}
    for name, shp in IN_SPECS:
        I[name] = nc.dram_tensor(name, shp, F32, kind="ExternalInput").ap()
    for name, dt, shp in CONST_SPECS:
        I[name] = nc.dram_tensor(name, shp, dt, kind="ExternalInput").ap()
    y_out = nc.dram_tensor("y", [T, D], F32, kind="ExternalOutput").ap()

    def scratch(name, shp, dt):
        kind = "ExternalOutput" if name in debug else "Internal"
        return nc.dram_tensor(name, shp, dt, kind=kind).ap()

    mixT_d = scratch("mixT_d", [D, T], BF16)
    x1_d = scratch("x1_d", [T, D], F32)
    xa_d = scratch("xa_d", [T, D], F32)
    xb_d = scratch("xb_d", [T, D], F32)
    xs_d = scratch("xs_d", [NROWS + 1, D], BF16)
    ys_d = scratch("ys_d", [NROWS + 1, D], F32)

    b = B(nc)
    S = b.S
    _cnt = [0]

    def uniq(name):
        _cnt[0] += 1
        return "sb%d_%s" % (_cnt[0], name)
    st = contextlib.ExitStack()
    with st:
        S.semstack = st
        def sb(name, shp, dt):
            return st.enter_context(nc.sbuf_tensor(uniq(name), shp, dt))

        ident_bf = sb("ident_bf", [128, 128], BF16)
        ident_f = sb("ident_f", [128, 128], F32)
        tri_bf = sb("tri_bf", [128, 128], BF16)
        tri_f = sb("tri_f", [128, 128], F32)
        tris_bf = sb("tris_bf", [128, 128], BF16)
        ones_bf = sb("ones_bf", [128, 128], BF16)
        ones_f = sb("ones_f", [128, 128], F32)
        ebase = sb("ebase", [128, NE], F32)
        for name, t_ in (("ident_bf", ident_bf), ("ident_f", ident_f), ("tri_bf", tri_bf), ("tri_f", tri_f),
                         ("tris_bf", tris_bf), ("ones_bf", ones_bf), ("ones_f", ones_f), ("ebase", ebase)):
            b.dma("sp", t_[:], I[name], w=["const"])
        idx_all = sb("idx_all", [128, NT, 4], I32)
        gate_all = sb("gate_all", [128, NT, 4], F32)
        with nc.sbuf_tensor("sb_zrow", [1, D], F32) as zrow:
            b.memset("dve", zrow[:], 0.0, w=["zrow"])
            b.dma("sp", ys_d[NROWS:NROWS + 1, :], zrow[:], r=["zrow"])

        ps_big = [st.enter_context(nc.psum_tensor("psb%d" % i, [128, 1024], F32)) for i in range(4)]
        ps = [ps_big[i // 2][:, (i % 2) * 512:(i % 2 + 1) * 512] for i in range(8)]

        def PS(i):
            return ("ps", i)

        def stream_xT(xsrc, blk, xstage, xT_blk, tag, nbuf=2):
            buf = blk % nbuf
            for j in range(4):
                tt = blk * 4 + j
                sbuf_i = tt % 2
                b.dma("pool", xstage[:, sbuf_i, :], xsrc[tt * 128:(tt + 1) * 128, :],
                      w=[("xstage", sbuf_i)])
                pst = ps[7][:].bitcast(BF16)
                for kc in range(KC):
                    b.tr(pst[:, kc * 128:(kc + 1) * 128], xstage[:, sbuf_i, kc * 128:(kc + 1) * 128], ident_bf[:],
                         r=[("xstage", sbuf_i), "const"], w=[PS(7)])
                b.evac(xT_blk[:, buf, :, j * 128:(j + 1) * 128],
                       pst.rearrange("p (k t) -> p k t", k=KC), r=[PS(7)], w=[(tag, buf)])

        def layer_norm(xt, g_bc, b_bc, out, keyin, keyout, tmpstat, tmpmv, tmpr, aff="pool"):
            b.S.add("dve", lambda e: e.bn_stats(tmpstat[:, 0, :], xt[:, 0:512]), [keyin], ["lnstat"])
            b.S.add("dve", lambda e: e.bn_stats(tmpstat[:, 1, :], xt[:, 512:1024]), [keyin], ["lnstat"])
            b.S.add("dve", lambda e: e.bn_aggr(tmpmv[:], tmpstat[:].rearrange("p a b -> p (a b)")), ["lnstat"], ["lnmv"])
            b.act(tmpr[:, 0:1], tmpmv[:, 1:2], AF.Ln, bias=epsc[:, 0:1], scale=1.0, r=["lnmv", "epsc"], w=["lnr0"])
            b.act(tmpr[:, 1:2], tmpr[:, 0:1], AF.Exp, scale=-0.5, r=["lnr0"], w=["lnr1"])
            b.ts("dve", xt, xt, tmpmv[:, 0:1], tmpr[:, 1:2], ALU.subtract, ALU.mult, r=[keyin, "lnmv", "lnr1"], w=[keyin])
            b.tt(aff, xt, xt, g_bc, ALU.mult, r=[keyin, "lnp"], w=[keyin])
            b.tt(aff, out, xt, b_bc, ALU.add, r=[keyin, "lnp"], w=[keyout])

        epsc = sb("epsc", [128, 2], F32)
        b.memset("dve", epsc[:, 0:1], LN_EPS, w=["epsc"])
        b.memset("dve", epsc[:, 1:2], 128.0 * RMS_EPS, w=["epsc"])

        xcur = I["x"]
        for l in range(nlayers):
            lam_init = 0.8 - 0.6 * math.exp(-0.3 * l)
            xnext = y_out if l == nlayers - 1 else (xa_d if l % 2 == 0 else xb_d)
            if "da" in phases:
                with contextlib.ExitStack() as ph:
                    def pb(name, shp, dt):
                        return ph.enter_context(nc.sbuf_tensor(uniq(name), shp, dt))
                    w_da = pb("w_da", [128, KC, 1536], BF16)
                    for kc in range(KC):
                        b.dma("pool", w_da[:, kc, :], I["w_in"][l, kc * 128:(kc + 1) * 128, 0:1536], w=["w_da"])
                    xstage = pb("xstage", [128, 2, D], BF16)
                    xT_blk = pb("xT_blk", [128, 2, KC, 512], BF16)
                    qT = pb("qT", [128, 4, T], BF16)
                    kT = pb("kT", [128, 4, T], BF16)
                    Vd = pb("Vd", [128, NT, 512], BF16)
                    lamt = pb("lamt", [128, 4, 64], F32)
                    lamv = pb("lamv", [128, 8], F32)
                    gcol = pb("gcol", [128, 2], F32)
                    for i, nm in enumerate(("lam_q1", "lam_k1", "lam_q2", "lam_k2")):
                        b.dma("sp", lamt[:, i, :], I[nm][l:l + 1, :].partition_broadcast(128), w=["lamt"])
                    b.dma("sp", gcol[:, 0:1], I["da_norm_g"][l:l + 1, :].rearrange("o p -> p o"), w=["gcol"], nc_ok=True)
                    b.tt("dve", lamt[:, 0, :], lamt[:, 0, :], lamt[:, 1, :], ALU.mult, r=["lamt"], w=["lamt"])
                    b.tt("dve", lamt[:, 2, :], lamt[:, 2, :], lamt[:, 3, :], ALU.mult, r=["lamt"], w=["lamt"])
                    b.S.add("dve", lambda e: e.reduce_sum(lamv[:, 0:1], lamt[:, 0, :], AX.X), ["lamt"], ["lamv"])
                    b.S.add("dve", lambda e: e.reduce_sum(lamv[:, 1:2], lamt[:, 2, :], AX.X), ["lamt"], ["lamv"])
                    b.act(lamv[:, 2:4], lamv[:, 0:2], AF.Exp, r=["lamv"], w=["lamv2"])
                    b.tt("dve", lamv[:, 4:5], lamv[:, 3:4], lamv[:, 2:3], ALU.subtract, r=["lamv2"], w=["lamv3"])
                    b.ts("dve", lamv[:, 5:6], lamv[:, 4:5], -lam_init, None, ALU.add, r=["lamv3"], w=["neglam"])
                    neglam = lamv[:, 5:6]
                    b.ts("dve", gcol[:, 1:2], gcol[:, 0:1], (1.0 - lam_init) * math.sqrt(128.0), None, ALU.mult,
                         r=["gcol"], w=["gcol2"])
                    for blk in range(8):
                        stream_xT(xcur, blk, xstage, xT_blk, "xT")
                        buf = blk % 2
                        for grp in range(8):
                            pa = ps[grp % 2]
                            for kc in range(KC):
                                b.mm(pa[:], w_da[:, kc, grp * 128:(grp + 1) * 128], xT_blk[:, buf, kc, :],
                                     start=(kc == 0), stop=(kc == KC - 1), r=["w_da", ("xT", buf)], w=[PS(grp % 2)])
                            dst = qT if grp < 4 else kT
                            b.evac(dst[:, grp % 4, blk * 512:(blk + 1) * 512], pa[:], r=[PS(grp % 2)],
                                   w=[("q" if grp < 4 else "k", blk)])
                        for j in range(4):
                            tt = blk * 4 + j
                            pa = ps[j % 2]
                            for kc in range(KC):
                                b.mm(pa[:], xT_blk[:, buf, kc, j * 128:(j + 1) * 128], w_da[:, kc, 1024:1536],
                                     start=(kc == 0), stop=(kc == KC - 1), r=["w_da", ("xT", buf)], w=[PS(j % 2)])
                            b.evac(Vd[:, tt, :], pa[:], r=[PS(j % 2)], w=[("v", tt)])
                    Pt = pb("Pt", [128, 2, 3, 512], BF16)
                    acc = pb("acc", [128, 2, 2, 512], F32)
                    Osb = pb("Osb", [128, 2, 512], F32)
                    Lsb = pb("Lsb", [128, 2, 512], F32)
                    Rc = pb("Rc", [128, 2, 512], F32)
                    oc = pb("oc", [128, 2, 512], F32)
                    sq = pb("sq", [128, 2, 512], F32)
                    rs = pb("rs", [128, 512], F32)
                    ob = pb("ob", [128, 2, 512], BF16)
                    items = []
                    for Qb in range(8):
                        for h in range(4):
                            nk = (Qb + 1) * 4
                            for kt in range(nk):
                                items.append((Qb, h, kt, nk))
                    ACC_ENG = ("pool", "dve")

                    def emit_S(i):
                        Qb, h, kt, nk = items[i]
                        diag = kt >= Qb * 4
                        q0 = (kt - Qb * 4) * 128 if diag else 0
                        n = 512 - q0
                        for c in range(2):
                            lo = c * 64
                            sbank = 2 * (i % 2) + c
                            b.mm(ps[sbank][:, 0:n], kT[lo:lo + 64, h, kt * 128:(kt + 1) * 128],
                                 qT[lo:lo + 64, h, Qb * 512 + q0:(Qb + 1) * 512],
                                 r=[("k", kt // 4), ("q", Qb)], w=[PS(sbank)])

                    def epi_0(Qb, h):
                        for c in range(2):
                            b.cp("act", Osb[:, c, :], ps[4 + c][:], r=[PS(4 + c)], w=[("Osb", c)])

                    def epi_1(Qb, h):
                        gp = (Qb * 4 + h) % 2
                        for c in range(2):
                            b.mm(ps[6 + c][:], ones_f[:], acc[:, c, gp, :], r=["const", ("acc", c, gp)], w=[PS(6 + c)])
                        for c in range(2):
                            b.act(Lsb[:, c, :], ps[6 + c][:], AF.Ln, r=[PS(6 + c)], w=[("Lsb", c)])
                            b.act(Lsb[:, c, :], Lsb[:, c, :], AF.Exp, scale=-1.0, r=[("Lsb", c)], w=[("Lsb", c)])

                    def epi_2(Qb, h):
                        gp = (Qb * 4 + h) % 2
                        for c in range(2):
                            b.tt("dve", Rc[:, c, :], Osb[:, c, :], Lsb[:, c, :], ALU.mult, r=[("Osb", c), ("Lsb", c)], w=[("Rc", c)])
                        b.stt(oc[:, gp, :], Rc[:, 1, :], neglam, Rc[:, 0, :], ALU.mult, ALU.add,
                              r=[("Rc", 0), ("Rc", 1), "neglam"], w=[("oc", gp)])
                        b.act(sq[:, gp, :], oc[:, gp, :], AF.Square, r=[("oc", gp)], w=[("sq", gp)])

                    def epi_3(Qb, h):
                        gp = (Qb * 4 + h) % 2
                        b.mm(ps[6][:], ones_f[:], sq[:, gp, :], r=[("sq", gp), "const"], w=[PS(6)])
                        b.act(rs[:], ps[6][:], AF.Ln, bias=epsc[:, 1:2], r=[PS(6), "epsc"], w=["rs"])
                        b.act(rs[:], rs[:], AF.Exp, scale=-0.5, r=["rs"], w=["rs"])
                        b.stt(ob[:, gp, :], oc[:, gp, :], gcol[:, 1:2], rs[:], ALU.mult, ALU.mult,
                              r=[("oc", gp), "rs", "gcol2"], w=[("ob", gp)])
                        b.dma("sp", mixT_d[h * 128:(h + 1) * 128, Qb * 512:(Qb + 1) * 512], ob[:, gp, :],
                              r=[("ob", gp)])

                    pend = []
                    emit_S(0)
                    for i, (Qb, h, kt, nk) in enumerate(items):
                        if i + 1 < len(items):
                            emit_S(i + 1)
                        gp = (Qb * 4 + h) % 2
                        diag = kt >= Qb * 4
                        q0 = (kt - Qb * 4) * 128 if diag else 0
                        n = 512 - q0
                        pbuf = i % 3
                        if kt == 0:
                            b.memset("pool", acc[:, 0, gp, :], 0.0, w=[("acc", 0, gp)])
                            b.memset("dve", acc[:, 1, gp, :], 0.0, w=[("acc", 1, gp)])
                        for c in range(2):
                            sbank = 2 * (i % 2) + c
                            b.act(Pt[:, c, pbuf, 0:n], ps[sbank][:, 0:n], AF.Exp, scale=0.125,
                                  r=[PS(sbank)], w=[("Pt", c, pbuf)])
                            if diag:
                                b.tt("pool", Pt[:, c, pbuf, 0:128], Pt[:, c, pbuf, 0:128], tri_bf[:], ALU.mult,
                                     r=[("Pt", c, pbuf), "const"], w=[("Pt", c, pbuf)])
                            b.mm(ps[4 + c][:, q0:512], Vd[:, kt, h * 128:(h + 1) * 128], Pt[:, c, pbuf, 0:n],
                                 start=(kt == 0), stop=(kt == nk - 1), r=[("v", kt), ("Pt", c, pbuf)], w=[PS(4 + c)])
                            b.tt(ACC_ENG[c], acc[:, c, gp, q0:512], acc[:, c, gp, q0:512], Pt[:, c, pbuf, 0:n], ALU.add,
                                 r=[("acc", c, gp), ("Pt", c, pbuf)], w=[("acc", c, gp)])
                        while pend and (pend[0][0] <= kt or kt == nk - 1):
                            _, fn, args = pend.pop(0)
                            fn(*args)
                        if kt == nk - 1:
                            epi_0(Qb, h)
                            pend = [(2, epi_1, (Qb, h)), (5, epi_2, (Qb, h)), (8, epi_3, (Qb, h))]
                    for _, fn, args in pend:
                        fn(*args)
                    S.end_phase()

            if "ml" in phases:
                with contextlib.ExitStack() as ph:
                    def pb(name, shp, dt):
                        return ph.enter_context(nc.sbuf_tensor(uniq(name), shp, dt))
                    mq = pb("mq", [64, 4, T], BF16)
                    mk = pb("mk", [64, 4, T], BF16)
                    mv = pb("mv", [128, NT, 512], BF16)
                    mo = pb("mo", [128, 4, T], BF16)
                    gtm = pb("gtm", [128, NT, 8], F32)
                    lf = pb("lf", [128, NT, 4], F32)
                    btm = pb("btm", [128, NT, 4], F32)
                    gbc = pb("gbc", [128, NT, 4], F32)
                    ew = pb("ew", [128, NT, 4], F32)
                    ebias = pb("ebias", [128, NT, 4], F32)
                    eg = pb("eg", [128, NT, 4], F32)
                    nl8 = pb("nl8", [128, 2], F32)
                    b.memset("dve", nl8[:, 0:1], -math.log(8.0), w=["nl8"])
                    b.memset("dve", nl8[:, 1:2], 1.0, w=["nl8"])
                    with contextlib.ExitStack() as ph2:
                        def pb2(name, shp, dt):
                            return ph2.enter_context(nc.sbuf_tensor(uniq(name), shp, dt))
                        w_ml = pb2("w_ml", [128, KC, 1544], BF16)
                        for kc in range(KC):
                            b.dma("pool", w_ml[:, kc, :], I["w_in"][l, kc * 128:(kc + 1) * 128, 1536:3080], w=["w_ml"])
                        xstage = pb2("xstage", [128, 2, D], BF16)
                        xT_blk = pb2("xT_blk", [128, 1, KC, 512], BF16)
                        gb_bc = pb2("gb_bc", [128, 8], F32)
                        b.dma("sp", gb_bc[:], I["gate_b"][l:l + 1, :].partition_broadcast(128), w=["gb_bc"])
                        cw = pb2("cw", [64, 4, 8], F32)
                        cb = pb2("cb", [64, 8], F32)
                        for j in range(4):
                            b.dma("sp", cw[:, j, :], I["conv_w"][l, j:j + 1, :].rearrange("o (g p) -> p (o g)", p=64), w=["cw"], nc_ok=True)
                        b.dma("sp", cb[:], I["conv_b"][l:l + 1, :].rearrange("o (g p) -> p (o g)", p=64), w=["cw"], nc_ok=True)
                        cin = pb2("cin", [64, 8, 515], F32)
                        ctmp = pb2("ctmp", [64, 2, 512], F32)
                        b.memset("dve", cin[:, :, 0:3], 0.0, w=[("cin", g) for g in range(8)])
                        for blk in range(8):
                            stream_xT(xcur, blk, xstage, xT_blk, "xT", nbuf=1)
                            buf = 0
                            for grp in range(8):
                                pa = ps[grp % 2]
                                for kc in range(KC):
                                    b.mm(pa[0:64, :], w_ml[:, kc, grp * 64:(grp + 1) * 64], xT_blk[:, buf, kc, :],
                                         start=(kc == 0), stop=(kc == KC - 1), r=["w_ml", ("xT", buf)], w=[PS(grp % 2)])
                                b.cp("act", cin[:, grp, 3:515], pa[0:64, :], r=[PS(grp % 2)], w=[("cin", grp)])
                                ct = ctmp[:, grp % 2, :]
                                b.ts("dve", ct, cin[:, grp, 0:512], cw[:, 0, grp:grp + 1], cb[:, grp:grp + 1], ALU.mult, ALU.add,
                                     r=[("cin", grp), "cw"], w=[("ctmp", grp % 2)])
                                for j in range(1, 4):
                                    b.stt(ct, cin[:, grp, j:j + 512], cw[:, j, grp:grp + 1], ct, ALU.mult, ALU.add,
                                          r=[("cin", grp), "cw", ("ctmp", grp % 2)], w=[("ctmp", grp % 2)])
                                dst = mq if grp < 4 else mk
                                b.act(dst[:, grp % 4, blk * 512:(blk + 1) * 512], ct, AF.Silu,
                                      r=[("ctmp", grp % 2)], w=[("mqk", blk)])
                                b.cp("pool", cin[:, grp, 0:3], cin[:, grp, 512:515], r=[("cin", grp)], w=[("cin", grp)])
                            for h in range(4):
                                pa = ps[2 + h % 2]
                                for kc in range(KC):
                                    b.mm(pa[:], w_ml[:, kc, 1024 + h * 128:1024 + (h + 1) * 128], xT_blk[:, buf, kc, :],
                                         start=(kc == 0), stop=(kc == KC - 1), r=["w_ml", ("xT", buf)], w=[PS(2 + h % 2)])
                                b.act(mo[:, h, blk * 512:(blk + 1) * 512], pa[:], AF.Sigmoid, r=[PS(2 + h % 2)], w=[("mo", blk)])
                            for j in range(4):
                                tt = blk * 4 + j
                                pa = ps[4 + j % 2]
                                for kc in range(KC):
                                    b.mm(pa[:], xT_blk[:, buf, kc, j * 128:(j + 1) * 128], w_ml[:, kc, 512:1024],
                                         start=(kc == 0), stop=(kc == KC - 1), r=["w_ml", ("xT", buf)], w=[PS(4 + j % 2)])
                                b.evac(mv[:, tt, :], pa[:], r=[PS(4 + j % 2)], w=[("mv", tt)])
                                for kc in range(KC):
                                    b.mm(ps[6][:, 0:8], xT_blk[:, buf, kc, j * 128:(j + 1) * 128], w_ml[:, kc, 1536:1544],
                                         start=(kc == 0), stop=(kc == KC - 1), r=["w_ml", ("xT", buf)], w=[PS(6)])
                                b.tt("dve", gtm[:, tt, :], ps[6][:, 0:8], gb_bc[:], ALU.add, r=[PS(6), "gb_bc"], w=["gtm"])
                    S.barrier()
                    b.act(lf[:], gtm[:, :, 4:8], AF.Exp, scale=-1.0, r=["gtm"], w=["lf"])
                    b.act(lf[:], lf[:], AF.Ln, bias=nl8[:, 1:2], r=["lf", "nl8"], w=["lf"])
                    b.ts("dve", lf[:], lf[:], -1.0, None, ALU.mult, r=["lf"], w=["lf"])
                    lf2 = lf[:].rearrange("p a b -> p (a b)")
                    b.mm(ps[6][:, 0:128], tri_f[:], lf2, r=["lf", "const"], w=[PS(6)])
                    b.cp("dve", btm[:].rearrange("p a b -> p (a b)"), ps[6][:, 0:128], r=[PS(6)], w=["btm"])
                    b.mm(ps[6][:, 128:256], ones_f[:], lf2, r=["lf", "const"], w=[PS(6)])
                    b.cp("dve", gbc[:].rearrange("p a b -> p (a b)"), ps[6][:, 128:256], r=[PS(6)], w=["gbc"])
                    b.tt("dve", ebias[:], gtm[:, :, 0:4], btm[:], ALU.subtract, r=["gtm", "btm"], w=["ebias"])
                    b.tt("dve", ew[:], ebias[:], gbc[:], ALU.add, r=["ebias", "gbc"], w=["ew"])
                    b.act(ew[:], ew[:], AF.Exp, r=["ew"], w=["ew"])
                    b.ts("dve", ebias[:], ebias[:], -math.log(8.0), None, ALU.add, r=["ebias", "ew"], w=["ebias2"])
                    b.act(eg[:], gbc[:], AF.Exp, r=["gbc"], w=["eg"])
                    Cst = pb("Cst", [64, 4, 256], F32)
                    Cbf = pb("Cbf", [64, 4, 256], BF16)
                    b.memset("dve", Cst[:], 0.0, w=[("C", h) for h in range(4)])
                    b.memset("pool", Cbf[:], 0.0, w=[("Cbf", h) for h in range(4)])
                    lfd = pb("lfd", [128, 2, 4, 128], F32)
                    Et = pb("Et", [128, 2, 4, 128], F32)
                    Eq = pb("Eq", [64, 2, 4, 128], F32)
                    qs = pb("qs", [64, 2, 4, 128], BF16)
                    STt = pb("STt", [128, 2, 4, 128], BF16)
                    kh = pb("kh", [128, 2, 4, 64], BF16)
                    dm = pb("dm", [128, 2, 4, 128], F32)
                    hT = pb("hT", [128, 2, 4, 128], F32)
                    hst = pb("hst", [128, 2, 4, 512], BF16)
                    H4 = range(4)

                    def front(tt):
                        c0 = tt * 128
                        u = tt % 2
                        blk = tt // 4
                        sb_ = 1 if u == 0 else 7
                        for h in H4:
                            b.ts("dve", lfd[:, u, h, :], ones_f[:], lf[:, tt, h:h + 1], None, ALU.mult,
                                 r=["lf", "const"], w=[("lfd", u, h)])
                        for h in H4:
                            b.mm(ps[0][:, h * 128:(h + 1) * 128], lfd[:, u, h, :], tri_f[:], r=[("lfd", u, h), "const"], w=[PS(0)])
                        for h in H4:
                            b.mm(ps[sb_][:, h * 128:(h + 1) * 128], mk[:, h, c0:c0 + 128], mq[:, h, c0:c0 + 128],
                                 r=[("mqk", blk)], w=[PS(sb_)])
                        pkt = ps[4][:].bitcast(BF16)
                        for h in H4:
                            b.tr(pkt[:, h * 64:(h + 1) * 64], mk[:, h, c0:c0 + 128], ident_bf[0:64, 0:64],
                                 r=[("mqk", blk), "const"], w=[PS(4)])
                        for h in H4:
                            b.act(Et[:, u, h, :], ps[0][:, h * 128:(h + 1) * 128], AF.Exp, bias=ebias[:, tt, h:h + 1],
                                  r=[PS(0), "ebias2"], w=[("Et", u, h)])
                        b.act(Eq[:, u, :, :], ps[0][0:64, :].rearrange("p (h t) -> p h t", h=4), AF.Exp, bias=nl8[0:64, 0:1],
                              r=[PS(0), "nl8"], w=[("Eq", u)])
                        for h in H4:
                            b.tt("pool", Et[:, u, h, :], Et[:, u, h, :], tri_f[:], ALU.mult, r=[("Et", u, h), "const"], w=[("Et", u, h)])

                    def front_b(tt):
                        c0 = tt * 128
                        u = tt % 2
                        blk = tt // 4
                        pkt = ps[4][:].bitcast(BF16)
                        b.tt("dve", qs[:, u, :, :], mq[:, :, c0:c0 + 128], Eq[:, u, :, :], ALU.mult,
                             r=[("mqk", blk), ("Eq", u)], w=[("qs", u)])
                        for h in H4:
                            b.ts("dve", kh[:, u, h, :], pkt[:, h * 64:(h + 1) * 64], ew[:, tt, h:h + 1], None, ALU.mult,
                                 r=[PS(4), "ew"], w=[("kh", u, h)])

                    def back(tt):
                        c0 = tt * 128
                        u = tt % 2
                        blk = tt // 4
                        sb_ = 1 if u == 0 else 7
                        b.tt("dve", STt[:, u, :, :], ps[sb_][:].rearrange("p (h t) -> p h t", h=4), Et[:, u, :, :], ALU.mult,
                             r=[PS(sb_)] + [("Et", u, h) for h in H4], w=[("ST", u)])
                        for h in H4:
                            b.mm(ps[2][:, h * 128:(h + 1) * 128], mv[:, tt, h * 128:(h + 1) * 128], STt[:, u, h, :],
                                 start=True, stop=False, r=[("mv", tt), ("ST", u)], w=[PS(2)])
                            b.mm(ps[2][:, h * 128:(h + 1) * 128], Cbf[:, h, 0:128], qs[:, u, h, :],
                                 start=False, stop=True, r=[("Cbf", h), ("qs", u)], w=[PS(2)])
                        for h in H4:
                            b.mm(ps[3][:, h * 128:(h + 1) * 128], ones_bf[:], STt[:, u, h, :],
                                 start=True, stop=False, r=["const", ("ST", u)], w=[PS(3)])
                            b.mm(ps[3][:, h * 128:(h + 1) * 128], Cbf[:, h, 128:256], qs[:, u, h, :],
                                 start=False, stop=True, r=[("Cbf", h), ("qs", u)], w=[PS(3)])
                        for h in H4:
                            pu = ps[5 + h // 2]
                            o0 = (h % 2) * 256
                            b.mm(pu[0:64, o0:o0 + 128], kh[:, u, h, :], mv[:, tt, h * 128:(h + 1) * 128],
                                 r=[("kh", u, h), ("mv", tt)], w=[PS(5 + h // 2)])
                            b.mm(pu[0:64, o0 + 128:o0 + 256], kh[:, u, h, :], ones_bf[:], r=[("kh", u, h), "const"],
                                 w=[PS(5 + h // 2)])

                    def back_2(tt):
                        c0 = tt * 128
                        u = tt % 2
                        blk = tt // 4
                        b.act(dm[:, u, :, :], ps[3][:].rearrange("p (h t) -> p h t", h=4), AF.Abs,
                              r=[PS(3)], w=[("dm", u)])
                        b.ts("dve", dm[:, u, :, :], dm[:, u, :, :], 1.0, None, ALU.max, r=[("dm", u)], w=[("dm", u)])
                        b.S.add("dve", lambda e, u=u: e.reciprocal(dm[:, u, :, :], dm[:, u, :, :]), [("dm", u)], [("dm", u)])
                        b.tt("dve", hT[:, u, :, :], ps[2][:].rearrange("p (h t) -> p h t", h=4), dm[:, u, :, :], ALU.mult,
                             r=[PS(2), ("dm", u)], w=[("hT", u)])
                        hp = blk % 2
                        b.tt("dve", hst[:, hp, :, (tt % 4) * 128:(tt % 4 + 1) * 128], hT[:, u, :, :],
                             mo[:, :, c0:c0 + 128], ALU.mult, r=[("hT", u), ("mo", blk)], w=[("hst", hp)])
                        if tt % 4 == 3:
                            for h in H4:
                                b.dma("sp", mixT_d[512 + h * 128:512 + (h + 1) * 128, blk * 512:(blk + 1) * 512],
                                      hst[:, hp, h, :], r=[("hst", hp)])
                        for h in H4:
                            pu = ps[5 + h // 2]
                            o0 = (h % 2) * 256
                            b.stt(Cst[:, h, :], Cst[:, h, :], eg[0:64, tt, h:h + 1], pu[0:64, o0:o0 + 256], ALU.mult, ALU.add,
                                  r=[("C", h), "eg", PS(5 + h // 2)], w=[("C", h)])
                        b.cp("act", Cbf[:], Cst[:], r=[("C", h) for h in H4], w=[("Cbf", h) for h in H4])

                    front(0)
                    front_b(0)
                    for tt in range(NT):
                        if tt + 1 < NT:
                            front(tt + 1)
                        back(tt)
                        if tt + 1 < NT:
                            front_b(tt + 1)
                        back_2(tt)
                    S.end_phase()

            if "op" in phases:
                with contextlib.ExitStack() as ph:
                    def pb(name, shp, dt):
                        return ph.enter_context(nc.sbuf_tensor(uniq(name), shp, dt))
                    w_o = pb("w_o", [128, KC, D], BF16)
                    for kc in range(KC):
                        b.dma("pool", w_o[:, kc, :], I["w_out"][l, kc * 128:(kc + 1) * 128, :], w=["w_o"])
                    w_r = pb("w_r", [128, KC, NE], F32)
                    b.dma("sp", w_r[:], I["w_router"][l].rearrange("(k p) e -> p k e", p=128), w=["w_r"])
                    br_bc = pb("br_bc", [128, NE], F32)
                    b.dma("sp", br_bc[:], I["b_router"][l:l + 1, :].partition_broadcast(128), w=["w_r"])
                    g_bc = pb("g_bc", [128, D], F32)
                    b_bc = pb("b_bc", [128, D], F32)
                    b.dma("sp", g_bc[:], I["ln1_g"][l:l + 1, :].partition_broadcast(128), w=["lnp"])
                    b.dma("sp", b_bc[:], I["ln1_b"][l:l + 1, :].partition_broadcast(128), w=["lnp"])
                    mixb = pb("mixb", [128, 2, KC, 512], BF16)
                    xt = pb("xt", [128, 2, D], F32)
                    x1t = pb("x1t", [128, 2, D], F32)
                    x1b = pb("x1b", [128, 2, D], BF16)
                    x1T = pb("x1T", [128, KC, 128], F32)
                    tstat = pb("tstat", [128, 2, 6], F32)
                    tmv = pb("tmv", [128, 2], F32)
                    tr_ = pb("tr_", [128, 2], F32)
                    lg = pb("lg", [128, NE], F32)
                    m8 = pb("m8", [128, 8], F32)
                    msk = pb("msk", [128, NE], F32)
                    mskb = pb("mskb", [128, NE], BF16)
                    ex = pb("ex", [128, NE], F32)
                    G = pb("G", [128, NE], F32)
                    gs = pb("gs", [128, 4], F32)
                    cnt = pb("cnt", [128, NE], F32)
                    pos = pb("pos", [128, NE], F32)
                    okm = pb("okm", [128, NE], F32)
                    val = pb("val", [128, NE], F32)
                    v8 = pb("v8", [128, 8], F32)
                    idf = pb("idf", [128, 4], F32)
                    junk = pb("junk", [128, NE], F32)
                    b.memset("dve", cnt[:], 0.0, w=["cnt"])
                    for blk in range(8):
                        mb = blk % 2
                        b.dma("sp", mixb[:, mb, :, :], mixT_d[:, blk * 512:(blk + 1) * 512].rearrange("(k p) t -> p k t", p=128),
                              w=[("mixb", mb)])
                        for j in range(4):
                            tt = blk * 4 + j
                            u = tt % 2
                            b.dma("sp", xt[:, u, :], xcur[tt * 128:(tt + 1) * 128, :], w=[("xt", u)])
                            for half in range(2):
                                pa = ps[half]
                                for kc in range(KC):
                                    b.mm(pa[:], mixb[:, mb, kc, j * 128:(j + 1) * 128], w_o[:, kc, half * 512:(half + 1) * 512],
                                         start=(kc == 0), stop=(kc == KC - 1), r=[("mixb", mb), "w_o"], w=[PS(half)])
                                b.stt(xt[:, u, half * 512:(half + 1) * 512], xt[:, u, half * 512:(half + 1) * 512], DN_ALPHA,
                                      pa[:], ALU.mult, ALU.add, r=[("xt", u), PS(half)], w=[("xt", u)])
                            layer_norm(xt[:, u, :], g_bc[:], b_bc[:], x1t[:, u, :], ("xt", u), ("x1t", u), tstat, tmv, tr_)
                            b.dma("sp", x1_d[tt * 128:(tt + 1) * 128, :], x1t[:, u, :], r=[("x1t", u)])
                            b.cp("act", x1b[:, u, :], x1t[:, u, :], r=[("x1t", u)], w=[("x1b", u)])
                            for kc in range(KC):
                                b.tr(ps[2 + kc // 4][:, (kc % 4) * 128:(kc % 4 + 1) * 128], x1t[:, u, kc * 128:(kc + 1) * 128],
                                     ident_f[:], r=[("x1t", u), "const"], w=[PS(2 + kc // 4)])
                            b.cp("act", x1T[:, 0:4, :], ps[2][:].rearrange("p (k t) -> p k t", k=4), r=[PS(2)], w=["x1T"])
                            b.cp("dve", x1T[:, 4:8, :], ps[3][:].rearrange("p (k t) -> p k t", k=4), r=[PS(3)], w=["x1T"])
                            for kc in range(KC):
                                b.mm(ps[4][:, 0:NE], x1T[:, kc, :], w_r[:, kc, :], start=(kc == 0), stop=(kc == KC - 1),
                                     r=["x1T", "w_r"], w=[PS(4)])
                            b.tt("dve", lg[:], ps[4][:, 0:NE], br_bc[:], ALU.add, r=[PS(4), "w_r"], w=["lg"])
                            b.S.add("dve", lambda e: e.max(m8[:], lg[:]), ["lg"], ["m8"])
                            b.ts("dve", msk[:], lg[:], m8[:, 3:4], None, ALU.is_ge, r=["lg", "m8"], w=["msk"])
                            b.cp("pool", mskb[:], msk[:], r=["msk"], w=["mskb"])
                            b.ts("dve", ex[:], lg[:], m8[:, 0:1], None, ALU.subtract, r=["lg", "m8"], w=["ex"])
                            b.act(ex[:], ex[:], AF.Exp, r=["ex"], w=["ex"])
                            b.stt(G[:], ex[:], 1.0, msk[:], ALU.mult, ALU.mult, r=["ex", "msk"], w=["G"], accum_out=gs[:, 0:1])
                            b.S.add("dve", lambda e: e.reciprocal(gs[:, 1:2], gs[:, 0:1]), ["G"], ["gs"])
                            b.ts("dve", G[:], G[:], gs[:, 1:2], None, ALU.mult, r=["G", "gs"], w=["G"])
                            b.mm(ps[5][:, 0:NE], tris_bf[:], mskb[:], r=["mskb", "const"], w=[PS(5)])
                            b.mm(ps[5][:, NE:2 * NE], ones_bf[:], mskb[:], r=["mskb", "const"], w=[PS(5)])
                            b.tt("dve", pos[:], ps[5][:, 0:NE], cnt[:], ALU.add, r=[PS(5), "cnt"], w=["pos"])
                            b.tt("dve", cnt[:], ps[5][:, NE:2 * NE], cnt[:], ALU.add, r=[PS(5), "cnt", "pos"], w=["cnt"])
                            b.ts("dve", okm[:], pos[:], float(CAP), None, ALU.is_lt, r=["pos"], w=["okm"])
                            b.tt("dve", okm[:], okm[:], msk[:], ALU.mult, r=["okm", "msk"], w=["okm"])
                            b.tt("dve", val[:], ebase[:], pos[:], ALU.subtract, r=["pos", "const"], w=["val"])
                            b.tt("dve", val[:], val[:], okm[:], ALU.mult, r=["val", "okm"], w=["val"])
                            b.S.add("dve", lambda e: e.max(v8[:], val[:]), ["val"], ["v8"])
                            b.ts("dve", idf[:], v8[:, 0:4], -1.0, float(NROWS), ALU.mult, ALU.add, r=["v8"], w=["idf"])
                            b.cp("dve", idx_all[:, tt, :], idf[:], r=["idf"], w=[("idx", tt)])
                            for k in range(4):
                                b.stt(junk[:], val[:], v8[:, k:k + 1], G[:], ALU.is_equal, ALU.mult,
                                      r=["val", "v8", "G"], w=["junk", ("gate", tt)], accum_out=gate_all[:, tt, k:k + 1])
                                b.scatter(xs_d[:, :], idx_all[:, tt, k:k + 1], x1b[:, u, :],
                                          r=[("idx", tt), ("x1b", u)])
                    S.end_phase()

            if "ex" in phases:
                with contextlib.ExitStack() as ph:
                    def pb(name, shp, dt):
                        return ph.enter_context(nc.sbuf_tensor(uniq(name), shp, dt))
                    NJ = CAP // 128
                    NCH = 12
                    wgu = pb("wgu", [128, 2, KC, 2048], BF16)
                    wdn = pb("wdn", [128, 2, KC, D], BF16)
                    wst = pb("wst", [128, 3, 2048], F32)
                    bgu = pb("bgu", [128, 2, 16], F32)
                    bgu1 = pb("bgu1", [128, 2, 8], F32)
                    bdn = pb("bdn", [128, 2, D], F32)
                    xst = pb("xst", [128, 2, D], BF16)
                    xsT = pb("xsT", [128, 2, KC, CAP], BF16)
                    gsb = pb("gsb", [128, 2, CAP], F32)
                    sg = pb("sg", [128, 2, CAP], F32)
                    glu = pb("glu", [128, 8, CAP], BF16)
                    ub = pb("ub", [128, 2, CAP], F32)
                    aT = pb("aT", [128, 8, CAP], BF16)
                    yt = pb("yt", [128, 2, D], F32)
                    NG = NE * NCH

                    def dma_chunk(g):
                        if g >= NG:
                            return
                        e, i = divmod(g, NCH)
                        slot = g % 3
                        if i < 8:
                            b.dma("sp", wst[:, slot, :], I["w_gu"][l, e, i * 128:(i + 1) * 128, :], w=[("wst", slot)])
                        else:
                            kp = i - 8
                            b.dma("sp", wst[:, slot, :].rearrange("p (k d) -> p k d", k=2),
                                  I["w_down"][l, e, kp * 256:(kp + 1) * 256, :].rearrange("(k p) d -> p k d", p=128),
                                  w=[("wst", slot)])

                    def cast_chunk(g):
                        if g >= NG:
                            return
                        e, i = divmod(g, NCH)
                        slot = g % 3
                        wb = e % 2
                        eng = "act" if g % 2 == 0 else "dve"
                        if i < 8:
                            b.cp(eng, wgu[:, wb, i, :], wst[:, slot, :], r=[("wst", slot)], w=[("wgu", wb, i)])
                        else:
                            kp = i - 8
                            b.cp(eng, wdn[:, wb, 2 * kp:2 * kp + 2, :], wst[:, slot, :].rearrange("p (k d) -> p k d", k=2),
                                 r=[("wst", slot)], w=[("wdn", wb, kp)])
                        dma_chunk(g + 3)

                    def load_bias(e):
                        wb = e % 2
                        b.dma("sp", bgu[:, wb, :], I["b_gu"][l, e:e + 1, :].rearrange("o (c p) -> p (o c)", p=128),
                              w=[("bgu", wb)], nc_ok=True)
                        b.dma("sp", bdn[:, wb, :], I["b_down"][l, e:e + 1, :].partition_broadcast(128), w=[("bdn", wb)])
                        b.ts("pool", bgu1[:, wb, :], bgu[:, wb, 8:16], 1.0, None, ALU.add, r=[("bgu", wb)], w=[("bgu1", wb)])

                    def load_x(e):
                        xb = e % 2
                        for j in range(NJ):
                            u = j % 2
                            b.dma("sp", xst[:, u, :], xs_d[e * CAP + j * 128:e * CAP + (j + 1) * 128, :],
                                  w=[("xst", u)])
                            pst = ps[6 + u][:].bitcast(BF16)
                            for kc in range(KC):
                                b.tr(pst[:, kc * 128:(kc + 1) * 128], xst[:, u, kc * 128:(kc + 1) * 128], ident_bf[:],
                                     r=[("xst", u), "const"], w=[PS(6 + u)])
                            b.evac(xsT[:, xb, :, j * 128:(j + 1) * 128], pst.rearrange("p (k t) -> p k t", k=KC),
                                   r=[PS(6 + u)], w=[("xsT", xb)])

                    for g in range(3):
                        dma_chunk(g)
                    load_bias(0)
                    load_x(0)
                    for g in range(NCH):
                        cast_chunk(g)
                    halves = [(0, 512), (512, CAP)]
                    for e in range(NE):
                        wb = e % 2
                        xb = e % 2
                        if e + 1 < NE:
                            load_bias(e + 1)
                        for fc in range(16):
                            u = fc % 2
                            pbig = ps_big[fc % 2]
                            bk = [PS((fc % 2) * 2), PS((fc % 2) * 2 + 1)]
                            for hi, (c0, c1) in enumerate(halves):
                                for kc in range(KC):
                                    b.mm(pbig[:, c0:c1], wgu[:, wb, kc, fc * 128:(fc + 1) * 128], xsT[:, xb, kc, c0:c1],
                                         start=(kc == 0), stop=(kc == KC - 1), r=[("wgu", wb, kc), ("xsT", xb)],
                                         w=[bk[hi]])
                            if fc < 8:
                                b.ts("dve", gsb[:, u, :], pbig[:, 0:CAP], bgu[:, wb, fc:fc + 1], 7.0, ALU.add, ALU.min,
                                     r=bk + [("bgu", wb)], w=[("gsb", u)])
                                b.act(sg[:, u, :], gsb[:, u, :], AF.Sigmoid, scale=1.702, r=[("gsb", u)], w=[("sg", u)])
                                b.tt("pool", glu[:, fc, :], gsb[:, u, :], sg[:, u, :], ALU.mult,
                                     r=[("gsb", u), ("sg", u)], w=[("glu", fc)])
                            else:
                                b.act(ub[:, u, :], pbig[:, 0:CAP], AF.Identity, bias=bgu1[:, wb, fc - 8:fc - 7],
                                      r=bk + [("bgu1", wb)], w=[("ub", u)])
                                b.ts("dve", ub[:, u, :], ub[:, u, :], 8.0, -6.0, ALU.min, ALU.max, r=[("ub", u)], w=[("ub", u)])
                                b.tt("pool", aT[:, fc - 8, :], ub[:, u, :], glu[:, fc - 8, :], ALU.mult,
                                     r=[("ub", u), ("glu", fc - 8)], w=[("aT", fc - 8)])
                            if e + 1 < NE and fc < NCH:
                                cast_chunk((e + 1) * NCH + fc)
                        if e + 1 < NE:
                            load_x(e + 1)
                        for j in range(NJ):
                            u = j % 2
                            for half in range(2):
                                pa = ps[4 + half]
                                for fc in range(8):
                                    b.mm(pa[:], aT[:, fc, j * 128:(j + 1) * 128], wdn[:, wb, fc, half * 512:(half + 1) * 512],
                                         start=(fc == 0), stop=(fc == 7), r=[("aT", fc), ("wdn", wb, fc // 2)], w=[PS(4 + half)])
                                b.tt("dve", yt[:, u, half * 512:(half + 1) * 512], pa[:], bdn[:, wb, half * 512:(half + 1) * 512],
                                     ALU.add, r=[PS(4 + half), ("bdn", wb)], w=[("yt", u)])
                            b.dma("pool", ys_d[e * CAP + j * 128:e * CAP + (j + 1) * 128, :], yt[:, u, :],
                                  r=[("yt", u)])
                    S.end_phase()

            if "cb" in phases:
                with contextlib.ExitStack() as ph:
                    def pb(name, shp, dt):
                        return ph.enter_context(nc.sbuf_tensor(uniq(name), shp, dt))
                    g_bc = pb("g2_bc", [128, D], F32)
                    b_bc = pb("b2_bc", [128, D], F32)
                    b.dma("sp", g_bc[:], I["ln2_g"][l:l + 1, :].partition_broadcast(128), w=["lnp"])
                    b.dma("sp", b_bc[:], I["ln2_b"][l:l + 1, :].partition_broadcast(128), w=["lnp"])
                    xt = pb("xt2", [128, 2, D], F32)
                    rows = pb("rows", [128, 2, 4, D], F32)
                    xo = pb("xo", [128, 2, D], F32)
                    tstat = pb("tstat2", [128, 2, 6], F32)
                    tmv = pb("tmv2", [128, 2], F32)
                    tr_ = pb("tr2_", [128, 2], F32)
                    def cb_load(tt):
                        u = tt % 2
                        b.dma("sp", xt[:, u, :], x1_d[tt * 128:(tt + 1) * 128, :], w=[("xt", u)])
                        for k in range(4):
                            b.gather(rows[:, u, k, :], ys_d[:, :], idx_all[:, tt, k:k + 1],
                                     r=[("idx", tt)], w=[("rows", u, k)])

                    cb_load(0)
                    for tt in range(NT):
                        u = tt % 2
                        if tt + 1 < NT:
                            cb_load(tt + 1)
                        b.ts("dve", xt[:, u, :], xt[:, u, :], DN_ALPHA, None, ALU.mult, r=[("xt", u)], w=[("xt", u)])
                        for k in range(4):
                            b.stt(xt[:, u, :], rows[:, u, k, :], gate_all[:, tt, k:k + 1], xt[:, u, :], ALU.mult, ALU.add,
                                  r=[("rows", u, k), ("gate", tt), ("xt", u)], w=[("xt", u)])
                        layer_norm(xt[:, u, :], g_bc[:], b_bc[:], xo[:, u, :], ("xt", u), ("xo", u), tstat, tmv, tr_, aff="dve")
                        b.dma("sp", xnext[tt * 128:(tt + 1) * 128, :], xo[:, u, :], r=[("xo", u)])
                    S.end_phase()
            xcur = xnext
        S.end_phase(final=True)
    return nc


_CACHE = {}


def kernel(**inputs):
    if "nc" not in _CACHE:
        _CACHE["nc"] = build()
    nc = _CACHE["nc"]
    consts = host_consts()
    x = np.ascontiguousarray(inputs["x"], dtype=np.float32)
    ncores = x.shape[0]
    shared = {k: np.ascontiguousarray(np.asarray(v, dtype=np.float32)) for k, v in inputs.items() if k != "x"}
    in_maps = []
    for c in range(ncores):
        m = dict(shared)
        m.update(consts)
        m["x"] = x[c]
        in_maps.append(m)
    res = run_bass_kernel_spmd(nc, in_maps, core_ids=list(range(ncores)))
    return np.stack([r["y"] for r in res.results], axis=0).astype(np.float32)
```

```python
import contextlib
import math
import numpy as np
import ml_dtypes
import concourse.bass as bass
import concourse.mybir as mybir
from concourse.bass_utils import run_bass_kernel_spmd

F32 = mybir.dt.float32
BF16 = mybir.dt.bfloat16
I32 = mybir.dt.int32
ALU = mybir.AluOpType
AF = mybir.ActivationFunctionType
AX = mybir.AxisListType

ENGS = ("pe", "act", "dve", "pool", "sp")
NDMASEM = 8
SEM_EPOCH = 30000
DMA_EPOCH = 1800

DEPTH = 4
T = 4096
NT = 32
D = 1024
KC = 8
IN_W = 3080
NE = 32
CAP = 640
NROWS = NE * CAP
DN_ALPHA = (2 * DEPTH) ** 0.25
LN_EPS = 1e-5
RMS_EPS = 1e-5


class Op:
    __slots__ = ("eng", "emit", "deps", "isdma", "sig", "sem", "val", "pre", "idx")


class Sched:
    def __init__(self, nc):
        self.nc = nc
        self.ops = {e: [] for e in ENGS}
        self.track = {}
        self.ndma = {e: 0 for e in ENGS}
        self.fence = []
        self.fenced = set(ENGS)

    def barrier(self):
        f = []
        for e in ENGS:
            last = None
            dm = []
            for op in reversed(self.ops[e]):
                if op.isdma:
                    if len(dm) < NDMASEM:
                        dm.append(op)
                elif last is None:
                    last = op
                if last is not None and len(dm) >= NDMASEM:
                    break
            if last is not None:
                f.append(last)
            f.extend(dm)
        self.fence = f
        self.fenced = set()

    def add(self, eng, emit, reads=(), writes=(), dma=False):
        op = Op()
        op.eng = eng
        op.emit = emit
        op.isdma = dma
        op.sig = False
        op.sem = None
        op.val = 0
        op.pre = None
        op.idx = len(self.ops[eng])
        deps = {}

        def need(p, raw):
            if p is op:
                return
            if p.isdma or p.eng != eng or dma:
                deps[id(p)] = p
            elif raw and eng != "pe":
                deps[id(p)] = p

        if eng not in self.fenced:
            self.fenced.add(eng)
            for p in self.fence:
                if p.isdma or p.eng != eng:
                    deps[id(p)] = p
        for k in reads:
            t = self.track.get(k)
            if t is None:
                t = [None, {}]
                self.track[k] = t
            if t[0] is not None:
                need(t[0], True)
        for k in writes:
            t = self.track.get(k)
            if t is None:
                t = [None, {}]
                self.track[k] = t
            if t[0] is not None:
                need(t[0], False)
            for r in t[1].values():
                need(r, False)
        for k in reads:
            t = self.track[k]
            key = (eng, op.idx) if dma else eng
            t[1][key] = op
        for k in writes:
            t = self.track[k]
            t[0] = op
            t[1] = {}
        op.deps = list(deps.values())
        if dma:
            j = self.ndma[eng]
            self.ndma[eng] = j + 1
            r = j // NDMASEM
            ep = r // DMA_EPOCH
            op.sem = ("dma", eng, j % NDMASEM, ep)
            op.val = 16 * (r % DMA_EPOCH + 1)
            if j >= NDMASEM:
                rp = r - 1
                op.pre = (("dma", eng, j % NDMASEM, rp // DMA_EPOCH), 16 * (rp % DMA_EPOCH + 1))
        self.ops[eng].append(op)
        return op

    def end_phase(self, final=False):
        nc = self.nc
        if not hasattr(self, "upto"):
            self.upto = {e: 0 for e in ENGS}
            self.semcount = {e: 0 for e in ENGS}
            self.sems = {}
            self.waited = {e: {} for e in ENGS}
        self.barrier()
        for p in self.fence:
            if not p.isdma:
                p.sig = True
        for e in ENGS:
            for op in self.ops[e][self.upto[e]:]:
                for p in op.deps:
                    if not p.isdma and p.idx >= self.upto[p.eng]:
                        p.sig = True
        for e in ENGS:
            c = self.semcount[e]
            for op in self.ops[e][self.upto[e]:]:
                if (not op.isdma) and op.sig:
                    op.sem = ("c", e, c // SEM_EPOCH)
                    op.val = c % SEM_EPOCH + 1
                    c += 1
            self.semcount[e] = c

        def getsem(k):
            if k not in self.sems:
                self.sems[k] = self.semstack.enter_context(nc.semaphore("s%d" % len(self.sems)))
            return self.sems[k]

        with nc.Block() as block:
            def replay(e, eng):
                waited = self.waited[e]
                for op in self.ops[e][self.upto[e]:]:
                    ws = [(p.sem, p.val) for p in op.deps if p.sem is not None]
                    if op.pre is not None:
                        ws.append(op.pre)
                    for s_, v in ws:
                        if waited.get(s_, 0) < v:
                            eng.wait_ge(getsem(s_), v)
                            waited[s_] = v
                    ins = op.emit(eng)
                    if op.isdma:
                        ins.then_inc(getsem(op.sem), 16)
                    elif op.sig:
                        ins.then_inc(getsem(op.sem), 1)
                    op.emit = None
                if final and e == "sp":
                    for q in ENGS:
                        dm = [op for op in self.ops[q] if op.isdma][-NDMASEM:]
                        for op in dm:
                            if waited.get(op.sem, 0) < op.val:
                                eng.wait_ge(getsem(op.sem), op.val)
                                waited[op.sem] = op.val
                self.upto[e] = len(self.ops[e])

            @block.tensor
            def _(eng):
                replay("pe", eng)

            @block.scalar
            def _(eng):
                replay("act", eng)

            @block.vector
            def _(eng):
                replay("dve", eng)

            @block.gpsimd
            def _(eng):
                replay("pool", eng)

            @block.sync
            def _(eng):
                replay("sp", eng)


class B:
    def __init__(self, nc):
        self.nc = nc
        self.S = Sched(nc)
        self.rr = 0

    def mm(self, out, lhsT, rhs, start=True, stop=True, r=(), w=()):
        self.S.add("pe", lambda e: e.matmul(out, lhsT, rhs, start=start, stop=stop), r, w)

    def tr(self, out, in_, ident, r=(), w=()):
        self.S.add("pe", lambda e: e.transpose(out, in_, ident), r, w)

    def act(self, out, in_, func, bias=0.0, scale=1.0, r=(), w=(), accum_out=None):
        if accum_out is None:
            self.S.add("act", lambda e: e.activation(out, in_, func, bias=bias, scale=scale), r, w)
        else:
            self.S.add("act", lambda e: e.activation(out, in_, func, bias=bias, scale=scale,
                                                     accum_out=accum_out), r, w)

    def ts(self, eng, out, in0, s1, s2, op0, op1=None, r=(), w=(), accum_out=None):
        if op1 is None:
            self.S.add(eng, lambda e: e.tensor_scalar(out, in0, s1, None, op0), r, w)
        elif accum_out is None:
            self.S.add(eng, lambda e: e.tensor_scalar(out, in0, s1, s2, op0, op1), r, w)
        else:
            self.S.add(eng, lambda e: e.tensor_scalar(out, in0, s1, s2, op0, op1, accum_out), r, w)

    def tt(self, eng, out, in0, in1, op, r=(), w=()):
        self.S.add(eng, lambda e: e.tensor_tensor(out, in0, in1, op), r, w)

    def stt(self, out, in0, scalar, in1, op0, op1, r=(), w=(), accum_out=None):
        if accum_out is None:
            self.S.add("dve", lambda e: e.scalar_tensor_tensor(out, in0, scalar, in1, op0, op1), r, w)
        else:
            self.S.add("dve", lambda e: e.scalar_tensor_tensor(out, in0, scalar, in1, op0, op1,
                                                               accum_out), r, w)

    def cp(self, eng, out, in_, r=(), w=()):
        if eng == "act":
            self.S.add("act", lambda e: e.copy(out, in_), r, w)
        else:
            self.S.add(eng, lambda e: e.tensor_copy(out, in_), r, w)

    def evac(self, out, in_, r=(), w=()):
        self.rr ^= 1
        self.cp("act" if self.rr else "dve", out, in_, r, w)

    def memset(self, eng, ap, val, r=(), w=()):
        self.S.add(eng, lambda e: e.memset(ap, val), r, w)

    def dma(self, eng, out, in_, r=(), w=(), nc_ok=False):
        if nc_ok:
            self.S.add(eng, lambda e: e.dma_start(out=out, in_=in_, allow_slow_non_contiguous=True),
                       r, w, dma=True)
        else:
            self.S.add(eng, lambda e: e.dma_start(out=out, in_=in_), r, w, dma=True)

    def scatter(self, out_dram, idx, in_sb, r=(), w=()):
        self.S.add("pool", lambda e: e.indirect_dma_start(
            out=out_dram, out_offset=bass.IndirectOffsetOnAxis(ap=idx, axis=0),
            in_=in_sb, in_offset=None), r, w, dma=True)

    def gather(self, out_sb, in_dram, idx, r=(), w=()):
        self.S.add("pool", lambda e: e.indirect_dma_start(
            out=out_sb, out_offset=None, in_=in_dram,
            in_offset=bass.IndirectOffsetOnAxis(ap=idx, axis=0),
            ), r, w, dma=True)


def host_consts():
    c = {}
    c["ident_bf"] = np.eye(128, dtype=np.float32).astype(ml_dtypes.bfloat16)
    c["ident_f"] = np.eye(128, dtype=np.float32)
    tri = (np.arange(128)[:, None] <= np.arange(128)[None, :]).astype(np.float32)
    c["tri_bf"] = tri.astype(ml_dtypes.bfloat16)
    c["tri_f"] = tri
    c["tris_bf"] = (np.arange(128)[:, None] < np.arange(128)[None, :]).astype(np.float32).astype(ml_dtypes.bfloat16)
    c["ones_bf"] = np.ones((128, 128), dtype=ml_dtypes.bfloat16)
    c["ones_f"] = np.ones((128, 128), dtype=np.float32)
    c["ebase"] = np.broadcast_to((NROWS - np.arange(NE) * CAP).astype(np.float32)[None, :], (128, NE)).copy()
    return c


CONST_SPECS = [("ident_bf", BF16, [128, 128]), ("ident_f", F32, [128, 128]), ("tri_bf", BF16, [128, 128]),
               ("tri_f", F32, [128, 128]), ("tris_bf", BF16, [128, 128]), ("ones_bf", BF16, [128, 128]),
               ("ones_f", F32, [128, 128]), ("ebase", F32, [128, NE])]

IN_SPECS = [("x", [T, D]), ("w_in", [DEPTH, D, IN_W]), ("conv_w", [DEPTH, 4, 512]), ("conv_b", [DEPTH, 512]),
            ("gate_b", [DEPTH, 8]), ("lam_q1", [DEPTH, 64]), ("lam_k1", [DEPTH, 64]), ("lam_q2", [DEPTH, 64]),
            ("lam_k2", [DEPTH, 64]), ("da_norm_g", [DEPTH, 128]), ("w_out", [DEPTH, D, D]),
            ("ln1_g", [DEPTH, D]), ("ln1_b", [DEPTH, D]), ("w_router", [DEPTH, D, NE]), ("b_router", [DEPTH, NE]),
            ("w_gu", [DEPTH, NE, D, 2048]), ("b_gu", [DEPTH, NE, 2048]), ("w_down", [DEPTH, NE, D, D]),
            ("b_down", [DEPTH, NE, D]), ("ln2_g", [DEPTH, D]), ("ln2_b", [DEPTH, D])]


def build(nlayers=DEPTH, phases=("da", "ml", "op", "ex", "cb"), debug=()):
    nc = bass.Bass("TRN2", target_bir_lowering=False)
    I = {}
    for name, shp in IN_SPECS:
        I[name] = nc.dram_tensor(name, shp, F32, kind="ExternalInput").ap()
    for name, dt, shp in CONST_SPECS:
        I[name] = nc.dram_tensor(name, shp, dt, kind="ExternalInput").ap()
    y_out = nc.dram_tensor("y", [T, D], F32, kind="ExternalOutput").ap()

    def scratch(name, shp, dt):
        kind = "ExternalOutput" if name in debug else "Internal"
        return nc.dram_tensor(name, shp, dt, kind=kind).ap()

    mixT_d = scratch("mixT_d", [D, T], BF16)
    x1_d = scratch("x1_d", [T, D], F32)
    xa_d = scratch("xa_d", [T, D], F32)
    xb_d = scratch("xb_d", [T, D], F32)
    xs_d = scratch("xs_d", [NROWS + 1, D], BF16)
    ys_d = scratch("ys_d", [NROWS + 1, D], F32)

    b = B(nc)
    S = b.S
    _cnt = [0]

    def uniq(name):
        _cnt[0] += 1
        return "sb%d_%s" % (_cnt[0], name)
    st = contextlib.ExitStack()
    with st:
        S.semstack = st
        def sb(name, shp, dt):
            return st.enter_context(nc.sbuf_tensor(uniq(name), shp, dt))

        ident_bf = sb("ident_bf", [128, 128], BF16)
        ident_f = sb("ident_f", [128, 128], F32)
        tri_bf = sb("tri_bf", [128, 128], BF16)
        tri_f = sb("tri_f", [128, 128], F32)
        tris_bf = sb("tris_bf", [128, 128], BF16)
        ones_bf = sb("ones_bf", [128, 128], BF16)
        ones_f = sb("ones_f", [128, 128], F32)
        ebase = sb("ebase", [128, NE], F32)
        for name, t_ in (("ident_bf", ident_bf), ("ident_f", ident_f), ("tri_bf", tri_bf), ("tri_f", tri_f),
                         ("tris_bf", tris_bf), ("ones_bf", ones_bf), ("ones_f", ones_f), ("ebase", ebase)):
            b.dma("sp", t_[:], I[name], w=["const"])
        idx_all = sb("idx_all", [128, NT, 4], I32)
        gate_all = sb("gate_all", [128, NT, 4], F32)
        with nc.sbuf_tensor("sb_zrow", [1, D], F32) as zrow:
            b.memset("dve", zrow[:], 0.0, w=["zrow"])
            b.dma("sp", ys_d[NROWS:NROWS + 1, :], zrow[:], r=["zrow"])

        ps_big = [st.enter_context(nc.psum_tensor("psb%d" % i, [128, 1024], F32)) for i in range(4)]
        ps = [ps_big[i // 2][:, (i % 2) * 512:(i % 2 + 1) * 512] for i in range(8)]

        def PS(i):
            return ("ps", i)

        def stream_xT(xsrc, blk, xstage, xT_blk, tag, nbuf=2):
            buf = blk % nbuf
            for j in range(4):
                tt = blk * 4 + j
                sbuf_i = tt % 2
                b.dma("pool", xstage[:, sbuf_i, :], xsrc[tt * 128:(tt + 1) * 128, :],
                      w=[("xstage", sbuf_i)])
                pst = ps[7][:].bitcast(BF16)
                for kc in range(KC):
                    b.tr(pst[:, kc * 128:(kc + 1) * 128], xstage[:, sbuf_i, kc * 128:(kc + 1) * 128], ident_bf[:],
                         r=[("xstage", sbuf_i), "const"], w=[PS(7)])
                b.evac(xT_blk[:, buf, :, j * 128:(j + 1) * 128],
                       pst.rearrange("p (k t) -> p k t", k=KC), r=[PS(7)], w=[(tag, buf)])

        def layer_norm(xt, g_bc, b_bc, out, keyin, keyout, tmpstat, tmpmv, tmpr, aff="pool"):
            b.S.add("dve", lambda e: e.bn_stats(tmpstat[:, 0, :], xt[:, 0:512]), [keyin], ["lnstat"])
            b.S.add("dve", lambda e: e.bn_stats(tmpstat[:, 1, :], xt[:, 512:1024]), [keyin], ["lnstat"])
            b.S.add("dve", lambda e: e.bn_aggr(tmpmv[:], tmpstat[:].rearrange("p a b -> p (a b)")), ["lnstat"], ["lnmv"])
            b.act(tmpr[:, 0:1], tmpmv[:, 1:2], AF.Ln, bias=epsc[:, 0:1], scale=1.0, r=["lnmv", "epsc"], w=["lnr0"])
            b.act(tmpr[:, 1:2], tmpr[:, 0:1], AF.Exp, scale=-0.5, r=["lnr0"], w=["lnr1"])
            b.ts("dve", xt, xt, tmpmv[:, 0:1], tmpr[:, 1:2], ALU.subtract, ALU.mult, r=[keyin, "lnmv", "lnr1"], w=[keyin])
            b.tt(aff, xt, xt, g_bc, ALU.mult, r=[keyin, "lnp"], w=[keyin])
            b.tt(aff, out, xt, b_bc, ALU.add, r=[keyin, "lnp"], w=[keyout])

        epsc = sb("epsc", [128, 2], F32)
        b.memset("dve", epsc[:, 0:1], LN_EPS, w=["epsc"])
        b.memset("dve", epsc[:, 1:2], 128.0 * RMS_EPS, w=["epsc"])

        xcur = I["x"]
        for l in range(nlayers):
            lam_init = 0.8 - 0.6 * math.exp(-0.3 * l)
            xnext = y_out if l == nlayers - 1 else (xa_d if l % 2 == 0 else xb_d)
            if "da" in phases:
                with contextlib.ExitStack() as ph:
                    def pb(name, shp, dt):
                        return ph.enter_context(nc.sbuf_tensor(uniq(name), shp, dt))
                    w_da = pb("w_da", [128, KC, 1536], BF16)
                    for kc in range(KC):
                        b.dma("pool", w_da[:, kc, :], I["w_in"][l, kc * 128:(kc + 1) * 128, 0:1536], w=["w_da"])
                    xstage = pb("xstage", [128, 2, D], BF16)
                    xT_blk = pb("xT_blk", [128, 2, KC, 512], BF16)
                    qT = pb("qT", [128, 4, T], BF16)
                    kT = pb("kT", [128, 4, T], BF16)
                    Vd = pb("Vd", [128, NT, 512], BF16)
                    lamt = pb("lamt", [128, 4, 64], F32)
                    lamv = pb("lamv", [128, 8], F32)
                    gcol = pb("gcol", [128, 2], F32)
                    for i, nm in enumerate(("lam_q1", "lam_k1", "lam_q2", "lam_k2")):
                        b.dma("sp", lamt[:, i, :], I[nm][l:l + 1, :].partition_broadcast(128), w=["lamt"])
                    b.dma("sp", gcol[:, 0:1], I["da_norm_g"][l:l + 1, :].rearrange("o p -> p o"), w=["gcol"], nc_ok=True)
                    b.tt("dve", lamt[:, 0, :], lamt[:, 0, :], lamt[:, 1, :], ALU.mult, r=["lamt"], w=["lamt"])
                    b.tt("dve", lamt[:, 2, :], lamt[:, 2, :], lamt[:, 3, :], ALU.mult, r=["lamt"], w=["lamt"])
                    b.S.add("dve", lambda e: e.reduce_sum(lamv[:, 0:1], lamt[:, 0, :], AX.X), ["lamt"], ["lamv"])
                    b.S.add("dve", lambda e: e.reduce_sum(lamv[:, 1:2], lamt[:, 2, :], AX.X), ["lamt"], ["lamv"])
                    b.act(lamv[:, 2:4], lamv[:, 0:2], AF.Exp, r=["lamv"], w=["lamv2"])
                    b.tt("dve", lamv[:, 4:5], lamv[:, 3:4], lamv[:, 2:3], ALU.subtract, r=["lamv2"], w=["lamv3"])
                    b.ts("dve", lamv[:, 5:6], lamv[:, 4:5], -lam_init, None, ALU.add, r=["lamv3"], w=["neglam"])
                    neglam = lamv[:, 5:6]
                    b.ts("dve", gcol[:, 1:2], gcol[:, 0:1], (1.0 - lam_init) * math.sqrt(128.0), None, ALU.mult,
                         r=["gcol"], w=["gcol2"])
                    for blk in range(8):
                        stream_xT(xcur, blk, xstage, xT_blk, "xT")
                        buf = blk % 2
                        for grp in range(8):
                            pa = ps[grp % 2]
                            for kc in range(KC):
                                b.mm(pa[:], w_da[:, kc, grp * 128:(grp + 1) * 128], xT_blk[:, buf, kc, :],
                                     start=(kc == 0), stop=(kc == KC - 1), r=["w_da", ("xT", buf)], w=[PS(grp % 2)])
                            dst = qT if grp < 4 else kT
                            b.evac(dst[:, grp % 4, blk * 512:(blk + 1) * 512], pa[:], r=[PS(grp % 2)],
                                   w=[("q" if grp < 4 else "k", blk)])
                        for j in range(4):
                            tt = blk * 4 + j
                            pa = ps[j % 2]
                            for kc in range(KC):
                                b.mm(pa[:], xT_blk[:, buf, kc, j * 128:(j + 1) * 128], w_da[:, kc, 1024:1536],
                                     start=(kc == 0), stop=(kc == KC - 1), r=["w_da", ("xT", buf)], w=[PS(j % 2)])
                            b.evac(Vd[:, tt, :], pa[:], r=[PS(j % 2)], w=[("v", tt)])
                    Pt = pb("Pt", [128, 2, 3, 512], BF16)
                    acc = pb("acc", [128, 2, 2, 512], F32)
                    Osb = pb("Osb", [128, 2, 512], F32)
                    Lsb = pb("Lsb", [128, 2, 512], F32)
                    Rc = pb("Rc", [128, 2, 512], F32)
                    oc = pb("oc", [128, 2, 512], F32)
                    sq = pb("sq", [128, 2, 512], F32)
                    rs = pb("rs", [128, 512], F32)
                    ob = pb("ob", [128, 2, 512], BF16)
                    items = []
                    for Qb in range(8):
                        for h in range(4):
                            nk = (Qb + 1) * 4
                            for kt in range(nk):
                                items.append((Qb, h, kt, nk))
                    ACC_ENG = ("pool", "dve")

                    def emit_S(i):
                        Qb, h, kt, nk = items[i]
                        diag = kt >= Qb * 4
                        q0 = (kt - Qb * 4) * 128 if diag else 0
                        n = 512 - q0
                        for c in range(2):
                            lo = c * 64
                            sbank = 2 * (i % 2) + c
                            b.mm(ps[sbank][:, 0:n], kT[lo:lo + 64, h, kt * 128:(kt + 1) * 128],
                                 qT[lo:lo + 64, h, Qb * 512 + q0:(Qb + 1) * 512],
                                 r=[("k", kt // 4), ("q", Qb)], w=[PS(sbank)])

                    def epi_0(Qb, h):
                        for c in range(2):
                            b.cp("act", Osb[:, c, :], ps[4 + c][:], r=[PS(4 + c)], w=[("Osb", c)])

                    def epi_1(Qb, h):
                        gp = (Qb * 4 + h) % 2
                        for c in range(2):
                            b.mm(ps[6 + c][:], ones_f[:], acc[:, c, gp, :], r=["const", ("acc", c, gp)], w=[PS(6 + c)])
                        for c in range(2):
                            b.act(Lsb[:, c, :], ps[6 + c][:], AF.Ln, r=[PS(6 + c)], w=[("Lsb", c)])
                            b.act(Lsb[:, c, :], Lsb[:, c, :], AF.Exp, scale=-1.0, r=[("Lsb", c)], w=[("Lsb", c)])

                    def epi_2(Qb, h):
                        gp = (Qb * 4 + h) % 2
                        for c in range(2):
                            b.tt("dve", Rc[:, c, :], Osb[:, c, :], Lsb[:, c, :], ALU.mult, r=[("Osb", c), ("Lsb", c)], w=[("Rc", c)])
                        b.stt(oc[:, gp, :], Rc[:, 1, :], neglam, Rc[:, 0, :], ALU.mult, ALU.add,
                              r=[("Rc", 0), ("Rc", 1), "neglam"], w=[("oc", gp)])
                        b.act(sq[:, gp, :], oc[:, gp, :], AF.Square, r=[("oc", gp)], w=[("sq", gp)])

                    def epi_3(Qb, h):
                        gp = (Qb * 4 + h) % 2
                        b.mm(ps[6][:], ones_f[:], sq[:, gp, :], r=[("sq", gp), "const"], w=[PS(6)])
                        b.act(rs[:], ps[6][:], AF.Ln, bias=epsc[:, 1:2], r=[PS(6), "epsc"], w=["rs"])
                        b.act(rs[:], rs[:], AF.Exp, scale=-0.5, r=["rs"], w=["rs"])
                        b.stt(ob[:, gp, :], oc[:, gp, :], gcol[:, 1:2], rs[:], ALU.mult, ALU.mult,
                              r=[("oc", gp), "rs", "gcol2"], w=[("ob", gp)])
                        b.dma("sp", mixT_d[h * 128:(h + 1) * 128, Qb * 512:(Qb + 1) * 512], ob[:, gp, :],
                              r=[("ob", gp)])

                    pend = []
                    emit_S(0)
                    for i, (Qb, h, kt, nk) in enumerate(items):
                        if i + 1 < len(items):
                            emit_S(i + 1)
                        gp = (Qb * 4 + h) % 2
                        diag = kt >= Qb * 4
                        q0 = (kt - Qb * 4) * 128 if diag else 0
                        n = 512 - q0
                        pbuf = i % 3
                        if kt == 0:
                            b.memset("pool", acc[:, 0, gp, :], 0.0, w=[("acc", 0, gp)])
                            b.memset("dve", acc[:, 1, gp, :], 0.0, w=[("acc", 1, gp)])
                        for c in range(2):
                            sbank = 2 * (i % 2) + c
                            b.act(Pt[:, c, pbuf, 0:n], ps[sbank][:, 0:n], AF.Exp, scale=0.125,
                                  r=[PS(sbank)], w=[("Pt", c, pbuf)])
                            if diag:
                                b.tt("pool", Pt[:, c, pbuf, 0:128], Pt[:, c, pbuf, 0:128], tri_bf[:], ALU.mult,
                                     r=[("Pt", c, pbuf), "const"], w=[("Pt", c, pbuf)])
                            b.mm(ps[4 + c][:, q0:512], Vd[:, kt, h * 128:(h + 1) * 128], Pt[:, c, pbuf, 0:n],
                                 start=(kt == 0), stop=(kt == nk - 1), r=[("v", kt), ("Pt", c, pbuf)], w=[PS(4 + c)])
                            b.tt(ACC_ENG[c], acc[:, c, gp, q0:512], acc[:, c, gp, q0:512], Pt[:, c, pbuf, 0:n], ALU.add,
                                 r=[("acc", c, gp), ("Pt", c, pbuf)], w=[("acc", c, gp)])
                        while pend and (pend[0][0] <= kt or kt == nk - 1):
                            _, fn, args = pend.pop(0)
                            fn(*args)
                        if kt == nk - 1:
                            epi_0(Qb, h)
                            pend = [(2, epi_1, (Qb, h)), (5, epi_2, (Qb, h)), (8, epi_3, (Qb, h))]
                    for _, fn, args in pend:
                        fn(*args)
                    S.end_phase()

            if "ml" in phases:
                with contextlib.ExitStack() as ph:
                    def pb(name, shp, dt):
                        return ph.enter_context(nc.sbuf_tensor(uniq(name), shp, dt))
                    mq = pb("mq", [64, 4, T], BF16)
                    mk = pb("mk", [64, 4, T], BF16)
                    mv = pb("mv", [128, NT, 512], BF16)
                    mo = pb("mo", [128, 4, T], BF16)
                    gtm = pb("gtm", [128, NT, 8], F32)
                    lf = pb("lf", [128, NT, 4], F32)
                    btm = pb("btm", [128, NT, 4], F32)
                    gbc = pb("gbc", [128, NT, 4], F32)
                    ew = pb("ew", [128, NT, 4], F32)
                    ebias = pb("ebias", [128, NT, 4], F32)
                    eg = pb("eg", [128, NT, 4], F32)
                    nl8 = pb("nl8", [128, 2], F32)
                    b.memset("dve", nl8[:, 0:1], -math.log(8.0), w=["nl8"])
                    b.memset("dve", nl8[:, 1:2], 1.0, w=["nl8"])
                    with contextlib.ExitStack() as ph2:
                        def pb2(name, shp, dt):
                            return ph2.enter_context(nc.sbuf_tensor(uniq(name), shp, dt))
                        w_ml = pb2("w_ml", [128, KC, 1544], BF16)
                        for kc in range(KC):
                            b.dma("pool", w_ml[:, kc, :], I["w_in"][l, kc * 128:(kc + 1) * 128, 1536:3080], w=["w_ml"])
                        xstage = pb2("xstage", [128, 2, D], BF16)
                        xT_blk = pb2("xT_blk", [128, 1, KC, 512], BF16)
                        gb_bc = pb2("gb_bc", [128, 8], F32)
                        b.dma("sp", gb_bc[:], I["gate_b"][l:l + 1, :].partition_broadcast(128), w=["gb_bc"])
                        cw = pb2("cw", [64, 4, 8], F32)
                        cb = pb2("cb", [64, 8], F32)
                        for j in range(4):
                            b.dma("sp", cw[:, j, :], I["conv_w"][l, j:j + 1, :].rearrange("o (g p) -> p (o g)", p=64), w=["cw"], nc_ok=True)
                        b.dma("sp", cb[:], I["conv_b"][l:l + 1, :].rearrange("o (g p) -> p (o g)", p=64), w=["cw"], nc_ok=True)
                        cin = pb2("cin", [64, 8, 515], F32)
                        ctmp = pb2("ctmp", [64, 2, 512], F32)
                        b.memset("dve", cin[:, :, 0:3], 0.0, w=[("cin", g) for g in range(8)])
                        for blk in range(8):
                            stream_xT(xcur, blk, xstage, xT_blk, "xT", nbuf=1)
                            buf = 0
                            for grp in range(8):
                                pa = ps[grp % 2]
                                for kc in range(KC):
                                    b.mm(pa[0:64, :], w_ml[:, kc, grp * 64:(grp + 1) * 64], xT_blk[:, buf, kc, :],
                                         start=(kc == 0), stop=(kc == KC - 1), r=["w_ml", ("xT", buf)], w=[PS(grp % 2)])
                                b.cp("act", cin[:, grp, 3:515], pa[0:64, :], r=[PS(grp % 2)], w=[("cin", grp)])
                                ct = ctmp[:, grp % 2, :]
                                b.ts("dve", ct, cin[:, grp, 0:512], cw[:, 0, grp:grp + 1], cb[:, grp:grp + 1], ALU.mult, ALU.add,
                                     r=[("cin", grp), "cw"], w=[("ctmp", grp % 2)])
                                for j in range(1, 4):
                                    b.stt(ct, cin[:, grp, j:j + 512], cw[:, j, grp:grp + 1], ct, ALU.mult, ALU.add,
                                          r=[("cin", grp), "cw", ("ctmp", grp % 2)], w=[("ctmp", grp % 2)])
                                dst = mq if grp < 4 else mk
                                b.act(dst[:, grp % 4, blk * 512:(blk + 1) * 512], ct, AF.Silu,
                                      r=[("ctmp", grp % 2)], w=[("mqk", blk)])
                                b.cp("pool", cin[:, grp, 0:3], cin[:, grp, 512:515], r=[("cin", grp)], w=[("cin", grp)])
                            for h in range(4):
                                pa = ps[2 + h % 2]
                                for kc in range(KC):
                                    b.mm(pa[:], w_ml[:, kc, 1024 + h * 128:1024 + (h + 1) * 128], xT_blk[:, buf, kc, :],
                                         start=(kc == 0), stop=(kc == KC - 1), r=["w_ml", ("xT", buf)], w=[PS(2 + h % 2)])
                                b.act(mo[:, h, blk * 512:(blk + 1) * 512], pa[:], AF.Sigmoid, r=[PS(2 + h % 2)], w=[("mo", blk)])
                            for j in range(4):
                                tt = blk * 4 + j
                                pa = ps[4 + j % 2]
                                for kc in range(KC):
                                    b.mm(pa[:], xT_blk[:, buf, kc, j * 128:(j + 1) * 128], w_ml[:, kc, 512:1024],
                                         start=(kc == 0), stop=(kc == KC - 1), r=["w_ml", ("xT", buf)], w=[PS(4 + j % 2)])
                                b.evac(mv[:, tt, :], pa[:], r=[PS(4 + j % 2)], w=[("mv", tt)])
                                for kc in range(KC):
                                    b.mm(ps[6][:, 0:8], xT_blk[:, buf, kc, j * 128:(j + 1) * 128], w_ml[:, kc, 1536:1544],
                                         start=(kc == 0), stop=(kc == KC - 1), r=["w_ml", ("xT", buf)], w=[PS(6)])
                                b.tt("dve", gtm[:, tt, :], ps[6][:, 0:8], gb_bc[:], ALU.add, r=[PS(6), "gb_bc"], w=["gtm"])
                    S.barrier()
                    b.act(lf[:], gtm[:, :, 4:8], AF.Exp, scale=-1.0, r=["gtm"], w=["lf"])
                    b.act(lf[:], lf[:], AF.Ln, bias=nl8[:, 1:2], r=["lf", "nl8"], w=["lf"])
                    b.ts("dve", lf[:], lf[:], -1.0, None, ALU.mult, r=["lf"], w=["lf"])
                    lf2 = lf[:].rearrange("p a b -> p (a b)")
                    b.mm(ps[6][:, 0:128], tri_f[:], lf2, r=["lf", "const"], w=[PS(6)])
                    b.cp("dve", btm[:].rearrange("p a b -> p (a b)"), ps[6][:, 0:128], r=[PS(6)], w=["btm"])
                    b.mm(ps[6][:, 128:256], ones_f[:], lf2, r=["lf", "const"], w=[PS(6)])
                    b.cp("dve", gbc[:].rearrange("p a b -> p (a b)"), ps[6][:, 128:256], r=[PS(6)], w=["gbc"])
                    b.tt("dve", ebias[:], gtm[:, :, 0:4], btm[:], ALU.subtract, r=["gtm", "btm"], w=["ebias"])
                    b.tt("dve", ew[:], ebias[:], gbc[:], ALU.add, r=["ebias", "gbc"], w=["ew"])
                    b.act(ew[:], ew[:], AF.Exp, r=["ew"], w=["ew"])
                    b.ts("dve", ebias[:], ebias[:], -math.log(8.0), None, ALU.add, r=["ebias", "ew"], w=["ebias2"])
                    b.act(eg[:], gbc[:], AF.Exp, r=["gbc"], w=["eg"])
                    Cst = pb("Cst", [64, 4, 256], F32)
                    Cbf = pb("Cbf", [64, 4, 256], BF16)
                    b.memset("dve", Cst[:], 0.0, w=[("C", h) for h in range(4)])
                    b.memset("pool", Cbf[:], 0.0, w=[("Cbf", h) for h in range(4)])
                    lfd = pb("lfd", [128, 2, 4, 128], F32)
                    Et = pb("Et", [128, 2, 4, 128], F32)
                    Eq = pb("Eq", [64, 2, 4, 128], F32)
                    qs = pb("qs", [64, 2, 4, 128], BF16)
                    STt = pb("STt", [128, 2, 4, 128], BF16)
                    kh = pb("kh", [128, 2, 4, 64], BF16)
                    dm = pb("dm", [128, 2, 4, 128], F32)
                    hT = pb("hT", [128, 2, 4, 128], F32)
                    hst = pb("hst", [128, 2, 4, 512], BF16)
                    H4 = range(4)

                    def front(tt):
                        c0 = tt * 128
                        u = tt % 2
                        blk = tt // 4
                        sb_ = 1 if u == 0 else 7
                        for h in H4:
                            b.ts("dve", lfd[:, u, h, :], ones_f[:], lf[:, tt, h:h + 1], None, ALU.mult,
                                 r=["lf", "const"], w=[("lfd", u, h)])
                        for h in H4:
                            b.mm(ps[0][:, h * 128:(h + 1) * 128], lfd[:, u, h, :], tri_f[:], r=[("lfd", u, h), "const"], w=[PS(0)])
                        for h in H4:
                            b.mm(ps[sb_][:, h * 128:(h + 1) * 128], mk[:, h, c0:c0 + 128], mq[:, h, c0:c0 + 128],
                                 r=[("mqk", blk)], w=[PS(sb_)])
                        pkt = ps[4][:].bitcast(BF16)
                        for h in H4:
                            b.tr(pkt[:, h * 64:(h + 1) * 64], mk[:, h, c0:c0 + 128], ident_bf[0:64, 0:64],
                                 r=[("mqk", blk), "const"], w=[PS(4)])
                        for h in H4:
                            b.act(Et[:, u, h, :], ps[0][:, h * 128:(h + 1) * 128], AF.Exp, bias=ebias[:, tt, h:h + 1],
                                  r=[PS(0), "ebias2"], w=[("Et", u, h)])
                        b.act(Eq[:, u, :, :], ps[0][0:64, :].rearrange("p (h t) -> p h t", h=4), AF.Exp, bias=nl8[0:64, 0:1],
                              r=[PS(0), "nl8"], w=[("Eq", u)])
                        for h in H4:
                            b.tt("pool", Et[:, u, h, :], Et[:, u, h, :], tri_f[:], ALU.mult, r=[("Et", u, h), "const"], w=[("Et", u, h)])

                    def front_b(tt):
                        c0 = tt * 128
                        u = tt % 2
                        blk = tt // 4
                        pkt = ps[4][:].bitcast(BF16)
                        b.tt("dve", qs[:, u, :, :], mq[:, :, c0:c0 + 128], Eq[:, u, :, :], ALU.mult,
                             r=[("mqk", blk), ("Eq", u)], w=[("qs", u)])
                        for h in H4:
                            b.ts("dve", kh[:, u, h, :], pkt[:, h * 64:(h + 1) * 64], ew[:, tt, h:h + 1], None, ALU.mult,
                                 r=[PS(4), "ew"], w=[("kh", u, h)])

                    def back(tt):
                        c0 = tt * 128
                        u = tt % 2
                        blk = tt // 4
                        sb_ = 1 if u == 0 else 7
                        b.tt("dve", STt[:, u, :, :], ps[sb_][:].rearrange("p (h t) -> p h t", h=4), Et[:, u, :, :], ALU.mult,
                             r=[PS(sb_)] + [("Et", u, h) for h in H4], w=[("ST", u)])
                        for h in H4:
                            b.mm(ps[2][:, h * 128:(h + 1) * 128], mv[:, tt, h * 128:(h + 1) * 128], STt[:, u, h, :],
                                 start=True, stop=False, r=[("mv", tt), ("ST", u)], w=[PS(2)])
                            b.mm(ps[2][:, h * 128:(h + 1) * 128], Cbf[:, h, 0:128], qs[:, u, h, :],
                                 start=False, stop=True, r=[("Cbf", h), ("qs", u)], w=[PS(2)])
                        for h in H4:
                            b.mm(ps[3][:, h * 128:(h + 1) * 128], ones_bf[:], STt[:, u, h, :],
                                 start=True, stop=False, r=["const", ("ST", u)], w=[PS(3)])
                            b.mm(ps[3][:, h * 128:(h + 1) * 128], Cbf[:, h, 128:256], qs[:, u, h, :],
                                 start=False, stop=True, r=[("Cbf", h), ("qs", u)], w=[PS(3)])
                        for h in H4:
                            pu = ps[5 + h // 2]
                            o0 = (h % 2) * 256
                            b.mm(pu[0:64, o0:o0 + 128], kh[:, u, h, :], mv[:, tt, h * 128:(h + 1) * 128],
                                 r=[("kh", u, h), ("mv", tt)], w=[PS(5 + h // 2)])
                            b.mm(pu[0:64, o0 + 128:o0 + 256], kh[:, u, h, :], ones_bf[:], r=[("kh", u, h), "const"],
                                 w=[PS(5 + h // 2)])

                    def back_2(tt):
                        c0 = tt * 128
                        u = tt % 2
                        blk = tt // 4
                        b.act(dm[:, u, :, :], ps[3][:].rearrange("p (h t) -> p h t", h=4), AF.Abs,
                              r=[PS(3)], w=[("dm", u)])
                        b.ts("dve", dm[:, u, :, :], dm[:, u, :, :], 1.0, None, ALU.max, r=[("dm", u)], w=[("dm", u)])
                        b.act(dm[:, u, :, :], dm[:, u, :, :], AF.Ln, r=[("dm", u)], w=[("dm", u)])
                        b.act(dm[:, u, :, :], dm[:, u, :, :], AF.Exp, scale=-1.0, r=[("dm", u)], w=[("dm", u)])
                        b.tt("dve", hT[:, u, :, :], ps[2][:].rearrange("p (h t) -> p h t", h=4), dm[:, u, :, :], ALU.mult,
                             r=[PS(2), ("dm", u)], w=[("hT", u)])
                        hp = blk % 2
                        b.tt("dve", hst[:, hp, :, (tt % 4) * 128:(tt % 4 + 1) * 128], hT[:, u, :, :],
                             mo[:, :, c0:c0 + 128], ALU.mult, r=[("hT", u), ("mo", blk)], w=[("hst", hp)])
                        if tt % 4 == 3:
                            for h in H4:
                                b.dma("sp", mixT_d[512 + h * 128:512 + (h + 1) * 128, blk * 512:(blk + 1) * 512],
                                      hst[:, hp, h, :], r=[("hst", hp)])
                        for h in H4:
                            pu = ps[5 + h // 2]
                            o0 = (h % 2) * 256
                            b.stt(Cst[:, h, :], Cst[:, h, :], eg[0:64, tt, h:h + 1], pu[0:64, o0:o0 + 256], ALU.mult, ALU.add,
                                  r=[("C", h), "eg", PS(5 + h // 2)], w=[("C", h)])
                        b.cp("act", Cbf[:], Cst[:], r=[("C", h) for h in H4], w=[("Cbf", h) for h in H4])

                    front(0)
                    front_b(0)
                    for tt in range(NT):
                        if tt + 1 < NT:
                            front(tt + 1)
                        back(tt)
                        if tt + 1 < NT:
                            front_b(tt + 1)
                        back_2(tt)
                    S.end_phase()

            if "op" in phases:
                with contextlib.ExitStack() as ph:
                    def pb(name, shp, dt):
                        return ph.enter_context(nc.sbuf_tensor(uniq(name), shp, dt))
                    w_o = pb("w_o", [128, KC, D], BF16)
                    for kc in range(KC):
                        b.dma("pool", w_o[:, kc, :], I["w_out"][l, kc * 128:(kc + 1) * 128, :], w=["w_o"])
                    w_r = pb("w_r", [128, KC, NE], F32)
                    b.dma("sp", w_r[:], I["w_router"][l].rearrange("(k p) e -> p k e", p=128), w=["w_r"])
                    br_bc = pb("br_bc", [128, NE], F32)
                    b.dma("sp", br_bc[:], I["b_router"][l:l + 1, :].partition_broadcast(128), w=["w_r"])
                    g_bc = pb("g_bc", [128, D], F32)
                    b_bc = pb("b_bc", [128, D], F32)
                    b.dma("sp", g_bc[:], I["ln1_g"][l:l + 1, :].partition_broadcast(128), w=["lnp"])
                    b.dma("sp", b_bc[:], I["ln1_b"][l:l + 1, :].partition_broadcast(128), w=["lnp"])
                    mixb = pb("mixb", [128, 2, KC, 512], BF16)
                    xt = pb("xt", [128, 2, D], F32)
                    x1t = pb("x1t", [128, 2, D], F32)
                    x1b = pb("x1b", [128, 2, D], BF16)
                    x1T = pb("x1T", [128, KC, 128], F32)
                    tstat = pb("tstat", [128, 2, 6], F32)
                    tmv = pb("tmv", [128, 2], F32)
                    tr_ = pb("tr_", [128, 2], F32)
                    lg = pb("lg", [128, NE], F32)
                    m8 = pb("m8", [128, 8], F32)
                    msk = pb("msk", [128, NE], F32)
                    mskb = pb("mskb", [128, NE], BF16)
                    ex = pb("ex", [128, NE], F32)
                    G = pb("G", [128, NE], F32)
                    gs = pb("gs", [128, 4], F32)
                    cnt = pb("cnt", [128, NE], F32)
                    pos = pb("pos", [128, NE], F32)
                    okm = pb("okm", [128, NE], F32)
                    val = pb("val", [128, NE], F32)
                    v8 = pb("v8", [128, 8], F32)
                    idf = pb("idf", [128, 4], F32)
                    junk = pb("junk", [128, NE], F32)
                    b.memset("dve", cnt[:], 0.0, w=["cnt"])
                    lg2 = pb("lg2", [128, 2, NE], F32)

                    def opA(tt):
                        blk, j = divmod(tt, 4)
                        mb = blk % 2
                        u = tt % 2
                        if j == 0:
                            b.dma("sp", mixb[:, mb, :, :], mixT_d[:, blk * 512:(blk + 1) * 512].rearrange("(k p) t -> p k t", p=128),
                                  w=[("mixb", mb)])
                        b.dma("sp", xt[:, u, :], xcur[tt * 128:(tt + 1) * 128, :], w=[("xt", u)])
                        for half in range(2):
                            pa = ps[half]
                            for kc in range(KC):
                                b.mm(pa[:], mixb[:, mb, kc, j * 128:(j + 1) * 128], w_o[:, kc, half * 512:(half + 1) * 512],
                                     start=(kc == 0), stop=(kc == KC - 1), r=[("mixb", mb), "w_o"], w=[PS(half)])
                            b.stt(xt[:, u, half * 512:(half + 1) * 512], xt[:, u, half * 512:(half + 1) * 512], DN_ALPHA,
                                  pa[:], ALU.mult, ALU.add, r=[("xt", u), PS(half)], w=[("xt", u)])
                        layer_norm(xt[:, u, :], g_bc[:], b_bc[:], x1t[:, u, :], ("xt", u), ("x1t", u), tstat, tmv, tr_, aff="dve")
                        b.dma("sp", x1_d[tt * 128:(tt + 1) * 128, :], x1t[:, u, :], r=[("x1t", u)])
                        b.cp("act", x1b[:, u, :], x1t[:, u, :], r=[("x1t", u)], w=[("x1b", u)])
                        for kc in range(KC):
                            b.tr(ps[2 + kc // 4][:, (kc % 4) * 128:(kc % 4 + 1) * 128], x1t[:, u, kc * 128:(kc + 1) * 128],
                                 ident_f[:], r=[("x1t", u), "const"], w=[PS(2 + kc // 4)])
                        b.cp("act", x1T[:, 0:4, :], ps[2][:].rearrange("p (k t) -> p k t", k=4), r=[PS(2)], w=["x1T"])
                        b.cp("dve", x1T[:, 4:8, :], ps[3][:].rearrange("p (k t) -> p k t", k=4), r=[PS(3)], w=["x1T"])
                        for kc in range(KC):
                            b.mm(ps[4][:, 0:NE], x1T[:, kc, :], w_r[:, kc, :], start=(kc == 0), stop=(kc == KC - 1),
                                 r=["x1T", "w_r"], w=[PS(4)])
                        b.tt("dve", lg2[:, u, :], ps[4][:, 0:NE], br_bc[:], ALU.add, r=[PS(4), "w_r"], w=[("lg", u)])

                    def opB(tt):
                        u = tt % 2
                        lg = lg2[:, u, :]
                        lk = ("lg", u)
                        b.S.add("dve", lambda e: e.max(m8[:], lg), [lk], ["m8"])
                        b.ts("dve", msk[:], lg, m8[:, 3:4], None, ALU.is_ge, r=[lk, "m8"], w=["msk"])
                        b.cp("pool", mskb[:], msk[:], r=["msk"], w=["mskb"])
                        b.ts("dve", ex[:], lg, m8[:, 0:1], None, ALU.subtract, r=[lk, "m8"], w=["ex"])
                        b.act(ex[:], ex[:], AF.Exp, r=["ex"], w=["ex"])
                        b.stt(G[:], ex[:], 1.0, msk[:], ALU.mult, ALU.mult, r=["ex", "msk"], w=["G"], accum_out=gs[:, 0:1])
                        b.S.add("dve", lambda e: e.reciprocal(gs[:, 1:2], gs[:, 0:1]), ["G"], ["gs"])
                        b.ts("dve", G[:], G[:], gs[:, 1:2], None, ALU.mult, r=["G", "gs"], w=["G"])
                        b.mm(ps[5][:, 0:NE], tris_bf[:], mskb[:], r=["mskb", "const"], w=[PS(5)])
                        b.mm(ps[5][:, NE:2 * NE], ones_bf[:], mskb[:], r=["mskb", "const"], w=[PS(5)])
                        b.tt("dve", pos[:], ps[5][:, 0:NE], cnt[:], ALU.add, r=[PS(5), "cnt"], w=["pos"])
                        b.tt("dve", cnt[:], ps[5][:, NE:2 * NE], cnt[:], ALU.add, r=[PS(5), "cnt", "pos"], w=["cnt"])
                        b.ts("dve", okm[:], pos[:], float(CAP), None, ALU.is_lt, r=["pos"], w=["okm"])
                        b.tt("dve", okm[:], okm[:], msk[:], ALU.mult, r=["okm", "msk"], w=["okm"])
                        b.tt("dve", val[:], ebase[:], pos[:], ALU.subtract, r=["pos", "const"], w=["val"])
                        b.tt("dve", val[:], val[:], okm[:], ALU.mult, r=["val", "okm"], w=["val"])
                        b.S.add("dve", lambda e: e.max(v8[:], val[:]), ["val"], ["v8"])
                        b.ts("dve", idf[:], v8[:, 0:4], -1.0, float(NROWS), ALU.mult, ALU.add, r=["v8"], w=["idf"])
                        b.cp("dve", idx_all[:, tt, :], idf[:], r=["idf"], w=[("idx", tt)])
                        for k in range(4):
                            b.stt(junk[:], val[:], v8[:, k:k + 1], G[:], ALU.is_equal, ALU.mult,
                                  r=["val", "v8", "G"], w=["junk", ("gate", tt)], accum_out=gate_all[:, tt, k:k + 1])
                            b.scatter(xs_d[:, :], idx_all[:, tt, k:k + 1], x1b[:, u, :],
                                      r=[("idx", tt), ("x1b", u)])

                    opA(0)
                    for tt in range(NT):
                        if tt + 1 < NT:
                            opA(tt + 1)
                        opB(tt)
                    S.end_phase()

            if "ex" in phases:
                with contextlib.ExitStack() as ph:
                    def pb(name, shp, dt):
                        return ph.enter_context(nc.sbuf_tensor(uniq(name), shp, dt))
                    NJ = CAP // 128
                    NCH = 12
                    wgu = pb("wgu", [128, 2, KC, 2048], BF16)
                    wdn = pb("wdn", [128, 2, KC, D], BF16)
                    wst = pb("wst", [128, 3, 2048], F32)
                    bgu = pb("bgu", [128, 2, 16], F32)
                    bgu1 = pb("bgu1", [128, 2, 8], F32)
                    bdn = pb("bdn", [128, 2, D], F32)
                    xst = pb("xst", [128, 2, D], BF16)
                    xsT = pb("xsT", [128, 2, KC, CAP], BF16)
                    gsb = pb("gsb", [128, 2, CAP], F32)
                    sg = pb("sg", [128, 2, CAP], F32)
                    glu = pb("glu", [128, 8, CAP], BF16)
                    ub = pb("ub", [128, 2, CAP], F32)
                    aT = pb("aT", [128, 8, CAP], BF16)
                    yt = pb("yt", [128, 2, D], F32)
                    NG = NE * NCH

                    def dma_chunk(g):
                        if g >= NG:
                            return
                        e, i = divmod(g, NCH)
                        slot = g % 3
                        if i < 8:
                            b.dma("sp", wst[:, slot, :], I["w_gu"][l, e, i * 128:(i + 1) * 128, :], w=[("wst", slot)])
                        else:
                            kp = i - 8
                            b.dma("sp", wst[:, slot, :].rearrange("p (k d) -> p k d", k=2),
                                  I["w_down"][l, e, kp * 256:(kp + 1) * 256, :].rearrange("(k p) d -> p k d", p=128),
                                  w=[("wst", slot)])

                    def cast_chunk(g):
                        if g >= NG:
                            return
                        e, i = divmod(g, NCH)
                        slot = g % 3
                        wb = e % 2
                        eng = "act" if g % 2 == 0 else "dve"
                        if i < 8:
                            b.cp(eng, wgu[:, wb, i, :], wst[:, slot, :], r=[("wst", slot)], w=[("wgu", wb, i)])
                        else:
                            kp = i - 8
                            b.cp(eng, wdn[:, wb, 2 * kp:2 * kp + 2, :], wst[:, slot, :].rearrange("p (k d) -> p k d", k=2),
                                 r=[("wst", slot)], w=[("wdn", wb, kp)])
                        dma_chunk(g + 3)

                    def load_bias(e):
                        wb = e % 2
                        b.dma("sp", bgu[:, wb, :], I["b_gu"][l, e:e + 1, :].rearrange("o (c p) -> p (o c)", p=128),
                              w=[("bgu", wb)], nc_ok=True)
                        b.dma("sp", bdn[:, wb, :], I["b_down"][l, e:e + 1, :].partition_broadcast(128), w=[("bdn", wb)])
                        b.ts("pool", bgu1[:, wb, :], bgu[:, wb, 8:16], 1.0, None, ALU.add, r=[("bgu", wb)], w=[("bgu1", wb)])

                    def load_x(e):
                        xb = e % 2
                        for j in range(NJ):
                            u = j % 2
                            b.dma("sp", xst[:, u, :], xs_d[e * CAP + j * 128:e * CAP + (j + 1) * 128, :],
                                  w=[("xst", u)])
                            pst = ps[6 + u][:].bitcast(BF16)
                            for kc in range(KC):
                                b.tr(pst[:, kc * 128:(kc + 1) * 128], xst[:, u, kc * 128:(kc + 1) * 128], ident_bf[:],
                                     r=[("xst", u), "const"], w=[PS(6 + u)])
                            b.evac(xsT[:, xb, :, j * 128:(j + 1) * 128], pst.rearrange("p (k t) -> p k t", k=KC),
                                   r=[PS(6 + u)], w=[("xsT", xb)])

                    for g in range(3):
                        dma_chunk(g)
                    load_bias(0)
                    load_x(0)
                    for g in range(NCH):
                        cast_chunk(g)
                    halves = [(0, 512), (512, CAP)]
                    for e in range(NE):
                        wb = e % 2
                        xb = e % 2
                        if e + 1 < NE:
                            load_bias(e + 1)
                        for fc in range(16):
                            u = fc % 2
                            pbig = ps_big[fc % 2]
                            bk = [PS((fc % 2) * 2), PS((fc % 2) * 2 + 1)]
                            for hi, (c0, c1) in enumerate(halves):
                                for kc in range(KC):
                                    b.mm(pbig[:, c0:c1], wgu[:, wb, kc, fc * 128:(fc + 1) * 128], xsT[:, xb, kc, c0:c1],
                                         start=(kc == 0), stop=(kc == KC - 1), r=[("wgu", wb, kc), ("xsT", xb)],
                                         w=[bk[hi]])
                            if fc < 8:
                                b.ts("dve", gsb[:, u, :], pbig[:, 0:CAP], bgu[:, wb, fc:fc + 1], 7.0, ALU.add, ALU.min,
                                     r=bk + [("bgu", wb)], w=[("gsb", u)])
                                b.act(sg[:, u, :], gsb[:, u, :], AF.Sigmoid, scale=1.702, r=[("gsb", u)], w=[("sg", u)])
                                b.tt("pool", glu[:, fc, :], gsb[:, u, :], sg[:, u, :], ALU.mult,
                                     r=[("gsb", u), ("sg", u)], w=[("glu", fc)])
                            else:
                                b.act(ub[:, u, :], pbig[:, 0:CAP], AF.Identity, bias=bgu1[:, wb, fc - 8:fc - 7],
                                      r=bk + [("bgu1", wb)], w=[("ub", u)])
                                b.ts("dve", ub[:, u, :], ub[:, u, :], 8.0, -6.0, ALU.min, ALU.max, r=[("ub", u)], w=[("ub", u)])
                                b.tt("pool", aT[:, fc - 8, :], ub[:, u, :], glu[:, fc - 8, :], ALU.mult,
                                     r=[("ub", u), ("glu", fc - 8)], w=[("aT", fc - 8)])
                            if e + 1 < NE and fc < NCH:
                                cast_chunk((e + 1) * NCH + fc)
                        if e + 1 < NE:
                            load_x(e + 1)
                        for j in range(NJ):
                            u = j % 2
                            for half in range(2):
                                pa = ps[4 + half]
                                for fc in range(8):
                                    b.mm(pa[:], aT[:, fc, j * 128:(j + 1) * 128], wdn[:, wb, fc, half * 512:(half + 1) * 512],
                                         start=(fc == 0), stop=(fc == 7), r=[("aT", fc), ("wdn", wb, fc // 2)], w=[PS(4 + half)])
                                b.tt("dve", yt[:, u, half * 512:(half + 1) * 512], pa[:], bdn[:, wb, half * 512:(half + 1) * 512],
                                     ALU.add, r=[PS(4 + half), ("bdn", wb)], w=[("yt", u)])
                            b.dma("pool", ys_d[e * CAP + j * 128:e * CAP + (j + 1) * 128, :], yt[:, u, :],
                                  r=[("yt", u)])
                    S.end_phase()

            if "cb" in phases:
                with contextlib.ExitStack() as ph:
                    def pb(name, shp, dt):
                        return ph.enter_context(nc.sbuf_tensor(uniq(name), shp, dt))
                    g_bc = pb("g2_bc", [128, D], F32)
                    b_bc = pb("b2_bc", [128, D], F32)
                    b.dma("sp", g_bc[:], I["ln2_g"][l:l + 1, :].partition_broadcast(128), w=["lnp"])
                    b.dma("sp", b_bc[:], I["ln2_b"][l:l + 1, :].partition_broadcast(128), w=["lnp"])
                    xt = pb("xt2", [128, 2, D], F32)
                    rows = pb("rows", [128, 2, 4, D], F32)
                    xo = pb("xo", [128, 2, D], F32)
                    tstat = pb("tstat2", [128, 2, 6], F32)
                    tmv = pb("tmv2", [128, 2], F32)
                    tr_ = pb("tr2_", [128, 2], F32)
                    def cb_load(tt):
                        u = tt % 2
                        b.dma("sp", xt[:, u, :], x1_d[tt * 128:(tt + 1) * 128, :], w=[("xt", u)])
                        for k in range(4):
                            b.gather(rows[:, u, k, :], ys_d[:, :], idx_all[:, tt, k:k + 1],
                                     r=[("idx", tt)], w=[("rows", u, k)])

                    cb_load(0)
                    for tt in range(NT):
                        u = tt % 2
                        if tt + 1 < NT:
                            cb_load(tt + 1)
                        b.ts("dve", xt[:, u, :], xt[:, u, :], DN_ALPHA, None, ALU.mult, r=[("xt", u)], w=[("xt", u)])
                        for k in range(4):
                            b.stt(xt[:, u, :], rows[:, u, k, :], gate_all[:, tt, k:k + 1], xt[:, u, :], ALU.mult, ALU.add,
                                  r=[("rows", u, k), ("gate", tt), ("xt", u)], w=[("xt", u)])
                        layer_norm(xt[:, u, :], g_bc[:], b_bc[:], xo[:, u, :], ("xt", u), ("xo", u), tstat, tmv, tr_, aff="dve")
                        b.dma("sp", xnext[tt * 128:(tt + 1) * 128, :], xo[:, u, :], r=[("xo", u)])
                    S.end_phase()
            xcur = xnext
        S.end_phase(final=True)
    return nc


_CACHE = {}


def kernel(**inputs):
    if "nc" not in _CACHE:
        _CACHE["nc"] = build()
    nc = _CACHE["nc"]
    consts = host_consts()
    x = np.ascontiguousarray(inputs["x"], dtype=np.float32)
    ncores = x.shape[0]
    shared = {k: np.ascontiguousarray(np.asarray(v, dtype=np.float32)) for k, v in inputs.items() if k != "x"}
    in_maps = []
    for c in range(ncores):
        m = dict(shared)
        m.update(consts)
        m["x"] = x[c]
        in_maps.append(m)
    res = run_bass_kernel_spmd(nc, in_maps, core_ids=list(range(ncores)))
    return np.stack([r["y"] for r in res.results], axis=0).astype(np.float32)
```
